# Optimizing a Trainium2 kernel written in Bass

```python
import math
import jax
import jax.numpy as jnp
from jax import lax
import numpy as np

D_MODEL = 1024
BATCH = 4
SEQ = 8192
DEPTH = 2

GRID_W = 64
CTX_LEN = 256
N_MIXERS = 2
N_HY_LAYERS = (DEPTH + N_MIXERS - 1) // N_MIXERS
N_ML_LAYERS = DEPTH // N_MIXERS
EPS = 1e-6
SHORT_W = 3

HY_EMB_BANDS = 16
HY_EMB_DIM = 1 + 2 * HY_EMB_BANDS
HY_FILTER_WIDTH = 64
HY_MAX_DECAY = math.log(1e-2) / 0.3
HY_MIN_DECAY = math.log(1e-2) / 1.5

ML_INNER = 2 * D_MODEL
ML_HEADS = 4
ML_HEAD_DIM = ML_INNER // ML_HEADS
ML_QKV_BLOCK = 4
ML_N_BLOCKS = ML_INNER // ML_QKV_BLOCK
ML_CHUNK = 128
ML_NORM_EPS = 1e-5

FFN_HIDDEN = 2816

kernel_name = 'hyena_mlstm_prefix_dit'


def _f32(a):
    return a.astype(jnp.float32)


def rmsnorm(x, g):
    x32 = _f32(x)
    y = x32 * lax.rsqrt(jnp.mean(x32 * x32, axis=-1, keepdims=True) + EPS)
    return (y * _f32(g)).astype(x.dtype)


def modulate(h, shift, scale):
    return h * (1.0 + scale) + shift


def adaln_params(cond, w, b):
    mod = jax.nn.silu(cond) @ w + b
    return jnp.split(mod[..., None, :], 6, axis=-1)


def short_conv(x, w, b):
    xp = jnp.pad(x, ((0, 0), (1, 1), (0, 0)))
    return w[0] * xp[:, :-2] + w[1] * xp[:, 1:-1] + w[2] * xp[:, 2:] + b


def grid_dwconv(x, w, b, rows, cols):
    B, L, C = x.shape
    img = x.reshape(B, rows, cols, C)
    y = lax.conv_general_dilated(img, w.astype(x.dtype)[:, :, None, :], window_strides=(1, 1),
                                 padding='SAME', dimension_numbers=('NHWC', 'HWIO', 'NHWC'),
                                 feature_group_count=C)
    return y.reshape(B, L, C) + b


def conv_ffn(u, w_up, conv_w, conv_b, w_down, rows, cols):
    a, g = jnp.split(u @ w_up, 2, axis=-1)
    g = grid_dwconv(g, conv_w, conv_b, rows, cols)
    return (jax.nn.silu(g) * a) @ w_down


def hyena_filters(L, f_w1, f_b1, f_w2, f_b2, f_w3, f_b3, f_wout, freq):
    t = jnp.linspace(0.0, 1.0, L, dtype=jnp.float32)[:, None]
    w = 2.0 * math.pi * jnp.arange(L, dtype=jnp.float32)[:, None] / L
    bands = jnp.linspace(1e-4, HY_EMB_BANDS - 1, HY_EMB_BANDS, dtype=jnp.float32)[None, :]
    pos = jnp.concatenate([t, jnp.cos(bands * w), -jnp.sin(bands * w)], axis=-1)
    fr = _f32(freq)
    h = jnp.sin(fr * (pos @ _f32(f_w1) + _f32(f_b1)))
    h = jnp.sin(fr * (h @ _f32(f_w2) + _f32(f_b2)))
    h = jnp.sin(fr * (h @ _f32(f_w3) + _f32(f_b3)))
    h = (h @ _f32(f_wout)).reshape(L, 2, D_MODEL)
    deltas = jnp.linspace(HY_MIN_DECAY, HY_MAX_DECAY, D_MODEL, dtype=jnp.float32)
    h = h * jnp.exp(-t * jnp.abs(deltas))[:, None, :]
    kern = jnp.concatenate([h[:, 0], jnp.zeros((1, D_MODEL), jnp.float32), h[:0:-1, 1]], axis=0)
    return kern / jnp.sum(jnp.abs(kern), axis=0, keepdims=True)


def hyena_mix(u, p):
    (w_in, b_in, sc_w, sc_b, f_w1, f_b1, f_w2, f_b2, f_w3, f_b3, f_wout, freq,
     bias, w_out, b_out) = p
    L = u.shape[1]
    proj = short_conv(u @ w_in + b_in, sc_w, sc_b)
    x0, x1, v = jnp.split(proj, 3, axis=-1)
    kern = hyena_filters(L, f_w1, f_b1, f_w2, f_b2, f_w3, f_b3, f_wout, freq)
    z = _f32(x1 * v)
    y = jnp.fft.irfft(jnp.fft.rfft(z, n=2 * L, axis=1) * jnp.fft.rfft(kern, n=2 * L, axis=0)[None],
                      n=2 * L, axis=1)[:, :L]
    y = (y + z * _f32(bias)).astype(u.dtype)
    return (x0 * y) @ w_out + b_out


def headwise(a, w):
    B, L, _ = a.shape
    out = jnp.einsum('blnc,nce->blne', a.reshape(B, L, ML_N_BLOCKS, ML_QKV_BLOCK), w)
    return out.reshape(B, L, ML_INNER)


def to_heads(a):
    B, L, _ = a.shape
    return _f32(a).reshape(B, L, ML_HEADS, ML_HEAD_DIM).transpose(0, 2, 1, 3)


def mlstm_chunkwise(q, k, v, ig, lf, state):
    B, H, L, dh = q.shape
    nc = L // ML_CHUNK

    def chunks(a):
        return jnp.moveaxis(a.reshape(B, H, nc, ML_CHUNK, *a.shape[3:]), 2, 0)

    lower = jnp.tril(jnp.ones((ML_CHUNK, ML_CHUNK), dtype=bool))

    def step(carry, xs):
        C, n, m = carry
        qc, kc, vc, igc, lfc = xs
        b = jnp.cumsum(lfc, axis=-1)
        a_inter = b + m[..., None]
        d = jnp.where(lower, b[..., :, None] - b[..., None, :] + igc[..., None, :], -jnp.inf)
        m_row = jnp.maximum(a_inter, jnp.max(d, axis=-1))
        w_inter = jnp.exp(a_inter - m_row)
        s = jnp.einsum('bhid,bhjd->bhij', qc, kc) * jnp.exp(d - m_row[..., None])
        num = (w_inter[..., None] * jnp.einsum('bhed,bhid->bhie', C, qc)
               + jnp.einsum('bhij,bhje->bhie', s, vc))
        den = w_inter * jnp.einsum('bhd,bhid->bhi', n, qc) + jnp.sum(s, axis=-1)
        h = num / jnp.maximum(jnp.abs(den), jnp.exp(-m_row))[..., None]
        b_last = b[..., -1]
        e = b_last[..., None] - b + igc
        m_new = jnp.maximum(b_last + m, jnp.max(e, axis=-1))
        g_old = jnp.exp(b_last + m - m_new)
        w_new = jnp.exp(e - m_new[..., None])
        C_new = g_old[..., None, None] * C + jnp.einsum('bhje,bhjd->bhed', vc * w_new[..., None], kc)
        n_new = g_old[..., None] * n + jnp.einsum('bhj,bhjd->bhd', w_new, kc)
        return (C_new, n_new, m_new), h

    state, h = lax.scan(step, state, (chunks(q), chunks(k), chunks(v), chunks(ig), chunks(lf)))
    return jnp.moveaxis(h, 0, 2).reshape(B, H, L, dh), state


def mlstm_final_state(k, v, ig, lf):
    b = jnp.cumsum(lf, axis=-1)
    b_last = b[..., -1]
    e = b_last[..., None] - b + ig
    m = jnp.maximum(b_last, jnp.max(e, axis=-1))
    w = jnp.exp(e - m[..., None])
    return (jnp.einsum('bhje,bhjd->bhed', v * w[..., None], k),
            jnp.einsum('bhj,bhjd->bhd', w, k), m)


def mlstm_prep(u, w_in, conv_w, conv_b, wq, wk, wv, w_gate, b_gate):
    B, L, _ = u.shape
    xm, z = jnp.split(u @ w_in, 2, axis=-1)
    xc = jax.nn.silu(short_conv(xm, conv_w, conv_b))
    q = headwise(xc, wq)
    k = headwise(xc, wk)
    v = headwise(xm, wv)
    gates = (q @ w_gate[:ML_INNER] + k @ w_gate[ML_INNER:2 * ML_INNER]
             + v @ w_gate[2 * ML_INNER:] + b_gate)
    gates = jnp.transpose(_f32(gates).reshape(B, L, 2, 2, ML_HEADS), (2, 3, 0, 4, 1))
    ig = gates[:, 0]
    lf = jax.nn.log_sigmoid(gates[:, 1])
    return to_heads(q), to_heads(k) * (ML_HEAD_DIM ** -0.5), to_heads(v), ig, lf, xc, z


def mlstm_out(h, xc, z, norm_w, skip, w_down):
    B, H, L, dh = h.shape
    mu = jnp.mean(h, axis=-1, keepdims=True)
    var = jnp.mean(jnp.square(h - mu), axis=-1, keepdims=True)
    hn = ((h - mu) * lax.rsqrt(var + ML_NORM_EPS)).transpose(0, 2, 1, 3).reshape(B, L, H * dh)
    hs = (hn * _f32(norm_w)).astype(xc.dtype) + skip * xc
    return (hs * jax.nn.silu(z)) @ w_down


def mlstm_mix(u_lat, u_ctx, p, need_ctx):
    w_in, conv_w, conv_b, wq, wk, wv, w_gate, b_gate, norm_w, skip, w_down = p

    def prep(u):
        return mlstm_prep(u, w_in, conv_w, conv_b, wq, wk, wv, w_gate, b_gate)

    def flip(a):
        return jnp.flip(a, axis=2)

    q, k, v, ig, lf, xc, z = prep(u_lat)
    qc, kc, vc, igc, lfc, xcc, zc = prep(u_ctx)
    if need_ctx:
        B = u_ctx.shape[0]
        zero = (jnp.zeros((B, ML_HEADS, ML_HEAD_DIM, ML_HEAD_DIM), jnp.float32),
                jnp.zeros((B, ML_HEADS, ML_HEAD_DIM), jnp.float32),
                jnp.zeros((B, ML_HEADS), jnp.float32))
        hcf, st_f = mlstm_chunkwise(qc, kc, vc, igc[0], lfc[0], zero)
        hcb, st_b = mlstm_chunkwise(flip(qc), flip(kc), flip(vc), flip(igc[1]), flip(lfc[1]), zero)
        y_ctx = mlstm_out(hcf + flip(hcb), xcc, zc, norm_w, skip, w_down)
    else:
        st_f = mlstm_final_state(kc, vc, igc[0], lfc[0])
        st_b = mlstm_final_state(flip(kc), flip(vc), flip(igc[1]), flip(lfc[1]))
        y_ctx = None
    hf, _ = mlstm_chunkwise(q, k, v, ig[0], lf[0], st_f)
    hb, _ = mlstm_chunkwise(flip(q), flip(k), flip(v), flip(ig[1]), flip(lf[1]), st_b)
    y_lat = mlstm_out(hf + flip(hb), xc, z, norm_w, skip, w_down)
    return y_lat, y_ctx


def setup_inputs(seed: int = 0) -> dict:
    key = jax.random.key(seed)
    keys = iter(jax.random.split(key, 48))

    def nrm(shape, scale):
        return jax.random.normal(next(keys), shape, dtype=jnp.float32) * scale

    D, NH, NM = D_MODEL, N_HY_LAYERS, N_ML_LAYERS
    fb = jnp.linspace(3.0, 6.0, ML_HEADS, dtype=jnp.float32)
    ml_b_gate = jnp.stack([nrm((NM, 2, ML_HEADS), 0.1), fb + nrm((NM, 2, ML_HEADS), 0.1)],
                          axis=2).reshape(NM, 4 * ML_HEADS)
    return {
        'x': nrm((BATCH, SEQ, D), 1.0),
        'c': nrm((BATCH, D), 1.0),
        'ctx': nrm((BATCH, CTX_LEN, D), 1.0),
        'c_ctx': nrm((D,), 1.0),
        'mod_w': nrm((DEPTH, D, 6 * D), 0.5 * D ** -0.5),
        'mod_b': nrm((DEPTH, 6 * D), 0.02),
        'norm_g': 1.0 + nrm((DEPTH, 2, D), 0.02),
        'final_g': 1.0 + nrm((D,), 0.02),
        'hy_w_in': nrm((NH, D, 3 * D), D ** -0.5),
        'hy_b_in': nrm((NH, 3 * D), 0.02),
        'hy_sc_w': nrm((NH, SHORT_W, 3 * D), SHORT_W ** -0.5),
        'hy_sc_b': nrm((NH, 3 * D), 0.02),
        'hy_f_w1': nrm((NH, HY_EMB_DIM, HY_FILTER_WIDTH), HY_EMB_DIM ** -0.5),
        'hy_f_b1': nrm((NH, HY_FILTER_WIDTH), 0.1),
        'hy_f_w2': nrm((NH, HY_FILTER_WIDTH, HY_FILTER_WIDTH), HY_FILTER_WIDTH ** -0.5),
        'hy_f_b2': nrm((NH, HY_FILTER_WIDTH), 0.1),
        'hy_f_w3': nrm((NH, HY_FILTER_WIDTH, HY_FILTER_WIDTH), HY_FILTER_WIDTH ** -0.5),
        'hy_f_b3': nrm((NH, HY_FILTER_WIDTH), 0.1),
        'hy_f_wout': nrm((NH, HY_FILTER_WIDTH, 2 * D), HY_FILTER_WIDTH ** -0.5),
        'hy_freq': 1.0 + nrm((NH, HY_FILTER_WIDTH), 0.1),
        'hy_bias': nrm((NH, D), 0.2),
        'hy_w_out': nrm((NH, D, D), D ** -0.5),
        'hy_b_out': nrm((NH, D), 0.02),
        'ml_w_in': nrm((NM, D, 2 * ML_INNER), D ** -0.5),
        'ml_conv_w': nrm((NM, SHORT_W, ML_INNER), SHORT_W ** -0.5),
        'ml_conv_b': nrm((NM, ML_INNER), 0.02),
        'ml_wq': nrm((NM, ML_N_BLOCKS, ML_QKV_BLOCK, ML_QKV_BLOCK), ML_QKV_BLOCK ** -0.5),
        'ml_wk': nrm((NM, ML_N_BLOCKS, ML_QKV_BLOCK, ML_QKV_BLOCK), ML_QKV_BLOCK ** -0.5),
        'ml_wv': nrm((NM, ML_N_BLOCKS, ML_QKV_BLOCK, ML_QKV_BLOCK), ML_QKV_BLOCK ** -0.5),
        'ml_w_gate': nrm((NM, 3 * ML_INNER, 4 * ML_HEADS), 0.1 * (3 * ML_INNER) ** -0.5),
        'ml_b_gate': ml_b_gate,
        'ml_norm_w': 1.0 + nrm((NM, ML_INNER), 0.02),
        'ml_skip': 1.0 + nrm((NM, ML_INNER), 0.02),
        'ml_w_down': nrm((NM, ML_INNER, D), ML_INNER ** -0.5),
        'ffn_w_up': nrm((DEPTH, D, 2 * FFN_HIDDEN), D ** -0.5),
        'ffn_conv_w': nrm((DEPTH, 3, 3, FFN_HIDDEN), 1.0 / 3.0),
        'ffn_conv_b': nrm((DEPTH, FFN_HIDDEN), 0.02),
        'ffn_w_down': nrm((DEPTH, FFN_HIDDEN, D), FFN_HIDDEN ** -0.5),
    }


def reference(x, c, ctx, c_ctx, mod_w, mod_b, norm_g, final_g,
              hy_w_in, hy_b_in, hy_sc_w, hy_sc_b, hy_f_w1, hy_f_b1, hy_f_w2, hy_f_b2,
              hy_f_w3, hy_f_b3, hy_f_wout, hy_freq, hy_bias, hy_w_out, hy_b_out,
              ml_w_in, ml_conv_w, ml_conv_b, ml_wq, ml_wk, ml_wv, ml_w_gate, ml_b_gate,
              ml_norm_w, ml_skip, ml_w_down,
              ffn_w_up, ffn_conv_w, ffn_conv_b, ffn_w_down):
    rows = x.shape[1] // GRID_W
    ctx_len = ctx.shape[1]
    hy_params = (hy_w_in, hy_b_in, hy_sc_w, hy_sc_b, hy_f_w1, hy_f_b1, hy_f_w2, hy_f_b2,
                 hy_f_w3, hy_f_b3, hy_f_wout, hy_freq, hy_bias, hy_w_out, hy_b_out)
    ml_params = (ml_w_in, ml_conv_w, ml_conv_b, ml_wq, ml_wk, ml_wv, ml_w_gate, ml_b_gate,
                 ml_norm_w, ml_skip, ml_w_down)
    for i in range(DEPTH):
        last = i == DEPTH - 1
        sh1, sc1, g1, sh2, sc2, g2 = adaln_params(c, mod_w[i], mod_b[i])
        csh1, csc1, cg1, csh2, csc2, cg2 = adaln_params(c_ctx, mod_w[i], mod_b[i])
        h_lat = modulate(rmsnorm(x, norm_g[i, 0]), sh1, sc1)
        h_ctx = modulate(rmsnorm(ctx, norm_g[i, 0]), csh1, csc1)
        j = i // N_MIXERS
        if i % N_MIXERS == 0:
            p = tuple(a[j] for a in hy_params)
            y_lat = hyena_mix(h_lat, p)
            y_ctx = None if last else hyena_mix(h_ctx, p)
        else:
            p = tuple(a[j] for a in ml_params)
            y_lat, y_ctx = mlstm_mix(h_lat, h_ctx, p, not last)
        x = x + g1 * y_lat
        ffn_p = (ffn_w_up[i], ffn_conv_w[i], ffn_conv_b[i], ffn_w_down[i])
        x = x + g2 * conv_ffn(modulate(rmsnorm(x, norm_g[i, 1]), sh2, sc2), *ffn_p, rows, GRID_W)
        if not last:
            ctx = ctx + cg1 * y_ctx
            ctx = ctx + cg2 * conv_ffn(modulate(rmsnorm(ctx, norm_g[i, 1]), csh2, csc2),
                                       *ffn_p, 1, ctx_len)
    return rmsnorm(x, final_g)
```

```python
import math
import numpy as np
import concourse.bass as bass
import concourse.mybir as mybir
from concourse.bass_utils import run_bass_kernel_spmd
from contextlib import ExitStack

F32 = mybir.dt.float32
BF16 = mybir.dt.bfloat16
ALU = mybir.AluOpType
AF = mybir.ActivationFunctionType
AX = mybir.AxisListType

CENG = ('pe', 'dve', 'act', 'pool')
DQ = ('sp', 'pool', 'act')

T = 8192
D = 1024
TC = 256
FH = 2816
NFC = 22
MI = 2048
EPS = 1e-6
MAGIC = 12582912.0
TWO_PI = 2.0 * math.pi


class Prog:
    NDS = 6

    def __init__(self, nc, es):
        self.nc = nc
        self.eng = dict(pe=nc.tensor, dve=nc.vector, act=nc.scalar, pool=nc.gpsimd, sp=nc.sync)
        self.streams = {e: [] for e in self.eng}
        self.ninstr = {e: 0 for e in self.eng}
        self.csem = {e: es.enter_context(nc.semaphore(f"c{e}")) for e in CENG}
        self.ccount = {e: 0 for e in CENG}
        self.dsem = {q: [es.enter_context(nc.semaphore(f"d{q}_{i}")) for i in range(self.NDS)] for q in DQ}
        self.dcount = {q: 0 for q in DQ}
        self.seen = {e: {} for e in self.eng}
        self.lastw = {}
        self.readers = {}

    def _deps(self, R, W):
        deps = []
        for k in R:
            t = self.lastw.get(k)
            if t is not None:
                deps.append(t)
        for k in W:
            t = self.lastw.get(k)
            if t is not None:
                deps.append(t)
            deps.extend(self.readers.get(k, ()))
        return deps

    def _emit_waits(self, e, deps, skip_pe_pe=False):
        seen = self.seen[e]
        need = {}
        for t in deps:
            if t[0] == 'c':
                _, pe_, n = t
                if skip_pe_pe and pe_ == 'pe' and e == 'pe':
                    continue
                key = ('c', pe_)
                val = n
            else:
                _, q, slot, v = t
                key = ('d', q, slot)
                val = v
            if seen.get(key, 0) >= val:
                continue
            if need.get(key, 0) < val:
                need[key] = val
        for key, val in need.items():
            seen[key] = val
            if key[0] == 'c':
                sem = self.csem[key[1]]
                self.streams[e].append(lambda eng, sem=sem, val=val: eng.wait_ge(sem, val))
            else:
                sem = self.dsem[key[1]][key[2]]
                self.streams[e].append(lambda eng, sem=sem, val=val: eng.wait_ge(sem, 16 * val))

    def _record(self, tok, R, W):
        for k in W:
            self.lastw[k] = tok
            self.readers[k] = []
        for k in R:
            self.readers.setdefault(k, []).append(tok)

    def op(self, e, fn, R=(), W=()):
        deps = self._deps(R, W)
        self._emit_waits(e, deps, skip_pe_pe=True)
        self.ccount[e] += 1
        n = self.ccount[e]
        sem = self.csem[e]
        self.streams[e].append(lambda eng, fn=fn, sem=sem: fn(eng).then_inc(sem, 1))
        self._record(('c', e, n), R, W)
        self.ninstr[e] += 1

    def dma(self, q, out, in_, R=(), W=(), **kw):
        deps = self._deps(R, W)
        self._emit_waits(q, deps)
        i = self.dcount[q]
        self.dcount[q] += 1
        slot = i % self.NDS
        v = i // self.NDS + 1
        sem = self.dsem[q][slot]
        self.streams[q].append(lambda eng, out=out, in_=in_, sem=sem, kw=kw: eng.dma_start(out=out, in_=in_, **kw).then_inc(sem, 16))
        self._record(('d', q, slot, v), R, W)
        self.ninstr[q] += 1

    def barrier(self):
        toks = []
        for e in CENG:
            if self.ccount[e] > 0:
                toks.append(('c', e, self.ccount[e]))
        for q in DQ:
            n = self.dcount[q]
            for slot in range(self.NDS):
                if n > slot:
                    toks.append(('d', q, slot, (n - 1 - slot) // self.NDS + 1))
        for e in self.eng:
            self._emit_waits(e, toks)
        self.lastw = {}
        self.readers = {}

    def flush(self):
        self.barrier()
        nc = self.nc
        st = self.streams
        with nc.Block() as block:
            @block.tensor
            def _(eng):
                for f in st['pe']:
                    f(eng)

            @block.vector
            def _(eng):
                for f in st['dve']:
                    f(eng)

            @block.scalar
            def _(eng):
                for f in st['act']:
                    f(eng)

            @block.gpsimd
            def _(eng):
                for f in st['pool']:
                    f(eng)

            @block.sync
            def _(eng):
                for f in st['sp']:
                    f(eng)
        self.streams = {e: [] for e in self.eng}

    def mm(self, out, lhsT, rhs, start, stop, R, W):
        self.op('pe', lambda e: e.matmul(out=out, lhsT=lhsT, rhs=rhs, start=start, stop=stop), R, W)

    def tr(self, out, in_, ident, R, W):
        self.op('pe', lambda e: e.transpose(out=out, in_=in_, identity=ident), R, W)

    def act(self, out, in_, func, R, W, scale=None, bias=None, accum_out=None):
        kw = {}
        if scale is not None:
            kw['scale'] = scale
        if bias is not None:
            kw['bias'] = bias
        if accum_out is not None:
            kw['accum_out'] = accum_out
        self.op('act', lambda e: e.activation(out=out, in_=in_, func=func, **kw), R, W)

    def ts(self, eng, out, in0, s1, s2, op0, op1, R, W):
        if op1 is None:
            self.op(eng, lambda e: e.tensor_scalar(out=out, in0=in0, scalar1=s1, scalar2=None, op0=op0), R, W)
        else:
            self.op(eng, lambda e: e.tensor_scalar(out=out, in0=in0, scalar1=s1, scalar2=s2, op0=op0, op1=op1), R, W)

    def tt(self, eng, out, in0, in1, op, R, W):
        self.op(eng, lambda e: e.tensor_tensor(out=out, in0=in0, in1=in1, op=op), R, W)

    def stt(self, out, in0, scalar, in1, op0, op1, R, W):
        self.op('dve', lambda e: e.scalar_tensor_tensor(out=out, in0=in0, scalar=scalar, in1=in1, op0=op0, op1=op1), R, W)

    def copy(self, eng, out, in_, R, W):
        if eng == 'act':
            self.op('act', lambda e: e.copy(out=out, in_=in_), R, W)
        else:
            self.op(eng, lambda e: e.tensor_copy(out=out, in_=in_), R, W)

    def memset(self, eng, ap, val, W):
        self.op(eng, lambda e: e.memset(ap, val), (), W)

    def recip(self, out, in_, R, W):
        self.op('dve', lambda e: e.reciprocal(out=out, in_=in_), R, W)


class Ctx:
    def __init__(self, nc, debug):
        self.nc = nc
        self.debug = debug
        self.inputs = {}
        self.feed = None
        self.dbg_names = []

    def din(self, name, arr):
        arr = np.ascontiguousarray(arr, dtype=np.float32)
        self.inputs[name] = arr
        return self.nc.dram_tensor(name, list(arr.shape), F32, kind="ExternalInput").ap()

    def dscr(self, name, shape, dt):
        if self.feed and name in self.feed:
            return self.nc.dram_tensor(name, list(shape), dt, kind="ExternalInput").ap()
        if self.debug and name in self.debug:
            self.dbg_names.append(name)
            return self.nc.dram_tensor(name, list(shape), dt, kind="ExternalOutput").ap()
        return self.nc.dram_tensor(name, list(shape), dt, kind="Internal").ap()


class Pool_:
    def __init__(self, es, nc, name, shape, dt, n, psum=False):
        mk = nc.psum_tensor if psum else nc.sbuf_tensor
        self.tiles = [es.enter_context(mk(f"{name}{i}", shape, dt)) for i in range(n)]
        self.name = name
        self.i = -1

    def next(self):
        self.i = (self.i + 1) % len(self.tiles)
        return self.tiles[self.i], (self.name, self.i)


def _pos_feats(idx, L):
    idx = np.asarray(idx, dtype=np.float64)
    t = (idx / (L - 1)).astype(np.float32).astype(np.float64)
    w = (2.0 * math.pi * idx.astype(np.float32) / np.float32(L)).astype(np.float64)
    bands = np.linspace(1e-4, 15.0, 16, dtype=np.float32).astype(np.float64)
    arg = bands[None, :] * w[:, None]
    return np.concatenate([t[:, None], np.cos(arg), -np.sin(arg)], axis=1).astype(np.float32)


def host_consts():
    C = {}
    C['ident'] = np.eye(128, dtype=np.float32)
    n = np.arange(128)
    k1 = np.arange(65)
    ang = 2 * np.pi * np.outer(n, k1) / 128.0
    C['FA'] = np.concatenate([np.cos(ang), -np.sin(ang[:, 1:64])], axis=1)
    n2 = n[:, None, None]
    kk = (k1[None, :, None] + 128 * n[None, None, :])
    th = 2 * np.pi * (n2 * kk % 16384) / 16384.0
    C['TC'] = np.stack([np.cos(th), np.sin(th)], axis=2)
    ph = 2 * np.pi * np.outer(n, n) / 128.0
    C['IR'] = np.stack([np.concatenate([np.cos(ph), np.sin(ph)], 1), np.concatenate([-np.sin(ph), np.cos(ph)], 1)], axis=1)
    wk = np.where((k1 == 0) | (k1 == 64), 1.0, 2.0) / 16384.0
    tw = 2 * np.pi * np.outer(k1, n) / 16384.0
    C['TWI'] = np.stack([wk[:, None] * np.cos(tw), wk[:, None] * np.sin(tw)], axis=1)
    n1 = np.arange(64)
    g = 2 * np.pi * np.outer(k1, n1) / 128.0
    C['G'] = np.stack([np.cos(g), -np.sin(g)], axis=1)
    N2, N1 = np.meshgrid(np.arange(128), np.arange(128), indexing='ij')
    fwd = N1 < 64
    idx = np.where(fwd, 128 * N1 + N2, 16384 - 128 * N1 - N2)
    idx = np.where(idx >= T, 0, idx)
    C['posR'] = _pos_feats(idx.reshape(-1), T).T.copy()
    deltas = np.abs(np.linspace(math.log(1e-2) / 1.5, math.log(1e-2) / 0.3, D, dtype=np.float32)).astype(np.float64)
    C['absdelta'] = deltas
    a_n1 = np.where(np.arange(128) < 64, 128.0 * np.arange(128), 16384.0 - 128.0 * np.arange(128)) / (T - 1)
    C['decL'] = np.stack([a_n1, (np.arange(128) < 64).astype(np.float64), (np.arange(128) >= 64).astype(np.float64)], axis=0)
    r0 = np.broadcast_to(-deltas[None, :], (128, D))
    r1 = -np.outer(np.arange(128) / (T - 1), deltas)
    C['decR'] = np.stack([r0, r1, -r1], axis=0)
    dch = np.concatenate([np.arange(255, 0, -1), np.arange(0, 256)])
    C['posC'] = _pos_feats(np.concatenate([dch, [0]]), TC).T.copy()
    C['tC'] = (dch / (TC - 1.0)).astype(np.float32)[None, :]
    C['ndelta'] = (-deltas).reshape(8, 128).T.copy()
    i = np.arange(128)
    C['U'] = (i[:, None] <= i[None, :]).astype(np.float32)
    C['UT'] = (i[:, None] >= i[None, :]).astype(np.float32)
    C['ones'] = np.ones((128, 128), dtype=np.float32)
    return {k: np.ascontiguousarray(v, dtype=np.float32) for k, v in C.items()}


class _NC:
    def __init__(self, nc):
        self._nc = nc
        self._uid = 0

    def __getattr__(self, k):
        return getattr(self._nc, k)

    def sbuf_tensor(self, name, shape, dt):
        self._uid += 1
        return self._nc.sbuf_tensor(f"{name}_{self._uid}", shape, dt)

    def psum_tensor(self, name, shape, dt):
        self._uid += 1
        return self._nc.psum_tensor(f"{name}_{self._uid}", shape, dt)


def build(inp, debug=False, stop_after=None, feed=None):
    nc_real = bass.Bass("TRN2", target_bir_lowering=False)
    nc = _NC(nc_real)
    cx = Ctx(nc, debug)
    cx.feed = feed
    HC = host_consts()
    es = ExitStack()
    P = Prog(nc, es)

    def sbp(name, shape, dt):
        return es.enter_context(nc.sbuf_tensor(name, shape, dt))

    x_d = cx.din('x', inp['x'])
    ctx_d = cx.din('ctx', inp['ctx'])
    cvec_d = cx.din('cvec', inp['cvec'])
    W = {}
    for k in ('mod_w', 'mod_b', 'norm_g', 'final_g', 'hy_w_in', 'hy_b_in', 'hy_sc_w', 'hy_sc_b', 'hy_w1d', 'hy_b1d',
              'hy_w2bd', 'hy_b2d', 'hy_w3bd', 'hy_b3d', 'hy_wo_st', 'hy_freqd', 'hy_bias', 'hy_w_out', 'hy_b_out',
              'ml_w_in', 'ml_conv_w', 'ml_conv_b', 'ml_bdq', 'ml_bdk', 'ml_bdv', 'ml_w_gate', 'ml_b_gate', 'ml_norm_w',
              'ml_skip', 'ml_w_down', 'ffn_w_up', 'ffn_conv_w', 'ffn_conv_b', 'ffn_w_down',
              'mod_b_col', 'norm_g_col', 'hy_b_in_col', 'hy_sc_w_col', 'hy_sc_b_col', 'hy_bias_col', 'ml_conv_w_col',
              'ml_conv_b_col', 'ml_norm_w_col', 'ml_skip_col', 'ffn_conv_w_col', 'ffn_conv_b_col'):
        W[k] = cx.din(k, inp[k])
    K = {k: cx.din('c_' + k, v) for k, v in HC.items()}
    out_d = nc.dram_tensor('out', [T, D], F32, kind="ExternalOutput").ap()

    MODROW = cx.dscr('MODROW', [2, 2, 6144], F32)
    P0 = cx.dscr('P0', [3 * D, T], BF16)
    P0C = cx.dscr('P0C', [3 * D, TC], BF16)
    Zd = cx.dscr('Zd', [D, T], BF16)
    X0d = cx.dscr('X0d', [D, T], BF16)
    KFd = cx.dscr('KFd', [16, 128, 2, 65, 64], BF16)
    RNd = cx.dscr('RNd', [128, 8], F32)
    Ytd = cx.dscr('Ytd', [D, T], F32)
    XA0 = cx.dscr('XA0', [T, D], F32)
    XB0 = cx.dscr('XB0', [T, D], F32)
    CA0 = cx.dscr('CA0', [TC, D], F32)
    CB0 = cx.dscr('CB0', [TC, D], F32)
    XA1 = cx.dscr('XA1', [T, D], F32)
    ZdC = cx.dscr('ZdC', [D, TC], BF16)
    XMd = cx.dscr('XMd', [MI, T], BF16)
    SZd = cx.dscr('SZd', [MI, T], BF16)
    XMC = cx.dscr('XMC', [MI, TC], BF16)
    Qfm = cx.dscr('Qfm', [MI, T], BF16)
    Kfm = cx.dscr('Kfm', [MI, T], BF16)
    Ktm = cx.dscr('Ktm', [T, MI], BF16)
    Vtm = cx.dscr('Vtm', [T, MI], BF16)
    SXC = cx.dscr('SXC', [MI, T], BF16)
    GPd = cx.dscr('GPd', [T, 4, 8], F32)
    KtmC = cx.dscr('KtmC', [TC, MI], BF16)
    VtmC = cx.dscr('VtmC', [TC, MI], BF16)
    GPdC = cx.dscr('GPdC', [TC, 4, 8], F32)
    HFB = cx.dscr('HFB', [2, T, MI], F32)
    X0dC = cx.dscr('X0dC', [D, TC], BF16)
    YtC = cx.dscr('YtC', [D, TC], F32)
    RNdC = cx.dscr('RNdC', [128, 8], F32)

    identb = sbp('identb', [128, 128], BF16)
    identf = sbp('identf', [128, 128], F32)
    modcol = sbp('modcol', [128, 2, 48, 2], F32)
    effs = sbp('effs', [128, 2, 2, 2, 8], F32)
    P.dma('sp', identf[:], K['ident'], W=['identf'])
    P.copy('dve', identb[:], identf[:], R=['identf'], W=['identb'])

    def phase_adaln():
        with ExitStack() as ps:
            sb = lambda n, s, d: ps.enter_context(nc.sbuf_tensor(n, s, d))
            cv = sb('cv', [128, 8, 2], F32)
            scv = sb('scv', [128, 8, 2], F32)
            mbcol = sb('mbcol', [128, 2, 48], F32)
            mbrow = sb('mbrow', [2, 2, 6144], F32)
            rowbuf = sb('rowbuf', [2, 2, 6144], F32)
            ngcol = sb('ngcol', [128, 2, 2, 8], F32)
            mwp = Pool_(ps, nc, 'mw', [128, 8, 1024], F32, 2)
            pcol = ps.enter_context(nc.psum_tensor('pcol', [128, 48, 2], F32))
            prow = Pool_(ps, nc, 'prow', [2, 512], F32, 2, psum=True)
            P.dma('sp', cv[:], cvec_d.rearrange("(k p) m -> p k m", p=128), W=['cv'])
            P.dma('sp', mbcol[:], W['mod_b_col'], W=['mbcol'])
            for l in range(2):
                P.dma('sp', mbrow[:, l, :], W['mod_b'][l:l + 1, :].partition_broadcast(2) if False else W['mod_b'][l:l + 1, :].broadcast_to([2, 6144]), W=['mbrow'])
            P.dma('sp', ngcol[:], W['norm_g_col'], W=['ngcol'])
            P.act(scv[:], cv[:], AF.Silu, R=['cv'], W=['scv'])
            for l in range(2):
                for j in range(6):
                    mw, mwk = mwp.next()
                    P.dma('sp', mw[:], W['mod_w'][l, :, j * 1024:(j + 1) * 1024].rearrange("(k p) n -> p k n", p=128), W=[mwk])
                    for m in range(8):
                        for k in range(8):
                            P.mm(pcol[:, j * 8 + m, :], mw[:, k, m * 128:(m + 1) * 128], scv[:, k, :], k == 0, k == 7,
                                 R=[mwk, 'scv'], W=['pcol'])
                    for half in range(2):
                        pr, prk = prow.next()
                        for k in range(8):
                            P.mm(pr[:], scv[:, k, :], mw[:, k, half * 512:(half + 1) * 512], k == 0, k == 7, R=[mwk, 'scv'], W=[prk])
                        o = j * 1024 + half * 512
                        P.tt('dve', rowbuf[:, l, o:o + 512], pr[:], mbrow[:, l, o:o + 512], ALU.add, R=[prk, 'mbrow'], W=['rowbuf'])
                for m in range(2):
                    P.tt('dve', modcol[:, l, :, m], pcol[:, :, m], mbcol[:, l, :], ALU.add, R=['pcol', 'mbcol'], W=['modcol'])
                for i in range(2):
                    for m in range(2):
                        sc_ap = modcol[:, l, 8 + 24 * i:16 + 24 * i, m]
                        P.ts('dve', effs[:, l, i, m, :], sc_ap, 1.0, None, ALU.add, None, R=['modcol'], W=['effs'])
                        P.tt('dve', effs[:, l, i, m, :], effs[:, l, i, m, :], ngcol[:, l, i, :], ALU.mult, R=['effs', 'ngcol'], W=['effs'])
            P.dma('sp', MODROW.rearrange("l m n -> m l n"), rowbuf[:], R=['rowbuf'], W=['MODROW'])
            P.flush()

    def shift_col(l, i, m):
        return modcol[:, l, 24 * i:24 * i + 8, m]

    class FrontEnd:
        def __init__(self, ps, nblk, nbuf=2):
            self.nblk = nblk
            self.xn = Pool_(ps, nc, 'fe_xn', [128, nblk, D], BF16, 1)
            self.junk = ps.enter_context(nc.sbuf_tensor('fe_junk', [128, D], BF16))
            self.ss = Pool_(ps, nc, 'fe_ss', [128, nblk], F32, 2)
            self.pT = Pool_(ps, nc, 'fe_pT', [128, nblk * 128], BF16, 2, psum=True)

        def run(self, xt, xtk, hT, hTk, l, i, m):
            nblk = self.nblk
            ss, ssk = self.ss.next()
            xn, xnk = self.xn.next()
            for j in range(nblk):
                P.act(self.junk[:], xt[:, j, :], AF.Square, R=[xtk], W=['fe_junk', ssk], accum_out=ss[:, j:j + 1])
            P.ts('dve', ss[:], ss[:], 1.0 / D, EPS, ALU.mult, ALU.add, R=[ssk], W=[ssk])
            P.act(ss[:], ss[:], AF.Sqrt, R=[ssk], W=[ssk])
            P.recip(ss[:], ss[:], R=[ssk], W=[ssk])
            for j in range(nblk):
                P.ts('pool', xn[:, j, :], xt[:, j, :], ss[:, j:j + 1], None, ALU.mult, None, R=[xtk, ssk], W=[xnk])
            for k in range(8):
                pT, pTk = self.pT.next()
                for j in range(nblk):
                    P.tr(pT[:, j * 128:(j + 1) * 128], xn[:, j, k * 128:(k + 1) * 128], identb[:], R=[xnk, 'identb'], W=[pTk])
                P.act(hT[:, k, :], pT[:], AF.Identity, R=[pTk, 'effs', 'modcol'], W=[hTk],
                      scale=effs[:, l, i, m, k:k + 1], bias=shift_col(l, i, m)[:, k:k + 1])

    def phase_h1(src, Tn, dst):
        nblk = 4 if Tn >= 512 else Tn // 128
        TT = nblk * 128
        with ExitStack() as ps:
            sb = lambda n, s, d: ps.enter_context(nc.sbuf_tensor(n, s, d))
            win = sb('win', [128, 8, 3 * D], BF16)
            bcol = sb('bcol', [128, 24], F32)
            fe = FrontEnd(ps, nblk)
            xtp = Pool_(ps, nc, 'xt', [128, nblk, D], F32, 2)
            hTp = Pool_(ps, nc, 'hT', [128, 8, TT], BF16, 2)
            pbp = Pool_(ps, nc, 'pb', [128, 6, TT], BF16, 2)
            pp = Pool_(ps, nc, 'pp', [128, TT], F32, 4, psum=True)
            for k in range(8):
                P.dma('pool', win[:, k, :], W['hy_w_in'][k * 128:(k + 1) * 128, :], W=['win'])
            P.dma('sp', bcol[:], W['hy_b_in_col'], W=['bcol'])
            m = 0 if Tn == T else 1
            for ti in range(Tn // TT):
                t0 = ti * TT
                xt, xtk = xtp.next()
                P.dma('sp', xt[:], src[t0:t0 + TT, :].rearrange("(j p) d -> p j d", p=128), W=[xtk])
                hT, hTk = hTp.next()
                fe.run(xt, xtk, hT, hTk, 0, 0, m)
                for og in range(4):
                    pb, pbk = pbp.next()
                    for oi in range(6):
                        oc = og * 6 + oi
                        pt, ptk = pp.next()
                        for k in range(8):
                            P.mm(pt[:], win[:, k, oc * 128:(oc + 1) * 128], hT[:, k, :], k == 0, k == 7, R=['win', hTk], W=[ptk])
                        if oi % 2 == 0:
                            P.act(pb[:, oi, :], pt[:], AF.Identity, R=[ptk, 'bcol'], W=[pbk], bias=bcol[:, oc:oc + 1])
                        else:
                            P.ts('dve', pb[:, oi, :], pt[:], bcol[:, oc:oc + 1], None, ALU.add, None, R=[ptk, 'bcol'], W=[pbk])
                    P.dma('sp', dst[og * 768:(og + 1) * 768, t0:t0 + TT].rearrange("(c p) t -> p c t", p=128), pb[:], R=[pbk], W=['P0'])
            P.flush()

    def phase_h2(src, Tn, zdst, x0dst):
        PW = 2048 if Tn >= 2048 else Tn
        npc = Tn // PW
        with ExitStack() as ps:
            sb = lambda n, s, d: ps.enter_context(nc.sbuf_tensor(n, s, d))
            scw = sb('scw', [128, 3, 24], F32)
            scb = sb('scb', [128, 24], F32)
            pinp = Pool_(ps, nc, 'pin', [128, 3, PW + 2], BF16, 2)
            accp = Pool_(ps, nc, 'acc', [128, 3, PW], F32, 2)
            zp = Pool_(ps, nc, 'zt', [128, PW], BF16, 2)
            x0p = Pool_(ps, nc, 'x0t', [128, PW], BF16, 2)
            P.dma('sp', scw[:], W['hy_sc_w_col'], W=['scw'])
            P.dma('sp', scb[:], W['hy_sc_b_col'], W=['scb'])
            srcv = src.rearrange("(j c p) t -> c p j t", j=3, c=8, p=128)
            for cc in range(8):
                for pi in range(npc):
                    t0 = pi * PW
                    pin, pink = pinp.next()
                    lo = 1 if pi == 0 else 0
                    hi = PW + 1 if pi == npc - 1 else PW + 2
                    if pi == 0:
                        P.memset('pool', pin[:, :, 0:1], 0.0, W=[pink])
                    if pi == npc - 1:
                        P.memset('pool', pin[:, :, PW + 1:PW + 2], 0.0, W=[pink])
                    P.dma('sp', pin[:, :, lo:hi], srcv[cc][:, :, t0 - 1 + lo:t0 - 1 + hi], W=[pink])
                    acc, acck = accp.next()
                    zt, ztk = zp.next()
                    x0t, x0k = x0p.next()
                    for j in range(3):
                        oc = j * 8 + cc
                        P.act(acc[:, j, :], pin[:, j, 1:PW + 1], AF.Identity, R=[pink, 'scw', 'scb'], W=[(acck, j)],
                              scale=scw[:, 1, oc:oc + 1], bias=scb[:, oc:oc + 1])
                        P.stt(acc[:, j, :], pin[:, j, 0:PW], scw[:, 0, oc:oc + 1], acc[:, j, :], ALU.mult, ALU.add,
                              R=[pink, 'scw', (acck, j)], W=[(acck, j)])
                        if j == 0:
                            P.stt(x0t[:], pin[:, j, 2:PW + 2], scw[:, 2, oc:oc + 1], acc[:, j, :], ALU.mult, ALU.add,
                                  R=[pink, 'scw', (acck, j)], W=[x0k])
                        else:
                            P.stt(acc[:, j, :], pin[:, j, 2:PW + 2], scw[:, 2, oc:oc + 1], acc[:, j, :], ALU.mult, ALU.add,
                                  R=[pink, 'scw', (acck, j)], W=[(acck, j)])
                    P.tt('pool', zt[:], acc[:, 1, :], acc[:, 2, :], ALU.mult, R=[(acck, 1), (acck, 2)], W=[ztk])
                    P.dma('sp', zdst[cc * 128:(cc + 1) * 128, t0:t0 + PW], zt[:], R=[ztk], W=['Zd'])
                    P.dma('sp', x0dst[cc * 128:(cc + 1) * 128, t0:t0 + PW], x0t[:], R=[x0k], W=['X0d'])
            P.flush()

    def sin_layer(pre, prek, frc, frb, tmpa, tmpk, tmpb, tmpbk, out, outk):
        P.ts('dve', tmpa, pre, frc, frb, ALU.mult, ALU.add, R=[prek, 'mlpc'], W=[tmpk])
        P.ts('dve', tmpb, tmpa, 1.0 / TWO_PI, MAGIC, ALU.mult, ALU.add, R=[tmpk], W=[tmpbk])
        P.ts('dve', tmpb, tmpb, -MAGIC, -TWO_PI, ALU.add, ALU.mult, R=[tmpbk], W=[tmpbk])
        P.tt('dve', tmpa, tmpa, tmpb, ALU.add, R=[tmpk, tmpbk], W=[tmpk])
        P.act(out, tmpa, AF.Sin, R=[tmpk], W=[outk])

    class FilterMLP:
        def __init__(self, ps):
            sb = lambda n, s, d: ps.enter_context(nc.sbuf_tensor(n, s, d))
            self.w1 = sb('mlp_w1', [33, 128], F32)
            self.w2 = sb('mlp_w2', [128, 128], F32)
            self.w3 = sb('mlp_w3', [128, 128], F32)
            self.cols = sb('mlp_cols', [128, 8], F32)
            P.dma('sp', self.w1[:], W['hy_w1d'], W=['mlpw'])
            P.dma('sp', self.w2[:], W['hy_w2bd'], W=['mlpw'])
            P.dma('sp', self.w3[:], W['hy_w3bd'], W=['mlpw'])
            P.dma('sp', self.cols[:, 0:1], W['hy_freqd'], W=['mlpc'])
            P.dma('sp', self.cols[:, 1:2], W['hy_b1d'], W=['mlpc'])
            P.dma('sp', self.cols[:, 2:3], W['hy_b2d'], W=['mlpc'])
            P.dma('sp', self.cols[:, 3:4], W['hy_b3d'], W=['mlpc'])
            P.ts('dve', self.cols[:, 4:7], self.cols[:, 1:4], self.cols[:, 0:1], None, ALU.mult, None, R=['mlpc'], W=['mlpc'])
            self.posp = Pool_(ps, nc, 'mlp_pos', [33, 512], F32, 2)
            self.ta = Pool_(ps, nc, 'mlp_ta', [128, 512], F32, 2)
            self.tb = Pool_(ps, nc, 'mlp_tb', [128, 512], F32, 2)
            self.h = Pool_(ps, nc, 'mlp_h', [128, 512], F32, 2)
            self.pm = Pool_(ps, nc, 'mlp_pm', [128, 512], F32, 2, psum=True)

        def run(self, pos_d, c0, n, out, outk):
            pos, posk = self.posp.next()
            P.dma('sp', pos[:, 0:n], pos_d[:, c0:c0 + n], W=[posk])
            cur, curk, ws = pos[:, 0:n], posk, [self.w1, self.w2, self.w3]
            for li in range(3):
                pm, pmk = self.pm.next()
                P.mm(pm[:, 0:n], ws[li][:], cur, True, True, R=['mlpw', curk], W=[pmk])
                ta, tak = self.ta.next()
                tb, tbk = self.tb.next()
                if li < 2:
                    h, hk = self.h.next()
                    o, ok = h[:, 0:n], hk
                else:
                    o, ok = out, outk
                sin_layer(pm[:, 0:n], pmk, self.cols[:, 0:1], self.cols[:, 4 + li:5 + li], ta[:, 0:n], tak, tb[:, 0:n], tbk, o, ok)
                cur, curk = o, ok

    class FFTConsts:
        def __init__(self, ps, inverse):
            sb = lambda n, s, d: ps.enter_context(nc.sbuf_tensor(n, s, d))
            self.FA = sb('FA', [128, 128], BF16)
            self.TC = sb('TC', [128, 65, 2, 128], BF16)
            P.dma('pool', self.FA[:], K['FA'], W=['fftc'])
            P.dma('pool', self.TC[:], K['TC'], W=['fftc'])
            self.B = sb('Bbuf', [128, 3, 65, 64], BF16)
            P.memset('dve', self.B[:, 1, 0:1, :], 0.0, W=['Bbuf'])
            P.memset('dve', self.B[:, 1, 64:65, :], 0.0, W=['Bbuf'])
            self.pa = Pool_(ps, nc, 'pa', [128, 4, 128], F32, 2, psum=True)
            self.xp = Pool_(ps, nc, 'xp', [128, 2, 4, 64], F32, 2, psum=True)

        def stage_a(self, src, srck, Kp):
            B = self.B
            for c4 in range(16):
                pa, pak = self.pa.next()
                for i in range(4):
                    c = c4 * 4 + i
                    P.mm(pa[:, i, :], src[0:Kp, c, :], self.FA[0:Kp, :], True, True, R=[srck, 'fftc'], W=[pak])
                o0 = B[:, 0, :, c4 * 4:c4 * 4 + 4].rearrange("p k c -> p c k")
                o2 = B[:, 2, :, c4 * 4:c4 * 4 + 4].rearrange("p k c -> p c k")
                o1 = B[:, 1, 1:64, c4 * 4:c4 * 4 + 4].rearrange("p k c -> p c k")
                P.act(o0, pa[:, :, 0:65], AF.Identity, R=[pak], W=['Bbuf'])
                P.act(o2, pa[:, :, 0:65], AF.Identity, R=[pak], W=['Bbuf'], scale=-1.0)
                P.copy('dve', o1, pa[:, :, 65:128], R=[pak], W=['Bbuf'])

        def stage_c(self, consume):
            B, TC = self.B, self.TC
            for kq in range(17):
                k10 = kq * 4
                nk = min(4, 65 - k10)
                xp, xpk = self.xp.next()
                for i in range(nk):
                    k1 = k10 + i
                    P.mm(xp[:, 0, i, :], TC[:, k1, 0, :], B[:, 0, k1, :], True, False, R=['fftc', 'Bbuf'], W=[xpk])
                    P.mm(xp[:, 0, i, :], TC[:, k1, 1, :], B[:, 1, k1, :], False, True, R=['fftc', 'Bbuf'], W=[xpk])
                    P.mm(xp[:, 1, i, :], TC[:, k1, 0, :], B[:, 1, k1, :], True, False, R=['fftc', 'Bbuf'], W=[xpk])
                    P.mm(xp[:, 1, i, :], TC[:, k1, 1, :], B[:, 2, k1, :], False, True, R=['fftc', 'Bbuf'], W=[xpk])
                consume(xp, xpk, k10, nk)

    def phase_k():
        with ExitStack() as ps:
            sb = lambda n, s, d: ps.enter_context(nc.sbuf_tensor(n, s, d))
            h3 = sb('h3', [128, 16384], BF16)
            with ExitStack() as ps2:
                mlp = FilterMLP(ps2)
                for ct in range(32):
                    mlp.run(K['posR'], ct * 512, 512, h3[:, ct * 512:(ct + 1) * 512], 'h3')
                h3v = h3[:].rearrange("p (a b) -> p a b", b=128)
                P.memset('pool', h3v[0:64, :, 64:128], 0.0, W=['h3'])
                P.memset('pool', h3v[64:128, :, 0:64], 0.0, W=['h3'])
                P.memset('pool', h3v[64:128, 0:1, 64:65], 0.0, W=['h3'])
                P.flush()
            fc = FFTConsts(ps, False)
            wo = sb('wo', [128, D], BF16)
            decL = sb('decL', [3, 128], F32)
            ones = sb('onesf', [128, 1], F32)
            P.dma('pool', wo[:], W['hy_wo_st'], W=['wo'])
            P.dma('sp', decL[:], K['decL'], W=['decL'])
            P.memset('dve', ones[:], 1.0, W=['onesf'])
            KAp = Pool_(ps, nc, 'KA', [128, 64, 128], BF16, 2)
            KFp = Pool_(ps, nc, 'KFs', [128, 2, 65, 64], BF16, 2)
            decRp = Pool_(ps, nc, 'decR', [3, 8, 64], F32, 3)
            decp = Pool_(ps, nc, 'dec', [128, 8, 64], F32, 2)
            tmpp = Pool_(ps, nc, 'ktmp', [128, 8, 64], F32, 2)
            partp = Pool_(ps, nc, 'kpart', [128, 64], F32, 2)
            accp = Pool_(ps, nc, 'kacc', [128, 64], F32, 2)
            rnp = Pool_(ps, nc, 'rn', [64, 1], F32, 2)
            pkp = Pool_(ps, nc, 'pk', [128, 8, 64], F32, 2, psum=True)
            pep = Pool_(ps, nc, 'pe', [128, 8, 64], F32, 1, psum=True)
            pnp = Pool_(ps, nc, 'pn', [64, 1], F32, 1, psum=True)
            for g in range(16):
                c0 = g * 64
                KA, KAk = KAp.next()
                acc, acck = accp.next()
                for nb in range(16):
                    dr, drk = decRp.next()
                    P.dma('sp', dr[:], K['decR'][:, nb * 8:(nb + 1) * 8, c0:c0 + 64], W=[drk])
                    pk, pkk = pkp.next()
                    for i in range(8):
                        n2 = nb * 8 + i
                        P.mm(pk[:, i, :], h3[:, n2 * 128:(n2 + 1) * 128], wo[:, c0:c0 + 64], True, True, R=['h3', 'wo'], W=[pkk])
                    pe_, pek = pep.next()
                    P.mm(pe_[:].rearrange('p a b -> p (a b)'), decL[:], dr[:].rearrange('p a b -> p (a b)'), True, True, R=['decL', drk], W=[pek])
                    dec, deck = decp.next()
                    P.act(dec[:], pe_[:], AF.Exp, R=[pek], W=[deck])
                    tmp, tmpk = tmpp.next()
                    P.tt('dve', tmp[:], pk[:], dec[:], ALU.mult, R=[pkk, deck], W=[tmpk])
                    tv = tmp[:].rearrange("p n c -> p c n")
                    P.copy('pool', KA[:, :, nb * 8:(nb + 1) * 8], tv, R=[tmpk], W=[KAk])
                    if nb == 0:
                        P.op('dve', lambda e, o=acc[:], i_=tv: e.tensor_reduce(out=o, in_=i_, axis=AX.X, op=ALU.add, apply_absolute_value=True),
                             R=[tmpk], W=[acck])
                    else:
                        part, partk = partp.next()
                        P.op('dve', lambda e, o=part[:], i_=tv: e.tensor_reduce(out=o, in_=i_, axis=AX.X, op=ALU.add, apply_absolute_value=True),
                             R=[tmpk], W=[partk])
                        P.tt('pool', acc[:], acc[:], part[:], ALU.add, R=[acck, partk], W=[acck])
                pn, pnk = pnp.next()
                P.mm(pn[:], acc[:], ones[:], True, True, R=[acck, 'onesf'], W=[pnk])
                rn, rnk = rnp.next()
                P.recip(rn[:], pn[:], R=[pnk], W=[rnk])
                P.dma('sp', RNd[(g % 2) * 64:(g % 2) * 64 + 64, g // 2:g // 2 + 1], rn[:], R=[rnk], W=['RNd'], allow_slow_non_contiguous=True)
                fc.stage_a(KA, KAk, 128)
                KFs, KFk = KFp.next()

                def consume(xp, xpk, k10, nk, KFs=KFs, KFk=KFk):
                    P.act(KFs[:, :, k10:k10 + nk, :], xp[:, :, 0:nk, :], AF.Identity, R=[xpk], W=[KFk])
                fc.stage_c(consume)
                P.dma('sp', KFd[g], KFs[:], R=[KFk], W=['KFd'])
            P.flush()

    def phase_z():
        with ExitStack() as ps:
            sb = lambda n, s, d: ps.enter_context(nc.sbuf_tensor(n, s, d))
            fc = FFTConsts(ps, True)
            IR = sb('IR', [128, 2, 256], BF16)
            TWI = sb('TWI', [65, 2, 128], F32)
            G = sb('G', [65, 2, 64], BF16)
            P.dma('pool', IR[:], K['IR'], W=['IR'])
            P.dma('sp', TWI[:], K['TWI'], W=['TWI'])
            P.dma('pool', G[:], K['G'], W=['G'])
            ZAp = Pool_(ps, nc, 'ZA', [64, 64, 128], BF16, 2)
            KFp = Pool_(ps, nc, 'KF', [128, 2, 65, 64], BF16, 1)
            Yp = Pool_(ps, nc, 'Y', [128, 2, 64, 65], BF16, 1)
            t1p = Pool_(ps, nc, 'zt1', [128, 4, 64], F32, 2)
            t2p = Pool_(ps, nc, 'zt2', [128, 4, 64], F32, 2)
            crp = Pool_(ps, nc, 'craw', [65, 2, 2, 128], F32, 2)
            m1p = Pool_(ps, nc, 'm1', [65, 2, 2, 128], F32, 2)
            m2p = Pool_(ps, nc, 'm2', [65, 2, 2, 128], F32, 2)
            C2p = Pool_(ps, nc, 'C2', [65, 2, 16, 128], BF16, 2)
            ybp = Pool_(ps, nc, 'ybuf', [64, 16, 128], F32, 2)
            cpp = Pool_(ps, nc, 'cp', [65, 2, 2, 128], F32, 2, psum=True)
            ypp = Pool_(ps, nc, 'yp', [64, 4, 128], F32, 2, psum=True)
            twc = TWI[:, 0:1, :].unsqueeze(1).broadcast_to([65, 2, 2, 128]) if False else None
            for g in range(16):
                c0 = g * 64
                ZA, ZAk = ZAp.next()
                P.dma('sp', ZA[:], Zd[c0:c0 + 64, :].rearrange("c (a b) -> a c b", b=128), W=[ZAk])
                KF, KFk = KFp.next()
                P.dma('sp', KF[:], KFd[g], W=[KFk])
                fc.stage_a(ZA, ZAk, 64)
                Y, Yk = Yp.next()

                def consume(xp, xpk, k10, nk, KF=KF, KFk=KFk, Y=Y, Yk=Yk):
                    t1, t1k = t1p.next()
                    t2, t2k = t2p.next()
                    yo = lambda ri: Y[:, ri, :, k10:k10 + nk].rearrange("p c k -> p k c")
                    P.tt('dve', t1[:, 0:nk, :], xp[:, 0, 0:nk, :], KF[:, 0, k10:k10 + nk, :], ALU.mult, R=[xpk, KFk], W=[t1k])
                    P.tt('dve', t2[:, 0:nk, :], xp[:, 1, 0:nk, :], KF[:, 1, k10:k10 + nk, :], ALU.mult, R=[xpk, KFk], W=[t2k])
                    P.tt('pool', yo(0), t1[:, 0:nk, :], t2[:, 0:nk, :], ALU.subtract, R=[t1k, t2k], W=[Yk])
                    t1, t1k = t1p.next()
                    t2, t2k = t2p.next()
                    P.tt('dve', t1[:, 0:nk, :], xp[:, 0, 0:nk, :], KF[:, 1, k10:k10 + nk, :], ALU.mult, R=[xpk, KFk], W=[t1k])
                    P.tt('dve', t2[:, 0:nk, :], xp[:, 1, 0:nk, :], KF[:, 0, k10:k10 + nk, :], ALU.mult, R=[xpk, KFk], W=[t2k])
                    P.tt('pool', yo(1), t1[:, 0:nk, :], t2[:, 0:nk, :], ALU.add, R=[t1k, t2k], W=[Yk])
                fc.stage_c(consume)
                for cs in range(4):
                    C2, C2k = C2p.next()
                    yb, ybk = ybp.next()
                    for cb in range(8):
                        cp, cpk = cpp.next()
                        for i in range(2):
                            c = cs * 16 + cb * 2 + i
                            o = cp[:, i, :, :].rearrange("p a b -> p (a b)")
                            P.mm(o, Y[:, 0, c, :], IR[:, 0, :], True, False, R=[Yk, 'IR'], W=[cpk])
                            P.mm(o, Y[:, 1, c, :], IR[:, 1, :], False, True, R=[Yk, 'IR'], W=[cpk])
                        cr, crk = crp.next()
                        P.copy('act', cr[:], cp[:], R=[cpk], W=[crk])
                        m1, m1k = m1p.next()
                        m2, m2k = m2p.next()
                        for i in range(2):
                            for r in range(2):
                                P.tt('dve' if r == 0 else 'pool', m1[:, i, r, :], cr[:, i, r, :], TWI[:, 0, :], ALU.mult, R=[crk, 'TWI'], W=[(m1k, i, r)])
                                P.tt('dve' if r == 1 else 'pool', m2[:, i, r, :], cr[:, i, r, :], TWI[:, 1, :], ALU.mult, R=[crk, 'TWI'], W=[(m2k, i, r)])
                        cc0 = cb * 2
                        P.tt('pool', C2[:, 0, cc0:cc0 + 2, :], m1[:, :, 0, :], m2[:, :, 1, :], ALU.subtract,
                             R=[(m1k, 0, 0), (m1k, 1, 0), (m2k, 0, 1), (m2k, 1, 1)], W=[(C2k, cb // 2)])
                        P.tt('dve', C2[:, 1, cc0:cc0 + 2, :], m2[:, :, 0, :], m1[:, :, 1, :], ALU.add,
                             R=[(m2k, 0, 0), (m2k, 1, 0), (m1k, 0, 1), (m1k, 1, 1)], W=[(C2k, cb // 2)])
                        if cb % 2 == 1:
                            q = cb // 2
                            yp, ypk = ypp.next()
                            o = yp[:].rearrange("p a b -> p (a b)")
                            P.mm(o, G[:, 0, :], C2[:, 0, q * 4:q * 4 + 4, :].rearrange("p a b -> p (a b)"), True, False, R=['G', (C2k, q)], W=[ypk])
                            P.mm(o, G[:, 1, :], C2[:, 1, q * 4:q * 4 + 4, :].rearrange("p a b -> p (a b)"), False, True, R=['G', (C2k, q)], W=[ypk])
                            P.copy('act', yb[:, q * 4:q * 4 + 4, :], yp[:], R=[ypk], W=[ybk])
                    cA = c0 + cs * 16
                    P.dma('sp', Ytd[cA:cA + 16, :].rearrange("c (a b) -> a c b", b=128), yb[:], R=[ybk], W=['Ytd'])
            P.flush()

    def load_row(sb_tile, key, src1d, q='sp'):
        P.dma(q, sb_tile, src1d.partition_broadcast(128), W=[key])

    def phase_h5(ysrc, zsrc, x0src, xin, xout, Tn, m, rnsrc):
        TT = 512 if Tn >= 512 else Tn
        nb = TT // 128
        with ExitStack() as ps:
            sb = lambda n, s, d: ps.enter_context(nc.sbuf_tensor(n, s, d))
            wout = sb('wout', [128, 8, D], BF16)
            P.dma('pool', wout[:], W['hy_w_out'].rearrange("(k p) n -> p k n", p=128), W=['wout'])
            rn = sb('rncol', [128, 8], F32)
            bias = sb('hbias', [128, 8], F32)
            P.dma('sp', rn[:], rnsrc, W=['rncol'])
            P.dma('sp', bias[:], W['hy_bias_col'], W=['hbias'])
            brow = sb('brow', [128, D], F32)
            grow = sb('grow', [128, D], F32)
            load_row(brow[:], 'brow', W['hy_b_out'])
            load_row(grow[:], 'grow', MODROW[0, m, 2048:3072])
            ytp = Pool_(ps, nc, 'yt', [128, 8, TT], F32, 2)
            ztp = Pool_(ps, nc, 'zt5', [128, 8, TT], BF16, 2)
            x0p = Pool_(ps, nc, 'x05', [128, 8, TT], BF16, 2)
            a1p = Pool_(ps, nc, 'a1', [128, TT], F32, 2)
            gtp = Pool_(ps, nc, 'gt', [128, 8, TT], BF16, 2)
            xtp = Pool_(ps, nc, 'xt5', [128, nb, D], F32, 2)
            tmp = Pool_(ps, nc, 'tmp5', [128, 512], F32, 3)
            pop = Pool_(ps, nc, 'po', [128, 512], F32, 4, psum=True)
            for ti in range(Tn // TT):
                t0 = ti * TT
                yt, ytk = ytp.next()
                zt, ztk = ztp.next()
                x0, x0k = x0p.next()
                xt, xtk = xtp.next()
                P.dma('sp', yt[:], ysrc[:, t0:t0 + TT].rearrange("(k p) t -> p k t", p=128), W=[ytk])
                P.dma('sp', zt[:], zsrc[:, t0:t0 + TT].rearrange("(k p) t -> p k t", p=128), W=[ztk])
                P.dma('sp', x0[:], x0src[:, t0:t0 + TT].rearrange("(k p) t -> p k t", p=128), W=[x0k])
                P.dma('sp', xt[:], xin[t0:t0 + TT, :].rearrange("(j p) d -> p j d", p=128), W=[xtk])
                gt, gtk = gtp.next()
                for k in range(8):
                    a1, a1k = a1p.next()
                    P.ts('pool', a1[:], zt[:, k, :], bias[:, k:k + 1], None, ALU.mult, None, R=[ztk, 'hbias'], W=[a1k])
                    P.stt(a1[:], yt[:, k, :], rn[:, k:k + 1], a1[:], ALU.mult, ALU.add, R=[ytk, 'rncol', a1k], W=[a1k])
                    P.tt('pool', gt[:, k, :], a1[:], x0[:, k, :], ALU.mult, R=[a1k, x0k], W=[(gtk, k)])
                for tb in range(nb):
                    for dh in range(2):
                        po, pok = pop.next()
                        for k in range(8):
                            P.mm(po[:], gt[:, k, tb * 128:(tb + 1) * 128], wout[:, k, dh * 512:(dh + 1) * 512], k == 0, k == 7,
                                 R=[(gtk, k), 'wout'], W=[pok])
                        tm, tmk = tmp.next()
                        dsl = slice(dh * 512, (dh + 1) * 512)
                        P.tt('dve', tm[:], po[:], brow[:, dsl], ALU.add, R=[pok, 'brow'], W=[tmk])
                        P.tt('pool', tm[:], tm[:], grow[:, dsl], ALU.mult, R=[tmk, 'grow'], W=[tmk])
                        P.tt('pool', xt[:, tb, dsl], tm[:], xt[:, tb, dsl], ALU.add, R=[tmk, xtk], W=[xtk])
                P.dma('sp', xout[t0:t0 + TT, :].rearrange("(j p) d -> p j d", p=128), xt[:], R=[xtk], W=['xout'])
            P.flush()

    def phase_ffn(l, xin, xout, Tn, m, final=False):
        lat = Tn == T
        halo = 64 if lat else 0
        CEN = 256
        NTOK = CEN + 2 * halo
        nblk = NTOK // 128
        gc = 64 if lat else 256
        R_ = CEN // gc
        RH = NTOK // gc
        ntile = Tn // CEN
        with ExitStack() as ps:
            sb = lambda n, s, d: ps.enter_context(nc.sbuf_tensor(n, s, d))
            wup = sb('wup', [128, 8, 2 * FH], BF16)
            wdn = sb('wdn', [128, NFC, D], BF16)
            for k in range(8):
                P.dma('pool', wup[:, k, :], W['ffn_w_up'][l, k * 128:(k + 1) * 128, :], W=['wup'])
            for fcc in range(NFC):
                P.dma('pool', wdn[:, fcc, :], W['ffn_w_down'][l, fcc * 128:(fcc + 1) * 128, :], W=['wdn'])
            cw = sb('cw', [128, 2, 9, NFC], F32)
            cb = sb('cb', [128, 2, NFC], F32)
            P.dma('sp', cw[:], W['ffn_conv_w_col'], W=['cw'])
            P.dma('sp', cb[:], W['ffn_conv_b_col'], W=['cw'])
            grow = sb('grow2', [128, D], F32)
            load_row(grow[:], 'grow2', MODROW[l, m, 5120:6144])
            if final:
                fgrow = sb('fgrow', [128, D], F32)
                load_row(fgrow[:], 'fgrow', W['final_g'][0])
                fss = Pool_(ps, nc, 'fss', [128, 2], F32, 2)
            fe = FrontEnd(ps, nblk)
            xhp = Pool_(ps, nc, 'xh', [128, nblk, D], BF16, 1)
            xcp = Pool_(ps, nc, 'xc', [128, 2, D], F32, 2)
            uTp = Pool_(ps, nc, 'uT', [128, 8, NTOK], BF16, 2)
            h2p = Pool_(ps, nc, 'h2', [128, NFC, CEN], BF16, 1)
            gsp = Pool_(ps, nc, 'gs', [128, NTOK], F32, 2)
            acp = Pool_(ps, nc, 'cacc', [128, CEN], F32, 2)
            sgp = Pool_(ps, nc, 'sg', [128, CEN], F32, 2)
            tmp = Pool_(ps, nc, 'tmpf', [128, 512], F32, 1 if final else 2)
            pap = Pool_(ps, nc, 'pa_f', [128, CEN], F32, 2, psum=True)
            pgp = Pool_(ps, nc, 'pg_f', [128, NTOK], F32, 2, psum=True)
            pop = Pool_(ps, nc, 'po_f', [128, 512], F32, 2, psum=True)
            taps = [(kr, kc) for kr in range(3) for kc in range(3)] if lat else [(1, 0), (1, 1), (1, 2)]
            for ti in range(ntile):
                t0 = ti * CEN
                xh, xhk = xhp.next()
                xc, xck = xcp.next()
                ts_ = t0 - halo
                if lat and ti == 0:
                    P.memset('pool', xh[:, 0, :], 0.0, W=[xhk])
                    P.dma('pool', xh[64:128, 0, :], xin[0:64, :], W=[xhk])
                    P.dma('pool', xh[:, 1:3, :], xin[64:320, :].rearrange("(j p) d -> p j d", p=128), W=[xhk])
                elif lat and ti == ntile - 1:
                    P.memset('pool', xh[:, 2, :], 0.0, W=[xhk])
                    P.dma('pool', xh[0:64, 2, :], xin[Tn - 64:Tn, :], W=[xhk])
                    P.dma('pool', xh[:, 0:2, :], xin[ts_:ts_ + 256, :].rearrange("(j p) d -> p j d", p=128), W=[xhk])
                else:
                    P.dma('pool', xh[:], xin[ts_:ts_ + NTOK, :].rearrange("(j p) d -> p j d", p=128), W=[xhk])
                P.dma('sp', xc[:], xin[t0:t0 + CEN, :].rearrange("(j p) d -> p j d", p=128), W=[xck])
                uT, uTk = uTp.next()
                fe.run(xh, xhk, uT, uTk, l, 1, m)
                h2, h2k = h2p.next()
                for fcc in range(NFC):
                    pa, pak = pap.next()
                    for k in range(8):
                        P.mm(pa[:], wup[:, k, fcc * 128:(fcc + 1) * 128], uT[:, k, halo:halo + CEN], k == 0, k == 7, R=['wup', uTk], W=[pak])
                    pg, pgk = pgp.next()
                    for k in range(8):
                        P.mm(pg[:], wup[:, k, FH + fcc * 128:FH + (fcc + 1) * 128], uT[:, k, :], k == 0, k == 7, R=['wup', uTk], W=[pgk])
                    gs, gsk = gsp.next()
                    P.copy('act', gs[:], pg[:], R=[pgk], W=[gsk])
                    if lat and ti == 0:
                        P.memset('pool', gs[:, 0:64], 0.0, W=[gsk])
                    if lat and ti == ntile - 1:
                        P.memset('pool', gs[:, NTOK - 64:NTOK], 0.0, W=[gsk])
                    gv = gs[:].rearrange("p (r c) -> p r c", c=gc)
                    acc, acck = acp.next()
                    av = acc[:].rearrange("p (r c) -> p r c", c=gc)
                    r0 = 1 if lat else 0
                    P.ts('dve', av, gv[:, r0:r0 + R_, :], cw[:, l, 4, fcc:fcc + 1], cb[:, l, fcc:fcc + 1], ALU.mult, ALU.add,
                         R=[gsk, 'cw'], W=[acck])
                    for (kr, kc) in taps:
                        if (kr, kc) == (1, 1):
                            continue
                        rr = r0 + kr - 1
                        clo = 1 if kc == 0 else 0
                        chi = gc - 1 if kc == 2 else gc
                        P.stt(av[:, :, clo:chi], gv[:, rr:rr + R_, clo + kc - 1:chi + kc - 1], cw[:, l, kr * 3 + kc, fcc:fcc + 1],
                              av[:, :, clo:chi], ALU.mult, ALU.add, R=[gsk, 'cw', acck], W=[acck])
                    sg, sgk = sgp.next()
                    P.act(sg[:], acc[:], AF.Silu, R=[acck], W=[sgk])
                    P.tt('dve', h2[:, fcc, :], sg[:], pa[:], ALU.mult, R=[sgk, pak], W=[(h2k, fcc)])
                if final:
                    ss, ssk = fss.next()
                for tb in range(2):
                    for dh in range(2):
                        po, pok = pop.next()
                        for fcc in range(NFC):
                            P.mm(po[:], h2[:, fcc, tb * 128:(tb + 1) * 128], wdn[:, fcc, dh * 512:(dh + 1) * 512], fcc == 0, fcc == NFC - 1,
                                 R=[(h2k, fcc), 'wdn'], W=[pok])
                        tm, tmk = tmp.next()
                        dsl = slice(dh * 512, (dh + 1) * 512)
                        P.tt('dve', tm[:], po[:], grow[:, dsl], ALU.mult, R=[pok, 'grow2'], W=[tmk])
                        P.tt('pool', xc[:, tb, dsl], tm[:], xc[:, tb, dsl], ALU.add, R=[tmk, xck], W=[xck])
                    if final:
                        P.act(fe.junk[:], xc[:, tb, :], AF.Square, R=[xck], W=['fe_junk', ssk], accum_out=ss[:, tb:tb + 1])
                if final:
                    P.ts('dve', ss[:], ss[:], 1.0 / D, EPS, ALU.mult, ALU.add, R=[ssk], W=[ssk])
                    P.act(ss[:], ss[:], AF.Sqrt, R=[ssk], W=[ssk])
                    P.recip(ss[:], ss[:], R=[ssk], W=[ssk])
                    for tb in range(2):
                        P.stt(xc[:, tb, :], xc[:, tb, :], ss[:, tb:tb + 1], fgrow[:], ALU.mult, ALU.mult, R=[xck, ssk, 'fgrow'], W=[xck])
                P.dma('sp', xout[t0:t0 + CEN, :].rearrange("(j p) d -> p j d", p=128), xc[:], R=[xck], W=['xout'])
            P.flush()

    def phase_kc(zsrc, ydst, rndst):
        with ExitStack() as ps:
            sb = lambda n, s, d: ps.enter_context(nc.sbuf_tensor(n, s, d))
            h3c = sb('h3c', [128, 512], F32)
            with ExitStack() as ps2:
                mlp = FilterMLP(ps2)
                mlp.run(K['posC'], 0, 512, h3c[:, 0:512], 'h3c')
                P.flush()
            wo = sb('wo_f', [128, 2, D], F32)
            P.memset('pool', wo[:], 0.0, W=['wo_f'])
            P.dma('sp', wo[0:64, 0, :], W['hy_wo_st'][0:64, :], W=['wo_f'])
            P.dma('sp', wo[64:128, 1, :], W['hy_wo_st'][64:128, :], W=['wo_f'])
            trow = sb('trow', [128, 511], F32)
            P.dma('sp', trow[:], K['tC'][0].partition_broadcast(128), W=['trow'])
            nd = sb('ndelta', [128, 8], F32)
            P.dma('sp', nd[:], K['ndelta'], W=['ndelta'])
            zcp = Pool_(ps, nc, 'zc', [128, TC], F32, 2)
            zbp = Pool_(ps, nc, 'zb', [128, TC], BF16, 2)
            decp = Pool_(ps, nc, 'decc', [128, 511], F32, 2)
            KLp = Pool_(ps, nc, 'KL', [128, 511], F32, 2)
            accp = Pool_(ps, nc, 'accc', [128, TC], F32, 2)
            nrm = sb('nrmc', [128, 8], F32)
            pkc = Pool_(ps, nc, 'pkc', [128, 512], F32, 2, psum=True)
            for k in range(8):
                pk, pkk = pkc.next()
                P.mm(pk[:, 0:256], wo[:, 1, k * 128:(k + 1) * 128], h3c[:, 0:256], True, True, R=['wo_f', 'h3c'], W=[pkk])
                P.mm(pk[:, 255:511], wo[:, 0, k * 128:(k + 1) * 128], h3c[:, 255:511], True, True, R=['wo_f', 'h3c'], W=[pkk])
                dec, deck = decp.next()
                P.act(dec[:], trow[:], AF.Exp, R=['trow', 'ndelta'], W=[deck], scale=nd[:, k:k + 1])
                KL, KLk = KLp.next()
                P.tt('dve', KL[:], pk[:, 0:511], dec[:], ALU.mult, R=[pkk, deck], W=[KLk])
                P.op('dve', lambda e, o=nrm[:, k:k + 1], i_=KL[:]: e.tensor_reduce(out=o, in_=i_, axis=AX.X, op=ALU.add, apply_absolute_value=True),
                     R=[KLk], W=[('nrmc', k)])
                zb, zbk = zbp.next()
                P.dma('sp', zb[:], zsrc[k * 128:(k + 1) * 128, :], W=[zbk])
                zc, zck = zcp.next()
                P.copy('pool', zc[:], zb[:], R=[zbk], W=[zck])
                acc, acck = accp.next()
                P.ts('dve', acc[:], KL[:, 255:511], zc[:, 0:1], None, ALU.mult, None, R=[KLk, zck], W=[acck])
                for s_ in range(1, TC):
                    P.stt(acc[:], KL[:, 255 - s_:511 - s_], zc[:, s_:s_ + 1], acc[:], ALU.mult, ALU.add, R=[KLk, zck, acck], W=[acck])
                P.dma('sp', ydst[k * 128:(k + 1) * 128, :], acc[:], R=[acck], W=['ydst'])
            P.recip(nrm[:], nrm[:], R=[('nrmc', k) for k in range(8)], W=['nrmc'])
            P.dma('sp', rndst, nrm[:], R=['nrmc'], W=['rndst'])
            P.flush()

    def phase_m1(src, Tn, xmdst, szdst):
        lat = Tn == T
        nblk = 4 if lat else Tn // 128
        TT = nblk * 128
        noc = 32 if lat else 16
        with ExitStack() as ps:
            sb = lambda n, s, d: ps.enter_context(nc.sbuf_tensor(n, s, d))
            win = sb('mwin', [128, 8, 2 * MI], BF16)
            fe = FrontEnd(ps, nblk)
            xtp = Pool_(ps, nc, 'mxt', [128, nblk, D], F32, 2)
            hTp = Pool_(ps, nc, 'mhT', [128, 8, TT], BF16, 2)
            pbp = Pool_(ps, nc, 'mpb', [128, 8, TT], BF16, 2)
            pp = Pool_(ps, nc, 'mpp', [128, TT], F32, 4, psum=True)
            for k in range(8):
                P.dma('pool', win[:, k, :], W['ml_w_in'][k * 128:(k + 1) * 128, :], W=['mwin'])
            m = 0 if lat else 1
            for ti in range(Tn // TT):
                t0 = ti * TT
                xt, xtk = xtp.next()
                P.dma('sp', xt[:], src[t0:t0 + TT, :].rearrange("(j p) d -> p j d", p=128), W=[xtk])
                hT, hTk = hTp.next()
                fe.run(xt, xtk, hT, hTk, 1, 0, m)
                for og in range(noc // 8):
                    pb, pbk = pbp.next()
                    for oi in range(8):
                        oc = og * 8 + oi
                        pt, ptk = pp.next()
                        for k in range(8):
                            P.mm(pt[:], win[:, k, oc * 128:(oc + 1) * 128], hT[:, k, :], k == 0, k == 7, R=['mwin', hTk], W=[ptk])
                        if oc >= 16:
                            P.act(pb[:, oi, :], pt[:], AF.Silu, R=[ptk], W=[pbk])
                        elif oi % 2 == 0:
                            P.copy('act', pb[:, oi, :], pt[:], R=[ptk], W=[pbk])
                        else:
                            P.copy('dve', pb[:, oi, :], pt[:], R=[ptk], W=[pbk])
                    if og < 2:
                        P.dma('sp', xmdst[og * 1024:(og + 1) * 1024, t0:t0 + TT].rearrange("(c p) t -> p c t", p=128), pb[:], R=[pbk], W=['xmdst'])
                    else:
                        o2 = og - 2
                        P.dma('sp', szdst[o2 * 1024:(o2 + 1) * 1024, t0:t0 + TT].rearrange("(c p) t -> p c t", p=128), pb[:], R=[pbk], W=['szdst'])
            P.flush()

    def phase_m2(xmsrc, Tn, qdst, kdst, ktdst, vtdst, sxdst, gpdst):
        lat = Tn == T
        TT = 512 if lat else Tn
        nblk = TT // 128
        DHS = 512.0 ** -0.5
        with ExitStack() as ps:
            sb = lambda n, s, d: ps.enter_context(nc.sbuf_tensor(n, s, d))
            bd = sb('bd', [128, 3, 16, 128], BF16)
            for j, nm in enumerate(('ml_bdq', 'ml_bdk', 'ml_bdv')):
                P.dma('pool', bd[:, j, :, :], W[nm].rearrange("c p m -> p c m"), W=['bd'])
            wg = sb('wg', [128, 48, 16], BF16)
            P.dma('pool', wg[:], W['ml_w_gate'].rearrange("(c p) n -> p c n", p=128), W=['wg'])
            bgrow = sb('bgrow', [128, 16], F32)
            load_row(bgrow[:], 'bgrow', W['ml_b_gate'])
            cwc = sb('mcw', [128, 3, 16], F32)
            cbc = sb('mcb', [128, 16], F32)
            skc = sb('mskip', [128, 16], F32)
            P.dma('sp', cwc[:], W['ml_conv_w_col'], W=['mcw'])
            P.dma('sp', cbc[:], W['ml_conv_b_col'], W=['mcw'])
            P.dma('sp', skc[:], W['ml_skip_col'], W=['mcw'])
            Um = sb('Um', [128, 3, 128], F32)
            P.dma('sp', Um[:, 0, :], K['U'], W=['Um'])
            P.dma('sp', Um[:, 1, :], K['UT'], W=['Um'])
            P.dma('sp', Um[:, 2, :], K['ones'], W=['Um'])
            xmp = Pool_(ps, nc, 'xmh', [128, 16, TT + 2], BF16, 2)
            xcp = Pool_(ps, nc, 'xcm', [128, 16, TT], BF16, 1)
            sxp = Pool_(ps, nc, 'sxc', [128, 16, TT], BF16, 1)
            accp = Pool_(ps, nc, 'macc', [128, TT], F32, 2)
            qp = Pool_(ps, nc, 'qfm', [128, 16, TT], BF16, 1)
            kp = Pool_(ps, nc, 'kfm', [128, 16, TT], BF16, 1)
            vp = Pool_(ps, nc, 'vfm', [128, 16, TT], BF16, 1)
            ktp = Pool_(ps, nc, 'ktm', [128, nblk, MI], BF16, 1)
            vtp = Pool_(ps, nc, 'vtm', [128, nblk, MI], BF16, 1)
            gpp = Pool_(ps, nc, 'gp', [128, nblk, 4, 8], F32, 2)
            gtp = Pool_(ps, nc, 'gates', [128, 16], F32, 2)
            spp = Pool_(ps, nc, 'spl', [128, 2, 4], F32, 2)
            t8p = Pool_(ps, nc, 'tmp8', [128, 8], F32, 2)
            pfm = Pool_(ps, nc, 'pfm', [128, TT], F32, 2, psum=True)
            ptm = Pool_(ps, nc, 'ptm', [128, 512], F32, 2, psum=True)
            pgp = Pool_(ps, nc, 'pgate', [128, 16], F32, 2, psum=True)
            pbp = Pool_(ps, nc, 'pbcum', [128, 2, 8], F32, 2, psum=True)
            for ti in range(Tn // TT):
                t0 = ti * TT
                xm, xmk = xmp.next()
                lo = 1 if ti == 0 else 0
                hi = TT + 1 if ti == Tn // TT - 1 else TT + 2
                if ti == 0:
                    P.memset('pool', xm[:, :, 0:1], 0.0, W=[xmk])
                if ti == Tn // TT - 1:
                    P.memset('pool', xm[:, :, TT + 1:TT + 2], 0.0, W=[xmk])
                P.dma('sp', xm[:, :, lo:hi], xmsrc[:, t0 - 1 + lo:t0 - 1 + hi].rearrange("(c p) t -> p c t", p=128), W=[xmk])
                xc, xck = xcp.next()
                sx, sxk = sxp.next()
                for cc in range(16):
                    acc, acck = accp.next()
                    P.act(acc[:], xm[:, cc, 1:TT + 1], AF.Identity, R=[xmk, 'mcw'], W=[acck], scale=cwc[:, 1, cc:cc + 1], bias=cbc[:, cc:cc + 1])
                    P.stt(acc[:], xm[:, cc, 0:TT], cwc[:, 0, cc:cc + 1], acc[:], ALU.mult, ALU.add, R=[xmk, 'mcw', acck], W=[acck])
                    P.stt(acc[:], xm[:, cc, 2:TT + 2], cwc[:, 2, cc:cc + 1], acc[:], ALU.mult, ALU.add, R=[xmk, 'mcw', acck], W=[acck])
                    P.act(xc[:, cc, :], acc[:], AF.Silu, R=[acck], W=[(xck, cc)])
                    if lat:
                        P.ts('pool', sx[:, cc, :], xc[:, cc, :], skc[:, cc:cc + 1], None, ALU.mult, None, R=[(xck, cc), 'mcw'], W=[sxk])
                if lat:
                    P.dma('sp', sxdst[:, t0:t0 + TT].rearrange("(c p) t -> p c t", p=128), sx[:], R=[sxk], W=['sxdst'])
                qf, qfk = qp.next()
                kf, kfk = kp.next()
                vf, vfk = vp.next()
                for cc in range(16):
                    for j, (dst_, dk, srct, srck) in enumerate(((qf, qfk, xc[:, cc, :], (xck, cc)), (kf, kfk, xc[:, cc, :], (xck, cc)),
                                                             (vf, vfk, xm[:, cc, 1:TT + 1], xmk))):
                        pf, pfk = pfm.next()
                        P.mm(pf[:], bd[:, j, cc, :], srct, True, True, R=['bd', srck], W=[pfk])
                        P.copy('act' if (cc + j) % 2 == 0 else 'dve', dst_[:, cc, :], pf[:], R=[pfk], W=[(dk, cc)])
                if lat:
                    P.dma('sp', qdst[:, t0:t0 + TT].rearrange("(c p) t -> p c t", p=128), qf[:], R=[(qfk, c_) for c_ in range(16)], W=['qdst'])
                    P.dma('sp', kdst[:, t0:t0 + TT].rearrange("(c p) t -> p c t", p=128), kf[:], R=[(kfk, c_) for c_ in range(16)], W=['kdst'])
                kt, ktk = ktp.next()
                vt, vtk = vtp.next()
                for blk in range(nblk):
                    bsl = slice(blk * 128, (blk + 1) * 128)
                    for j, (dst_, dk, which) in enumerate(((kt, ktk, 1), (vt, vtk, 2))):
                        for c4 in range(4):
                            pt, ptk = ptm.next()
                            for i in range(4):
                                cc = c4 * 4 + i
                                lhs = xc[:, cc, bsl] if which == 1 else xm[:, cc, 1 + blk * 128:1 + (blk + 1) * 128]
                                P.mm(pt[:, i * 128:(i + 1) * 128], lhs, bd[:, which, cc, :], True, True,
                                     R=['bd', (xck, cc) if which == 1 else xmk], W=[ptk])
                            P.copy('act' if (c4 + j) % 2 == 0 else 'dve', dst_[:, blk, c4 * 512:(c4 + 1) * 512], pt[:], R=[ptk], W=[dk])
                P.dma('sp', ktdst[t0:t0 + TT, :].rearrange("(j p) n -> p j n", p=128), kt[:], R=[ktk], W=['ktdst'])
                P.dma('sp', vtdst[t0:t0 + TT, :].rearrange("(j p) n -> p j n", p=128), vt[:], R=[vtk], W=['vtdst'])
                gp, gpk = gpp.next()
                for blk in range(nblk):
                    bsl = slice(blk * 128, (blk + 1) * 128)
                    pg, pgk = pgp.next()
                    n_ = 0
                    for j, (src_, sk) in enumerate(((qf, qfk), (kf, kfk), (vf, vfk))):
                        for cc in range(16):
                            P.mm(pg[:], src_[:, cc, bsl], wg[:, j * 16 + cc, :], n_ == 0, n_ == 47, R=[(sk, cc), 'wg'], W=[pgk])
                            n_ += 1
                    gt, gtk = gtp.next()
                    P.tt('dve', gt[:], pg[:], bgrow[:], ALU.add, R=[pgk, 'bgrow'], W=[gtk])
                    gv = gt[:].rearrange("p (d g h) -> p d g h", d=2, g=2)
                    sp_, spk = spp.next()
                    P.act(sp_[:], gv[:, :, 1, :], AF.Exp, R=[gtk], W=[spk], scale=-1.0)
                    P.act(sp_[:], sp_[:], AF.Ln, R=[spk], W=[spk], bias=1.0)
                    pb, pbk = pbp.next()
                    P.mm(pb[:, 0, 0:4], Um[:, 0, :], sp_[:, 0, :], True, True, R=['Um', spk], W=[pbk])
                    P.mm(pb[:, 0, 4:8], Um[:, 1, :], sp_[:, 1, :], True, True, R=['Um', spk], W=[pbk])
                    P.mm(pb[:, 1, :], Um[:, 2, :], sp_[:].rearrange("p d h -> p (d h)"), True, True, R=['Um', spk], W=[pbk])
                    P.act(gp[:, blk, 0:2, :], pb[:], AF.Exp, R=[pbk], W=[gpk], scale=-1.0)
                    t8, t8k = t8p.next()
                    P.tt('dve', t8[:].rearrange("p (d h) -> p d h", d=2), gv[:, :, 0, :], pb[:, 0, :].rearrange("p (d h) -> p d h", d=2), ALU.add,
                         R=[gtk, pbk], W=[t8k])
                    P.act(gp[:, blk, 2, :], t8[:], AF.Exp, R=[t8k], W=[gpk], bias=float(math.log(DHS)))
                    P.tt('dve', gp[:, blk, 3, :], gp[:, blk, 2, :], gp[:, blk, 1, :], ALU.mult, R=[gpk], W=[gpk])
                P.dma('sp', gpdst[t0:t0 + TT, :, :].rearrange("(j p) a b -> p j a b", p=128), gp[:], R=[gpk], W=['gpdst'])
            P.flush()

    def phase_m3():
        with ExitStack() as ps:
            sb = lambda n, s, d: ps.enter_context(nc.sbuf_tensor(n, s, d))
            pdC = ps.enter_context(nc.psum_tensor('pdC', [128, 4, 512], F32))
            Ct = [sb(f'Ct{c}', [128, 4, 512], F32) for c in range(8)]
            Cb = [sb(f'Cb{c}', [128, 4, 512], BF16) for c in range(8)]
            nt = sb('nt', [128, 8, 4], F32)
            nb = sb('nb', [128, 8, 4, 2], BF16)
            maskf = sb('maskf', [128, 2, 128], F32)
            onesb = sb('onesb', [128, 2], BF16)
            P.dma('sp', maskf[:, 0, :], K['U'], W=['maskf'])
            P.dma('sp', maskf[:, 1, :], K['UT'], W=['maskf'])
            P.memset('dve', onesb[:], 1.0, W=['onesb'])
            for c in range(8):
                P.memset('pool', Ct[c][:], 0.0, W=[('Ct', c)])
                P.memset('dve', Cb[c][:], 0.0, W=[('Cb', c)])
            P.memset('dve', nt[:], 0.0, W=[('nt', c) for c in range(8)])
            P.memset('dve', nb[:], 0.0, W=[('nb', c) for c in range(8)])
            qTp = Pool_(ps, nc, 'qT', [128, 4, 128], BF16, 4)
            kTp = Pool_(ps, nc, 'kT', [128, 4, 128], BF16, 4)
            ktp = Pool_(ps, nc, 'ktm3', [128, 512], BF16, 4)
            vtp = Pool_(ps, nc, 'vtm3', [128, 512], BF16, 4)
            gpp = Pool_(ps, nc, 'gp3', [128, 4, 8], F32, 6)
            Stp = Pool_(ps, nc, 'St', [128, 128], BF16, 3)
            k2p = Pool_(ps, nc, 'k2', [128, 512], BF16, 3)
            hop = Pool_(ps, nc, 'hout', [128, 512], F32, 3)
            smp = Pool_(ps, nc, 'sm3', [128, 4], F32, 4)
            pmp = Pool_(ps, nc, 'pmisc', [128, 512], F32, 2, psum=True)
            pnp = Pool_(ps, nc, 'pnum', [128, 512], F32, 2, psum=True)
            gpcache = {}

            def part_a(h, dr, c, srcs, full):
                ktsrc, vtsrc, gpsrc = srcs
                t0 = c * 128
                col = dr * 4 + h
                key = (id(gpsrc), dr, c)
                if key not in gpcache:
                    gp, gpk = gpp.next()
                    P.dma('sp', gp[:], gpsrc[t0:t0 + 128, :, :], W=[gpk])
                    gpcache.clear()
                    gpcache[key] = (gp, gpk)
                    gpcache[('other', dr)] = None
                gp, gpk = gpcache[key]
                kt, ktk = ktp.next()
                vt, vtk = vtp.next()
                P.dma('sp', kt[:], ktsrc[t0:t0 + 128, h * 512:(h + 1) * 512], W=[ktk])
                P.dma('sp', vt[:], vtsrc[t0:t0 + 128, h * 512:(h + 1) * 512], W=[vtk])
                pm, pmk = pmp.next()
                st = dict(h=h, dr=dr, c=c, gp=gp, gpk=gpk, kt=kt, ktk=ktk, vt=vt, vtk=vtk, col=col, full=full, pm=pm, pmk=pmk)
                k2, k2k = k2p.next()
                P.ts('pool', k2[:], kt[:], gp[:, 3, col:col + 1], None, ALU.mult, None, R=[ktk, gpk], W=[k2k])
                st.update(k2=k2, k2k=k2k)
                if full:
                    qT, qTk = qTp.next()
                    kT, kTk = kTp.next()
                    P.dma('sp', qT[:], Qfm[h * 512:(h + 1) * 512, t0:t0 + 128].rearrange("(dc p) t -> p dc t", p=128), W=[qTk])
                    P.dma('sp', kT[:], Kfm[h * 512:(h + 1) * 512, t0:t0 + 128].rearrange("(dc p) t -> p dc t", p=128), W=[kTk])
                    pS, pSk = pm[:, 0:128], (pmk, 'S')
                    for dc in range(4):
                        P.mm(pS, kT[:, dc, :], qT[:, dc, :], dc == 0, dc == 3, R=[kTk, qTk], W=[pSk])
                    St, Stk = Stp.next()
                    P.stt(St[:], pS, gp[:, 2, col:col + 1], maskf[:, dr, :], ALU.mult, ALU.mult, R=[pSk, gpk, 'maskf'], W=[Stk])
                    st.update(qT=qT, qTk=qTk, St=St, Stk=Stk)
                return st

            def part_b(st):
                h, dr, c, col = st['h'], st['dr'], st['c'], st['col']
                ch = h * 2 + dr
                gp, gpk = st['gp'], st['gpk']
                t0 = c * 128
                if st['full']:
                    qT, qTk, St, Stk, vt, vtk = st['qT'], st['qTk'], st['St'], st['Stk'], st['vt'], st['vtk']
                    pn, pnk = pnp.next()
                    P.mm(pn[:], St[:], vt[:], True, False, R=[Stk, vtk], W=[pnk])
                    for dc in range(4):
                        P.mm(pn[:], qT[:, dc, :], Cb[ch][:, dc, :], False, dc == 3, R=[qTk, ('Cb', ch)], W=[pnk])
                    pd, pdk = st['pm'][:, 128:130], (st['pmk'], 'den')
                    P.mm(pd, St[:], onesb[:], True, False, R=[Stk, 'onesb'], W=[pdk])
                    for dc in range(4):
                        P.mm(pd, qT[:, dc, :], nb[:, ch, dc, :], False, dc == 3, R=[qTk, ('nb', ch)], W=[pdk])
                    sm, smk = smp.next()
                    P.act(sm[:, 0:1], st['pm'][:, 128:129], AF.Abs, R=[pdk, gpk], W=[smk], scale=gp[:, 0, col:col + 1])
                    P.ts('dve', sm[:, 0:1], sm[:, 0:1], 1.0, None, ALU.max, None, R=[smk], W=[smk])
                    P.recip(sm[:, 1:2], sm[:, 0:1], R=[smk], W=[smk])
                    P.tt('dve', sm[:, 2:3], sm[:, 1:2], gp[:, 0, col:col + 1], ALU.mult, R=[smk, gpk], W=[smk])
                    ho, hok = hop.next()
                    P.act(ho[:], pn[:], AF.Identity, R=[pnk, smk], W=[hok], scale=sm[:, 2:3])
                    P.dma('sp', HFB[dr, t0:t0 + 128, h * 512:(h + 1) * 512], ho[:], R=[hok], W=['HFB'])
                k2, k2k, vt, vtk = st['k2'], st['k2k'], st['vt'], st['vtk']
                for dc in range(4):
                    P.mm(pdC[:, dc, :], k2[:, dc * 128:(dc + 1) * 128], vt[:], True, True, R=[k2k, vtk], W=[('pdC', dc)])
                pdn, pdnk = st['pm'][:, 256:264].rearrange("p (a b) -> p a b", b=2), (st['pmk'], 'dn')
                for dc in range(4):
                    P.mm(pdn[:, dc, :], k2[:, dc * 128:(dc + 1) * 128], onesb[:], True, True, R=[k2k, 'onesb'], W=[pdnk])
                glc = gp[:, 1, col:col + 1]
                for dc in range(4):
                    P.stt(Ct[ch][:, dc, :], Ct[ch][:, dc, :], glc, pdC[:, dc, :], ALU.mult, ALU.add, R=[('Ct', ch), gpk, ('pdC', dc)], W=[('Ct', ch)])
                P.copy('act', Cb[ch][:], Ct[ch][:], R=[('Ct', ch)], W=[('Cb', ch)])
                P.stt(nt[:, ch, :], nt[:, ch, :], glc, pdn[:, :, 0], ALU.mult, ALU.add, R=[('nt', ch), gpk, pdnk], W=[('nt', ch)])
                P.copy('pool', nb[:, ch, :, 0], nt[:, ch, :], R=[('nt', ch)], W=[('nb', ch)])
                P.copy('pool', nb[:, ch, :, 1], nt[:, ch, :], R=[('nt', ch)], W=[('nb', ch)])

            sched = []
            for s_ in range(2):
                for h in range(4):
                    for dr in range(2):
                        sched.append((h, dr, s_ if dr == 0 else 1 - s_, (KtmC, VtmC, GPdC), False))
            for s_ in range(64):
                for h in range(4):
                    for dr in range(2):
                        sched.append((h, dr, s_ if dr == 0 else 63 - s_, (Ktm, Vtm, GPd), True))
            gp_tiles = {}

            def get_gp(gpsrc, c):
                key = (id(gpsrc), c)
                if key not in gp_tiles:
                    if len(gp_tiles) >= 4:
                        gp_tiles.pop(next(iter(gp_tiles)))
                    gp, gpk = gpp.next()
                    P.dma('sp', gp[:], gpsrc[c * 128:(c + 1) * 128, :, :], W=[gpk])
                    gp_tiles[key] = (gp, gpk)
                return gp_tiles[key]

            def part_a2(h, dr, c, srcs, full):
                gp, gpk = get_gp(srcs[2], c)
                gpcache.clear()
                gpcache[(id(srcs[2]), dr, c)] = (gp, gpk)
                return part_a(h, dr, c, srcs, full)

            prev = None
            for item in sched:
                cur = part_a2(*item)
                if prev is not None:
                    part_b(prev)
                prev = cur
            part_b(prev)
            P.flush()

    def phase_m4(xin, xout):
        with ExitStack() as ps:
            sb = lambda n, s, d: ps.enter_context(nc.sbuf_tensor(n, s, d))
            wdn = sb('mwdn', [128, 16, D], BF16)
            P.dma('pool', wdn[:], W['ml_w_down'].rearrange("(c p) n -> p c n", p=128), W=['mwdn'])
            nwc = sb('nwc', [128, 16], F32)
            P.dma('sp', nwc[:], W['ml_norm_w_col'], W=['nwc'])
            grow = sb('grow4', [128, D], F32)
            load_row(grow[:], 'grow4', MODROW[1, 0, 2048:3072])
            hfp = Pool_(ps, nc, 'hf', [128, MI], F32, 2)
            hbp = Pool_(ps, nc, 'hb', [128, MI], F32, 2)
            hnp = Pool_(ps, nc, 'hn', [128, MI], BF16, 2)
            stp = Pool_(ps, nc, 'bst', [128, 4, 6], F32, 2)
            mvp = Pool_(ps, nc, 'bmv', [128, 4, 2], F32, 2)
            sxp = Pool_(ps, nc, 'sx4', [128, 16, 128], BF16, 2)
            szp = Pool_(ps, nc, 'sz4', [128, 16, 128], BF16, 2)
            m1p = Pool_(ps, nc, 'm14', [128, 128], F32, 3)
            mfp = Pool_(ps, nc, 'mfm', [128, 16, 128], BF16, 2)
            xtp = Pool_(ps, nc, 'xt4', [128, D], F32, 2)
            tmp = Pool_(ps, nc, 'tmp4', [128, 512], F32, 2)
            pTp = Pool_(ps, nc, 'pT4', [128, 4, 128], BF16, 2, psum=True)
            pop = Pool_(ps, nc, 'po4', [128, 512], F32, 2, psum=True)
            for blk in range(T // 128):
                t0 = blk * 128
                hf, hfk = hfp.next()
                hb, hbk = hbp.next()
                sx, sxk = sxp.next()
                sz, szk = szp.next()
                xt, xtk = xtp.next()
                P.dma('sp', hf[:], HFB[0, t0:t0 + 128, :], W=[hfk])
                P.dma('sp', hb[:], HFB[1, t0:t0 + 128, :], W=[hbk])
                P.dma('sp', sx[:], SXC[:, t0:t0 + 128].rearrange("(c p) t -> p c t", p=128), W=[sxk])
                P.dma('sp', sz[:], SZd[:, t0:t0 + 128].rearrange("(c p) t -> p c t", p=128), W=[szk])
                P.dma('sp', xt[:], xin[t0:t0 + 128, :], W=[xtk])
                P.tt('pool', hf[:], hf[:], hb[:], ALU.add, R=[hfk, hbk], W=[hfk])
                bst, bstk = stp.next()
                mv, mvk = mvp.next()
                for h in range(4):
                    P.op('dve', lambda e, o=bst[:, h, :], i_=hf[:, h * 512:(h + 1) * 512]: e.bn_stats(out=o, in_=i_), R=[hfk], W=[(bstk, h)])
                    P.op('dve', lambda e, o=mv[:, h, :], i_=bst[:, h, :]: e.bn_aggr(out=o, in_=i_), R=[(bstk, h)], W=[(mvk, h)])
                mvall = [(mvk, h) for h in range(4)]
                P.ts('dve', mv[:, :, 1], mv[:, :, 1], 1e-5, None, ALU.add, None, R=mvall, W=mvall)
                P.act(mv[:, :, 1], mv[:, :, 1], AF.Sqrt, R=mvall, W=mvall)
                P.recip(mv[:, :, 1], mv[:, :, 1], R=mvall, W=mvall)
                hn, hnk = hnp.next()
                for h in range(4):
                    P.ts('dve', hn[:, h * 512:(h + 1) * 512], hf[:, h * 512:(h + 1) * 512], mv[:, h, 0:1], mv[:, h, 1:2], ALU.subtract, ALU.mult,
                         R=[hfk] + mvall, W=[(hnk, h)])
                mf, mfk = mfp.next()
                for c4 in range(4):
                    pT, pTk = pTp.next()
                    for i in range(4):
                        cc = c4 * 4 + i
                        P.tr(pT[:, i, :], hn[:, cc * 128:(cc + 1) * 128], identb[:], R=[(hnk, cc // 4), 'identb'], W=[pTk])
                    for i in range(4):
                        cc = c4 * 4 + i
                        m1, m1k = m1p.next()
                        P.stt(m1[:], pT[:, i, :], nwc[:, cc:cc + 1], sx[:, cc, :], ALU.mult, ALU.add, R=[pTk, 'nwc', sxk], W=[m1k])
                        P.tt('pool', mf[:, cc, :], m1[:], sz[:, cc, :], ALU.mult, R=[m1k, szk], W=[(mfk, cc)])
                for dh in range(2):
                    po, pok = pop.next()
                    for cc in range(16):
                        P.mm(po[:], mf[:, cc, :], wdn[:, cc, dh * 512:(dh + 1) * 512], cc == 0, cc == 15, R=[(mfk, cc), 'mwdn'], W=[pok])
                    tm, tmk = tmp.next()
                    dsl = slice(dh * 512, (dh + 1) * 512)
                    P.tt('dve', tm[:], po[:], grow[:, dsl], ALU.mult, R=[pok, 'grow4'], W=[tmk])
                    P.tt('pool', xt[:, dsl], tm[:], xt[:, dsl], ALU.add, R=[tmk, xtk], W=[xtk])
                P.dma('sp', xout[t0:t0 + 128, :], xt[:], R=[xtk], W=['xout'])
            P.flush()
    only = stop_after
    run = lambda name: (only is None) or (name in only)
    phase_adaln()
    if run('lat0'):
        phase_h1(x_d, T, P0)
        phase_h2(P0, T, Zd, X0d)
        phase_k()
        phase_z()
        phase_h5(Ytd, Zd, X0d, x_d, XA0, T, 0, RNd)
        phase_ffn(0, XA0, XB0, T, 0)
    if run('ctx0') or run('c1'):
        phase_h1(ctx_d, TC, P0C)
    if run('ctx0') or run('c2'):
        phase_h2(P0C, TC, ZdC, X0dC)
    if run('ctx0') or run('c3'):
        phase_kc(ZdC, YtC, RNdC)
    if run('ctx0') or run('c4'):
        phase_h5(YtC, ZdC, X0dC, ctx_d, CA0, TC, 1, RNdC)
    if run('ctx0') or run('c5'):
        phase_ffn(0, CA0, CB0, TC, 1)
    if run('m1'):
        phase_m1(XB0, T, XMd, SZd)
        phase_m1(CB0, TC, XMC, None)
    if run('m2'):
        phase_m2(XMd, T, Qfm, Kfm, Ktm, Vtm, SXC, GPd)
        phase_m2(XMC, TC, None, None, KtmC, VtmC, None, GPdC)
    if run('m3'):
        phase_m3()
    if run('m4'):
        phase_m4(XB0, XA1)
    if run('f1'):
        phase_ffn(1, XA1, out_d, T, 0, final=True)
    return nc_real, cx


def _blockdiag(w):
    out = np.zeros((16, 128, 128), dtype=np.float32)
    for ch in range(16):
        for n in range(32):
            out[ch, 4 * n:4 * n + 4, 4 * n:4 * n + 4] = w[32 * ch + n]
    return out


def prep_shared(inputs):
    f = lambda k: np.asarray(inputs[k], dtype=np.float32)
    S = {}
    for k in ('mod_w', 'mod_b', 'norm_g', 'ffn_w_up', 'ffn_conv_w', 'ffn_conv_b', 'ffn_w_down'):
        S[k] = f(k)
    S['final_g'] = f('final_g').reshape(1, D)
    for k in ('hy_w_in', 'hy_b_in', 'hy_sc_w', 'hy_sc_b', 'hy_bias', 'hy_w_out', 'hy_b_out', 'ml_w_in', 'ml_conv_w', 'ml_conv_b',
              'ml_w_gate', 'ml_b_gate', 'ml_norm_w', 'ml_skip', 'ml_w_down'):
        S[k] = f(k)[0]
    w1, w2, w3 = f('hy_f_w1')[0], f('hy_f_w2')[0], f('hy_f_w3')[0]
    S['hy_w1d'] = np.concatenate([w1, w1], axis=1)
    z = np.zeros((64, 64), np.float32)
    S['hy_w2bd'] = np.block([[w2, z], [z, w2]])
    S['hy_w3bd'] = np.block([[w3, z], [z, w3]])
    dup = lambda v: np.concatenate([v, v]).reshape(128, 1)
    S['hy_b1d'] = dup(f('hy_f_b1')[0])
    S['hy_b2d'] = dup(f('hy_f_b2')[0])
    S['hy_b3d'] = dup(f('hy_f_b3')[0])
    S['hy_freqd'] = dup(f('hy_freq')[0])
    wo = f('hy_f_wout')[0]
    S['hy_wo_st'] = np.concatenate([wo[:, :D], wo[:, D:]], axis=0)
    def col(v):
        v = np.asarray(v, dtype=np.float32)
        n = v.shape[-1] // 128
        v = v.reshape(v.shape[:-1] + (n, 128))
        return np.moveaxis(v, -1, 0)
    S['mod_b_col'] = col(S['mod_b'])
    S['norm_g_col'] = col(S['norm_g'])
    S['hy_b_in_col'] = col(S['hy_b_in'])
    S['hy_sc_w_col'] = col(S['hy_sc_w'])
    S['hy_sc_b_col'] = col(S['hy_sc_b'])
    S['hy_bias_col'] = col(S['hy_bias'])
    S['ml_conv_w_col'] = col(S['ml_conv_w'])
    S['ml_conv_b_col'] = col(S['ml_conv_b'])
    S['ml_norm_w_col'] = col(S['ml_norm_w'])
    S['ml_skip_col'] = col(S['ml_skip'])
    S['ffn_conv_w_col'] = col(S['ffn_conv_w'].reshape(2, 9, FH))
    S['ffn_conv_b_col'] = col(S['ffn_conv_b'])
    S['ml_bdq'] = _blockdiag(f('ml_wq')[0])
    S['ml_bdk'] = _blockdiag(f('ml_wk')[0])
    S['ml_bdv'] = _blockdiag(f('ml_wv')[0])
    return {k: np.ascontiguousarray(v, dtype=np.float32) for k, v in S.items()}


def prep_core(inputs, S, b):
    d = dict(S)
    d['x'] = np.ascontiguousarray(inputs['x'][b], dtype=np.float32)
    d['ctx'] = np.ascontiguousarray(inputs['ctx'][b], dtype=np.float32)
    d['cvec'] = np.ascontiguousarray(np.stack([inputs['c'][b], inputs['c_ctx']], axis=1), dtype=np.float32)
    return d


def kernel(**inputs):
    S = prep_shared(inputs)
    cores = [prep_core(inputs, S, b % 4) for b in range(8)]
    nc, cx = build(cores[0])
    HC = host_consts()
    in_maps = []
    for c in cores:
        m = dict(c)
        for k, v in HC.items():
            m['c_' + k] = v
        in_maps.append(m)
    res = run_bass_kernel_spmd(nc, in_maps, core_ids=list(range(8)))
    out = np.stack([np.asarray(res.results[b]['out'], dtype=np.float32) for b in range(4)], axis=0)
    return out
```

```python
import math
import numpy as np
import concourse.bass as bass
import concourse.mybir as mybir
from concourse.bass_utils import run_bass_kernel_spmd
from contextlib import ExitStack

F32 = mybir.dt.float32
BF16 = mybir.dt.bfloat16
ALU = mybir.AluOpType
AF = mybir.ActivationFunctionType
AX = mybir.AxisListType

CENG = ('pe', 'dve', 'act', 'pool')
DQ = ('sp', 'pool', 'act')

T = 8192
D = 1024
TC = 256
FH = 2816
NFC = 22
MI = 2048
EPS = 1e-6
MAGIC = 12582912.0
TWO_PI = 2.0 * math.pi


class Prog:
    NDS = 6
    SAME_ENGINE_RELAX = True
    LONG = 200

    def __init__(self, nc, es):
        self.toklen = {}
        self.nc = nc
        self.eng = dict(pe=nc.tensor, dve=nc.vector, act=nc.scalar, pool=nc.gpsimd, sp=nc.sync)
        self.streams = {e: [] for e in self.eng}
        self.ninstr = {e: 0 for e in self.eng}
        self.csem = {e: es.enter_context(nc.semaphore(f"c{e}")) for e in CENG}
        self.ccount = {e: 0 for e in CENG}
        self.dsem = {q: [es.enter_context(nc.semaphore(f"d{q}_{i}")) for i in range(self.NDS)] for q in DQ}
        self.dcount = {q: 0 for q in DQ}
        self.seen = {e: {} for e in self.eng}
        self.lastw = {}
        self.readers = {}

    def _deps(self, R, W):
        deps = []
        for k in R:
            t = self.lastw.get(k)
            if t is not None:
                deps.append((t, 'raw'))
        for k in W:
            t = self.lastw.get(k)
            if t is not None:
                deps.append((t, 'waw'))
            deps.extend((r, 'war') for r in self.readers.get(k, ()))
        return deps

    def _emit_waits(self, e, deps, skip_pe_pe=False):
        seen = self.seen[e]
        need = {}
        for t, kind in deps:
            if t[0] == 'c':
                _, pe_, n = t
                if pe_ == e:
                    if skip_pe_pe and e == 'pe':
                        continue
                    if self.SAME_ENGINE_RELAX and (kind != 'raw' or self.toklen.get((pe_, n), 0) >= self.LONG):
                        continue
                key = ('c', pe_)
                val = n
            else:
                _, q, slot, v = t
                key = ('d', q, slot)
                val = v
            if seen.get(key, 0) >= val:
                continue
            if need.get(key, 0) < val:
                need[key] = val
        for key, val in need.items():
            seen[key] = val
            if key[0] == 'c':
                sem = self.csem[key[1]]
                self.streams[e].append(lambda eng, sem=sem, val=val: eng.wait_ge(sem, val))
            else:
                sem = self.dsem[key[1]][key[2]]
                self.streams[e].append(lambda eng, sem=sem, val=val: eng.wait_ge(sem, 16 * val))

    def _record(self, tok, R, W):
        for k in W:
            self.lastw[k] = tok
            self.readers[k] = []
        for k in R:
            self.readers.setdefault(k, []).append(tok)

    def op(self, e, fn, R=(), W=(), ln=0):
        deps = self._deps(R, W)
        self._emit_waits(e, deps, skip_pe_pe=True)
        self.ccount[e] += 1
        n = self.ccount[e]
        self.toklen[(e, n)] = ln
        sem = self.csem[e]
        self.streams[e].append(lambda eng, fn=fn, sem=sem: fn(eng).then_inc(sem, 1))
        self._record(('c', e, n), R, W)
        self.ninstr[e] += 1

    def dma(self, q, out, in_, R=(), W=(), **kw):
        deps = self._deps(R, W)
        self._emit_waits(q, deps)
        i = self.dcount[q]
        self.dcount[q] += 1
        slot = i % self.NDS
        v = i // self.NDS + 1
        sem = self.dsem[q][slot]
        self.streams[q].append(lambda eng, out=out, in_=in_, sem=sem, kw=kw: eng.dma_start(out=out, in_=in_, **kw).then_inc(sem, 16))
        self._record(('d', q, slot, v), R, W)
        self.ninstr[q] += 1

    def barrier(self):
        toks = []
        for e in CENG:
            if self.ccount[e] > 0:
                toks.append(('c', e, self.ccount[e]))
        for q in DQ:
            n = self.dcount[q]
            for slot in range(self.NDS):
                if n > slot:
                    toks.append(('d', q, slot, (n - 1 - slot) // self.NDS + 1))
        for e in self.eng:
            self._emit_waits(e, [(t, 'bar') for t in toks])
        self.lastw = {}
        self.readers = {}

    def flush(self):
        self.barrier()
        nc = self.nc
        st = self.streams
        with nc.Block() as block:
            @block.tensor
            def _(eng):
                for f in st['pe']:
                    f(eng)

            @block.vector
            def _(eng):
                for f in st['dve']:
                    f(eng)

            @block.scalar
            def _(eng):
                for f in st['act']:
                    f(eng)

            @block.gpsimd
            def _(eng):
                for f in st['pool']:
                    f(eng)

            @block.sync
            def _(eng):
                for f in st['sp']:
                    f(eng)
        self.streams = {e: [] for e in self.eng}

    def mm(self, out, lhsT, rhs, start, stop, R, W):
        self.op('pe', lambda e: e.matmul(out=out, lhsT=lhsT, rhs=rhs, start=start, stop=stop), R, W)

    def tr(self, out, in_, ident, R, W):
        self.op('pe', lambda e: e.transpose(out=out, in_=in_, identity=ident), R, W)

    @staticmethod
    def _ln(ap):
        n = 1
        for d in ap.shape[1:]:
            n *= int(d)
        return n

    def act(self, out, in_, func, R, W, scale=None, bias=None, accum_out=None):
        kw = {}
        if scale is not None:
            kw['scale'] = scale
        if bias is not None:
            kw['bias'] = bias
        if accum_out is not None:
            kw['accum_out'] = accum_out
        self.op('act', lambda e: e.activation(out=out, in_=in_, func=func, **kw), R, W, ln=0 if accum_out is not None else self._ln(out))

    def ts(self, eng, out, in0, s1, s2, op0, op1, R, W):
        if op1 is None:
            self.op(eng, lambda e: e.tensor_scalar(out=out, in0=in0, scalar1=s1, scalar2=None, op0=op0), R, W, ln=self._ln(out))
        else:
            self.op(eng, lambda e: e.tensor_scalar(out=out, in0=in0, scalar1=s1, scalar2=s2, op0=op0, op1=op1), R, W, ln=self._ln(out))

    def tt(self, eng, out, in0, in1, op, R, W):
        self.op(eng, lambda e: e.tensor_tensor(out=out, in0=in0, in1=in1, op=op), R, W, ln=self._ln(out))

    def stt(self, out, in0, scalar, in1, op0, op1, R, W):
        self.op('dve', lambda e: e.scalar_tensor_tensor(out=out, in0=in0, scalar=scalar, in1=in1, op0=op0, op1=op1), R, W, ln=self._ln(out))

    def copy(self, eng, out, in_, R, W):
        if eng == 'act':
            self.op('act', lambda e: e.copy(out=out, in_=in_), R, W, ln=self._ln(out))
        else:
            self.op(eng, lambda e: e.tensor_copy(out=out, in_=in_), R, W, ln=self._ln(out))

    def memset(self, eng, ap, val, W):
        self.op(eng, lambda e: e.memset(ap, val), (), W, ln=self._ln(ap))

    def recip(self, out, in_, R, W):
        self.op('dve', lambda e: e.reciprocal(out=out, in_=in_), R, W)


class Ctx:
    def __init__(self, nc, debug):
        self.nc = nc
        self.debug = debug
        self.inputs = {}
        self.feed = None
        self.dbg_names = []

    def din(self, name, arr):
        arr = np.ascontiguousarray(arr, dtype=np.float32)
        self.inputs[name] = arr
        return self.nc.dram_tensor(name, list(arr.shape), F32, kind="ExternalInput").ap()

    def dscr(self, name, shape, dt):
        if self.feed and name in self.feed:
            return self.nc.dram_tensor(name, list(shape), dt, kind="ExternalInput").ap()
        if self.debug and name in self.debug:
            self.dbg_names.append(name)
            return self.nc.dram_tensor(name, list(shape), dt, kind="ExternalOutput").ap()
        return self.nc.dram_tensor(name, list(shape), dt, kind="Internal").ap()


class Pool_:
    def __init__(self, es, nc, name, shape, dt, n, psum=False):
        mk = nc.psum_tensor if psum else nc.sbuf_tensor
        self.tiles = [es.enter_context(mk(f"{name}{i}", shape, dt)) for i in range(n)]
        self.name = name
        self.i = -1

    def next(self):
        self.i = (self.i + 1) % len(self.tiles)
        return self.tiles[self.i], (self.name, self.i)


def _pos_feats(idx, L):
    idx = np.asarray(idx, dtype=np.float64)
    t = (idx / (L - 1)).astype(np.float32).astype(np.float64)
    w = (2.0 * math.pi * idx.astype(np.float32) / np.float32(L)).astype(np.float64)
    bands = np.linspace(1e-4, 15.0, 16, dtype=np.float32).astype(np.float64)
    arg = bands[None, :] * w[:, None]
    return np.concatenate([t[:, None], np.cos(arg), -np.sin(arg)], axis=1).astype(np.float32)


def host_consts():
    C = {}
    C['ident'] = np.eye(128, dtype=np.float32)
    n = np.arange(128)
    k1 = np.arange(65)
    ang = 2 * np.pi * np.outer(n, k1) / 128.0
    C['FA'] = np.concatenate([np.cos(ang), -np.sin(ang[:, 1:64])], axis=1)
    n2 = n[:, None, None]
    kk = (k1[None, :, None] + 128 * n[None, None, :])
    th = 2 * np.pi * (n2 * kk % 16384) / 16384.0
    C['TC'] = np.stack([np.cos(th), np.sin(th)], axis=2)
    ph = 2 * np.pi * np.outer(n, n) / 128.0
    C['IR'] = np.stack([np.concatenate([np.cos(ph), np.sin(ph)], 1), np.concatenate([-np.sin(ph), np.cos(ph)], 1)], axis=1)
    wk = np.where((k1 == 0) | (k1 == 64), 1.0, 2.0) / 16384.0
    tw = 2 * np.pi * np.outer(k1, n) / 16384.0
    C['TWI'] = np.stack([wk[:, None] * np.cos(tw), wk[:, None] * np.sin(tw)], axis=1)
    n1 = np.arange(64)
    g = 2 * np.pi * np.outer(k1, n1) / 128.0
    C['G'] = np.stack([np.cos(g), -np.sin(g)], axis=1)
    N2, N1 = np.meshgrid(np.arange(128), np.arange(128), indexing='ij')
    fwd = N1 < 64
    idx = np.where(fwd, 128 * N1 + N2, 16384 - 128 * N1 - N2)
    idx = np.where(idx >= T, 0, idx)
    C['posR'] = _pos_feats(idx.reshape(-1), T).T.copy()
    deltas = np.abs(np.linspace(math.log(1e-2) / 1.5, math.log(1e-2) / 0.3, D, dtype=np.float32)).astype(np.float64)
    C['absdelta'] = deltas
    a_n1 = np.where(np.arange(128) < 64, 128.0 * np.arange(128), 16384.0 - 128.0 * np.arange(128)) / (T - 1)
    C['decL'] = np.stack([a_n1, (np.arange(128) < 64).astype(np.float64), (np.arange(128) >= 64).astype(np.float64)], axis=0)
    r0 = np.broadcast_to(-deltas[None, :], (128, D))
    r1 = -np.outer(np.arange(128) / (T - 1), deltas)
    C['decR'] = np.stack([r0, r1, -r1], axis=0)
    dch = np.concatenate([np.arange(255, 0, -1), np.arange(0, 256)])
    C['posC'] = _pos_feats(np.concatenate([dch, [0]]), TC).T.copy()
    C['tC'] = (dch / (TC - 1.0)).astype(np.float32)[None, :]
    C['ndelta'] = (-deltas).reshape(8, 128).T.copy()
    i = np.arange(128)
    C['U'] = (i[:, None] <= i[None, :]).astype(np.float32)
    C['UT'] = (i[:, None] >= i[None, :]).astype(np.float32)
    C['ones'] = np.ones((128, 128), dtype=np.float32)
    return {k: np.ascontiguousarray(v, dtype=np.float32) for k, v in C.items()}


class _NC:
    def __init__(self, nc):
        self._nc = nc
        self._uid = 0

    def __getattr__(self, k):
        return getattr(self._nc, k)

    def sbuf_tensor(self, name, shape, dt):
        self._uid += 1
        return self._nc.sbuf_tensor(f"{name}_{self._uid}", shape, dt)

    def psum_tensor(self, name, shape, dt):
        self._uid += 1
        return self._nc.psum_tensor(f"{name}_{self._uid}", shape, dt)


def build(inp, debug=False, stop_after=None, feed=None):
    nc_real = bass.Bass("TRN2", target_bir_lowering=False)
    nc = _NC(nc_real)
    cx = Ctx(nc, debug)
    cx.feed = feed
    HC = host_consts()
    es = ExitStack()
    P = Prog(nc, es)

    def sbp(name, shape, dt):
        return es.enter_context(nc.sbuf_tensor(name, shape, dt))

    x_d = cx.din('x', inp['x'])
    ctx_d = cx.din('ctx', inp['ctx'])
    cvec_d = cx.din('cvec', inp['cvec'])
    W = {}
    for k in ('mod_w', 'mod_b', 'norm_g', 'final_g', 'hy_w_in', 'hy_b_in', 'hy_sc_w', 'hy_sc_b', 'hy_w1d', 'hy_b1d',
              'hy_w2bd', 'hy_b2d', 'hy_w3bd', 'hy_b3d', 'hy_wo_st', 'hy_freqd', 'hy_bias', 'hy_w_out', 'hy_b_out',
              'ml_w_in', 'ml_conv_w', 'ml_conv_b', 'ml_bdq', 'ml_bdk', 'ml_bdv', 'ml_w_gate', 'ml_b_gate', 'ml_norm_w',
              'ml_skip', 'ml_w_down', 'ffn_w_up', 'ffn_conv_w', 'ffn_conv_b', 'ffn_w_down',
              'mod_b_col', 'norm_g_col', 'hy_b_in_col', 'hy_sc_w_col', 'hy_sc_b_col', 'hy_bias_col', 'ml_conv_w_col',
              'ml_conv_b_col', 'ml_norm_w_col', 'ml_skip_col', 'ffn_conv_w_col', 'ffn_conv_b_col'):
        W[k] = cx.din(k, inp[k])
    K = {k: cx.din('c_' + k, v) for k, v in HC.items()}
    out_d = nc.dram_tensor('out', [T, D], F32, kind="ExternalOutput").ap()

    MODROW = cx.dscr('MODROW', [2, 2, 6144], F32)
    P0 = cx.dscr('P0', [3 * D, T], BF16)
    P0C = cx.dscr('P0C', [3 * D, TC], BF16)
    Zd = cx.dscr('Zd', [D, T], BF16)
    X0d = cx.dscr('X0d', [D, T], BF16)
    KFd = cx.dscr('KFd', [16, 128, 2, 65, 64], BF16)
    RNd = cx.dscr('RNd', [128, 8], F32)
    Ytd = cx.dscr('Ytd', [D, T], F32)
    XA0 = cx.dscr('XA0', [T, D], F32)
    XB0 = cx.dscr('XB0', [T, D], F32)
    CA0 = cx.dscr('CA0', [TC, D], F32)
    CB0 = cx.dscr('CB0', [TC, D], F32)
    XA1 = cx.dscr('XA1', [T, D], F32)
    ZdC = cx.dscr('ZdC', [D, TC], BF16)
    XMd = cx.dscr('XMd', [MI, T], BF16)
    SZd = cx.dscr('SZd', [MI, T], BF16)
    XMC = cx.dscr('XMC', [MI, TC], BF16)
    Qfm = cx.dscr('Qfm', [MI, T], BF16)
    Kfm = cx.dscr('Kfm', [MI, T], BF16)
    Ktm = cx.dscr('Ktm', [T, MI], BF16)
    Vtm = cx.dscr('Vtm', [T, MI], BF16)
    SXC = cx.dscr('SXC', [MI, T], BF16)
    GPd = cx.dscr('GPd', [T, 4, 8], F32)
    KtmC = cx.dscr('KtmC', [TC, MI], BF16)
    VtmC = cx.dscr('VtmC', [TC, MI], BF16)
    GPdC = cx.dscr('GPdC', [TC, 4, 8], F32)
    HFB = cx.dscr('HFB', [2, T, MI], F32)
    X0dC = cx.dscr('X0dC', [D, TC], BF16)
    YtC = cx.dscr('YtC', [D, TC], F32)
    RNdC = cx.dscr('RNdC', [128, 8], F32)

    identb = sbp('identb', [128, 128], BF16)
    identf = sbp('identf', [128, 128], F32)
    modcol = sbp('modcol', [128, 2, 48, 2], F32)
    effs = sbp('effs', [128, 2, 2, 2, 8], F32)
    P.dma('sp', identf[:], K['ident'], W=['identf'])
    P.copy('dve', identb[:], identf[:], R=['identf'], W=['identb'])

    def phase_adaln():
        with ExitStack() as ps:
            sb = lambda n, s, d: ps.enter_context(nc.sbuf_tensor(n, s, d))
            cv = sb('cv', [128, 8, 2], F32)
            scv = sb('scv', [128, 8, 2], F32)
            mbcol = sb('mbcol', [128, 2, 48], F32)
            mbrow = sb('mbrow', [2, 2, 6144], F32)
            rowbuf = sb('rowbuf', [2, 2, 6144], F32)
            ngcol = sb('ngcol', [128, 2, 2, 8], F32)
            mwp = Pool_(ps, nc, 'mw', [128, 8, 1024], F32, 2)
            pcol = ps.enter_context(nc.psum_tensor('pcol', [128, 48, 2], F32))
            prow = Pool_(ps, nc, 'prow', [2, 512], F32, 2, psum=True)
            P.dma('sp', cv[:], cvec_d.rearrange("(k p) m -> p k m", p=128), W=['cv'])
            P.dma('sp', mbcol[:], W['mod_b_col'], W=['mbcol'])
            for l in range(2):
                P.dma('sp', mbrow[:, l, :], W['mod_b'][l:l + 1, :].partition_broadcast(2) if False else W['mod_b'][l:l + 1, :].broadcast_to([2, 6144]), W=['mbrow'])
            P.dma('sp', ngcol[:], W['norm_g_col'], W=['ngcol'])
            P.act(scv[:], cv[:], AF.Silu, R=['cv'], W=['scv'])
            for l in range(2):
                for j in range(6):
                    mw, mwk = mwp.next()
                    P.dma('sp', mw[:], W['mod_w'][l, :, j * 1024:(j + 1) * 1024].rearrange("(k p) n -> p k n", p=128), W=[mwk])
                    for m in range(8):
                        for k in range(8):
                            P.mm(pcol[:, j * 8 + m, :], mw[:, k, m * 128:(m + 1) * 128], scv[:, k, :], k == 0, k == 7,
                                 R=[mwk, 'scv'], W=['pcol'])
                    for half in range(2):
                        pr, prk = prow.next()
                        for k in range(8):
                            P.mm(pr[:], scv[:, k, :], mw[:, k, half * 512:(half + 1) * 512], k == 0, k == 7, R=[mwk, 'scv'], W=[prk])
                        o = j * 1024 + half * 512
                        P.tt('dve', rowbuf[:, l, o:o + 512], pr[:], mbrow[:, l, o:o + 512], ALU.add, R=[prk, 'mbrow'], W=['rowbuf'])
                for m in range(2):
                    P.tt('dve', modcol[:, l, :, m], pcol[:, :, m], mbcol[:, l, :], ALU.add, R=['pcol', 'mbcol'], W=['modcol'])
                for i in range(2):
                    for m in range(2):
                        sc_ap = modcol[:, l, 8 + 24 * i:16 + 24 * i, m]
                        P.ts('dve', effs[:, l, i, m, :], sc_ap, 1.0, None, ALU.add, None, R=['modcol'], W=['effs'])
                        P.tt('dve', effs[:, l, i, m, :], effs[:, l, i, m, :], ngcol[:, l, i, :], ALU.mult, R=['effs', 'ngcol'], W=['effs'])
            P.dma('sp', MODROW.rearrange("l m n -> m l n"), rowbuf[:], R=['rowbuf'], W=['MODROW'])
            P.flush()

    def shift_col(l, i, m):
        return modcol[:, l, 24 * i:24 * i + 8, m]

    class FrontEnd:
        def __init__(self, ps, nblk, nbuf=2):
            self.nblk = nblk
            self.xn = Pool_(ps, nc, 'fe_xn', [128, nblk, D], BF16, 1)
            self.junk = ps.enter_context(nc.sbuf_tensor('fe_junk', [128, D], BF16))
            self.ss = Pool_(ps, nc, 'fe_ss', [128, nblk], F32, 2)
            self.pT = Pool_(ps, nc, 'fe_pT', [128, nblk * 128], BF16, 2, psum=True)

        def run(self, xt, xtk, hT, hTk, l, i, m):
            nblk = self.nblk
            ss, ssk = self.ss.next()
            xn, xnk = self.xn.next()
            for j in range(nblk):
                P.act(self.junk[:], xt[:, j, :], AF.Square, R=[xtk], W=['fe_junk', ssk], accum_out=ss[:, j:j + 1])
            P.ts('dve', ss[:], ss[:], 1.0 / D, EPS, ALU.mult, ALU.add, R=[ssk], W=[ssk])
            P.act(ss[:], ss[:], AF.Sqrt, R=[ssk], W=[ssk])
            P.recip(ss[:], ss[:], R=[ssk], W=[ssk])
            for j in range(nblk):
                P.ts('pool', xn[:, j, :], xt[:, j, :], ss[:, j:j + 1], None, ALU.mult, None, R=[xtk, ssk], W=[xnk])
            for k in range(8):
                pT, pTk = self.pT.next()
                for j in range(nblk):
                    P.tr(pT[:, j * 128:(j + 1) * 128], xn[:, j, k * 128:(k + 1) * 128], identb[:], R=[xnk, 'identb'], W=[pTk])
                P.act(hT[:, k, :], pT[:], AF.Identity, R=[pTk, 'effs', 'modcol'], W=[hTk],
                      scale=effs[:, l, i, m, k:k + 1], bias=shift_col(l, i, m)[:, k:k + 1])

    def phase_h1(src, Tn, dst):
        nblk = 4 if Tn >= 512 else Tn // 128
        TT = nblk * 128
        with ExitStack() as ps:
            sb = lambda n, s, d: ps.enter_context(nc.sbuf_tensor(n, s, d))
            win = sb('win', [128, 8, 3 * D], BF16)
            bcol = sb('bcol', [128, 24], F32)
            fe = FrontEnd(ps, nblk)
            xtp = Pool_(ps, nc, 'xt', [128, nblk, D], F32, 2)
            hTp = Pool_(ps, nc, 'hT', [128, 8, TT], BF16, 2)
            pbp = Pool_(ps, nc, 'pb', [128, 6, TT], BF16, 2)
            pp = Pool_(ps, nc, 'pp', [128, TT], F32, 4, psum=True)
            for k in range(8):
                P.dma('pool', win[:, k, :], W['hy_w_in'][k * 128:(k + 1) * 128, :], W=['win'])
            P.dma('sp', bcol[:], W['hy_b_in_col'], W=['bcol'])
            m = 0 if Tn == T else 1
            for ti in range(Tn // TT):
                t0 = ti * TT
                xt, xtk = xtp.next()
                P.dma('sp', xt[:], src[t0:t0 + TT, :].rearrange("(j p) d -> p j d", p=128), W=[xtk])
                hT, hTk = hTp.next()
                fe.run(xt, xtk, hT, hTk, 0, 0, m)
                for og in range(4):
                    pb, pbk = pbp.next()
                    for oi in range(6):
                        oc = og * 6 + oi
                        pt, ptk = pp.next()
                        for k in range(8):
                            P.mm(pt[:], win[:, k, oc * 128:(oc + 1) * 128], hT[:, k, :], k == 0, k == 7, R=['win', hTk], W=[ptk])
                        if oi % 2 == 0:
                            P.act(pb[:, oi, :], pt[:], AF.Identity, R=[ptk, 'bcol'], W=[pbk], bias=bcol[:, oc:oc + 1])
                        else:
                            P.ts('dve', pb[:, oi, :], pt[:], bcol[:, oc:oc + 1], None, ALU.add, None, R=[ptk, 'bcol'], W=[pbk])
                    P.dma('sp', dst[og * 768:(og + 1) * 768, t0:t0 + TT].rearrange("(c p) t -> p c t", p=128), pb[:], R=[pbk], W=['P0'])
            P.flush()

    def phase_h2(src, Tn, zdst, x0dst):
        PW = 2048 if Tn >= 2048 else Tn
        npc = Tn // PW
        with ExitStack() as ps:
            sb = lambda n, s, d: ps.enter_context(nc.sbuf_tensor(n, s, d))
            scw = sb('scw', [128, 3, 24], F32)
            scb = sb('scb', [128, 24], F32)
            pinp = Pool_(ps, nc, 'pin', [128, 3, PW + 2], BF16, 2)
            accp = Pool_(ps, nc, 'acc', [128, 3, PW], F32, 2)
            zp = Pool_(ps, nc, 'zt', [128, PW], BF16, 2)
            x0p = Pool_(ps, nc, 'x0t', [128, PW], BF16, 2)
            P.dma('sp', scw[:], W['hy_sc_w_col'], W=['scw'])
            P.dma('sp', scb[:], W['hy_sc_b_col'], W=['scb'])
            srcv = src.rearrange("(j c p) t -> c p j t", j=3, c=8, p=128)
            for cc in range(8):
                for pi in range(npc):
                    t0 = pi * PW
                    pin, pink = pinp.next()
                    lo = 1 if pi == 0 else 0
                    hi = PW + 1 if pi == npc - 1 else PW + 2
                    if pi == 0:
                        P.memset('pool', pin[:, :, 0:1], 0.0, W=[pink])
                    if pi == npc - 1:
                        P.memset('pool', pin[:, :, PW + 1:PW + 2], 0.0, W=[pink])
                    P.dma('sp', pin[:, :, lo:hi], srcv[cc][:, :, t0 - 1 + lo:t0 - 1 + hi], W=[pink])
                    acc, acck = accp.next()
                    zt, ztk = zp.next()
                    x0t, x0k = x0p.next()
                    for j in range(3):
                        oc = j * 8 + cc
                        P.act(acc[:, j, :], pin[:, j, 1:PW + 1], AF.Identity, R=[pink, 'scw', 'scb'], W=[(acck, j)],
                              scale=scw[:, 1, oc:oc + 1], bias=scb[:, oc:oc + 1])
                        P.stt(acc[:, j, :], pin[:, j, 0:PW], scw[:, 0, oc:oc + 1], acc[:, j, :], ALU.mult, ALU.add,
                              R=[pink, 'scw', (acck, j)], W=[(acck, j)])
                        if j == 0:
                            P.stt(x0t[:], pin[:, j, 2:PW + 2], scw[:, 2, oc:oc + 1], acc[:, j, :], ALU.mult, ALU.add,
                                  R=[pink, 'scw', (acck, j)], W=[x0k])
                        else:
                            P.stt(acc[:, j, :], pin[:, j, 2:PW + 2], scw[:, 2, oc:oc + 1], acc[:, j, :], ALU.mult, ALU.add,
                                  R=[pink, 'scw', (acck, j)], W=[(acck, j)])
                    P.tt('pool', zt[:], acc[:, 1, :], acc[:, 2, :], ALU.mult, R=[(acck, 1), (acck, 2)], W=[ztk])
                    P.dma('sp', zdst[cc * 128:(cc + 1) * 128, t0:t0 + PW], zt[:], R=[ztk], W=['Zd'])
                    P.dma('sp', x0dst[cc * 128:(cc + 1) * 128, t0:t0 + PW], x0t[:], R=[x0k], W=['X0d'])
            P.flush()

    def sin_layer(pre, prek, frc, frb, tmpa, tmpk, tmpb, tmpbk, out, outk):
        P.ts('dve', tmpa, pre, frc, frb, ALU.mult, ALU.add, R=[prek, 'mlpc'], W=[tmpk])
        P.ts('dve', tmpb, tmpa, 1.0 / TWO_PI, MAGIC, ALU.mult, ALU.add, R=[tmpk], W=[tmpbk])
        P.ts('dve', tmpb, tmpb, -MAGIC, -TWO_PI, ALU.add, ALU.mult, R=[tmpbk], W=[tmpbk])
        P.tt('dve', tmpa, tmpa, tmpb, ALU.add, R=[tmpk, tmpbk], W=[tmpk])
        P.act(out, tmpa, AF.Sin, R=[tmpk], W=[outk])

    class FilterMLP:
        def __init__(self, ps):
            sb = lambda n, s, d: ps.enter_context(nc.sbuf_tensor(n, s, d))
            self.w1 = sb('mlp_w1', [33, 128], F32)
            self.w2 = sb('mlp_w2', [128, 128], F32)
            self.w3 = sb('mlp_w3', [128, 128], F32)
            self.cols = sb('mlp_cols', [128, 8], F32)
            P.dma('sp', self.w1[:], W['hy_w1d'], W=['mlpw'])
            P.dma('sp', self.w2[:], W['hy_w2bd'], W=['mlpw'])
            P.dma('sp', self.w3[:], W['hy_w3bd'], W=['mlpw'])
            P.dma('sp', self.cols[:, 0:1], W['hy_freqd'], W=['mlpc'])
            P.dma('sp', self.cols[:, 1:2], W['hy_b1d'], W=['mlpc'])
            P.dma('sp', self.cols[:, 2:3], W['hy_b2d'], W=['mlpc'])
            P.dma('sp', self.cols[:, 3:4], W['hy_b3d'], W=['mlpc'])
            P.ts('dve', self.cols[:, 4:7], self.cols[:, 1:4], self.cols[:, 0:1], None, ALU.mult, None, R=['mlpc'], W=['mlpc'])
            self.posp = Pool_(ps, nc, 'mlp_pos', [33, 512], F32, 2)
            self.ta = Pool_(ps, nc, 'mlp_ta', [128, 512], F32, 2)
            self.tb = Pool_(ps, nc, 'mlp_tb', [128, 512], F32, 2)
            self.h = Pool_(ps, nc, 'mlp_h', [128, 512], F32, 2)
            self.pm = Pool_(ps, nc, 'mlp_pm', [128, 512], F32, 2, psum=True)

        def run(self, pos_d, c0, n, out, outk):
            pos, posk = self.posp.next()
            P.dma('sp', pos[:, 0:n], pos_d[:, c0:c0 + n], W=[posk])
            cur, curk, ws = pos[:, 0:n], posk, [self.w1, self.w2, self.w3]
            for li in range(3):
                pm, pmk = self.pm.next()
                P.mm(pm[:, 0:n], ws[li][:], cur, True, True, R=['mlpw', curk], W=[pmk])
                ta, tak = self.ta.next()
                tb, tbk = self.tb.next()
                if li < 2:
                    h, hk = self.h.next()
                    o, ok = h[:, 0:n], hk
                else:
                    o, ok = out, outk
                sin_layer(pm[:, 0:n], pmk, self.cols[:, 0:1], self.cols[:, 4 + li:5 + li], ta[:, 0:n], tak, tb[:, 0:n], tbk, o, ok)
                cur, curk = o, ok

    class FFTConsts:
        def __init__(self, ps, inverse):
            sb = lambda n, s, d: ps.enter_context(nc.sbuf_tensor(n, s, d))
            self.FA = sb('FA', [128, 128], BF16)
            self.TC = sb('TC', [128, 65, 2, 128], BF16)
            P.dma('pool', self.FA[:], K['FA'], W=['fftc'])
            P.dma('pool', self.TC[:], K['TC'], W=['fftc'])
            self.B = sb('Bbuf', [128, 3, 65, 64], BF16)
            P.memset('dve', self.B[:, 1, 0:1, :], 0.0, W=['Bbuf'])
            P.memset('dve', self.B[:, 1, 64:65, :], 0.0, W=['Bbuf'])
            self.pa = Pool_(ps, nc, 'pa', [128, 4, 128], F32, 2, psum=True)
            self.xp = Pool_(ps, nc, 'xp', [128, 2, 4, 64], F32, 2, psum=True)

        def stage_a(self, src, srck, Kp):
            B = self.B
            for c4 in range(16):
                pa, pak = self.pa.next()
                for i in range(4):
                    c = c4 * 4 + i
                    P.mm(pa[:, i, :], src[0:Kp, c, :], self.FA[0:Kp, :], True, True, R=[srck, 'fftc'], W=[pak])
                o0 = B[:, 0, :, c4 * 4:c4 * 4 + 4].rearrange("p k c -> p c k")
                o2 = B[:, 2, :, c4 * 4:c4 * 4 + 4].rearrange("p k c -> p c k")
                o1 = B[:, 1, 1:64, c4 * 4:c4 * 4 + 4].rearrange("p k c -> p c k")
                P.act(o0, pa[:, :, 0:65], AF.Identity, R=[pak], W=['Bbuf'])
                P.act(o2, pa[:, :, 0:65], AF.Identity, R=[pak], W=['Bbuf'], scale=-1.0)
                P.copy('dve', o1, pa[:, :, 65:128], R=[pak], W=['Bbuf'])

        def stage_c(self, consume):
            B, TC = self.B, self.TC
            for kq in range(17):
                k10 = kq * 4
                nk = min(4, 65 - k10)
                xp, xpk = self.xp.next()
                for i in range(nk):
                    k1 = k10 + i
                    P.mm(xp[:, 0, i, :], TC[:, k1, 0, :], B[:, 0, k1, :], True, False, R=['fftc', 'Bbuf'], W=[xpk])
                    P.mm(xp[:, 0, i, :], TC[:, k1, 1, :], B[:, 1, k1, :], False, True, R=['fftc', 'Bbuf'], W=[xpk])
                    P.mm(xp[:, 1, i, :], TC[:, k1, 0, :], B[:, 1, k1, :], True, False, R=['fftc', 'Bbuf'], W=[xpk])
                    P.mm(xp[:, 1, i, :], TC[:, k1, 1, :], B[:, 2, k1, :], False, True, R=['fftc', 'Bbuf'], W=[xpk])
                consume(xp, xpk, k10, nk)

    def phase_k():
        with ExitStack() as ps:
            sb = lambda n, s, d: ps.enter_context(nc.sbuf_tensor(n, s, d))
            h3 = sb('h3', [128, 16384], BF16)
            with ExitStack() as ps2:
                mlp = FilterMLP(ps2)
                for ct in range(32):
                    mlp.run(K['posR'], ct * 512, 512, h3[:, ct * 512:(ct + 1) * 512], 'h3')
                h3v = h3[:].rearrange("p (a b) -> p a b", b=128)
                P.memset('pool', h3v[0:64, :, 64:128], 0.0, W=['h3'])
                P.memset('pool', h3v[64:128, :, 0:64], 0.0, W=['h3'])
                P.memset('pool', h3v[64:128, 0:1, 64:65], 0.0, W=['h3'])
                P.flush()
            fc = FFTConsts(ps, False)
            wo = sb('wo', [128, D], BF16)
            decL = sb('decL', [3, 128], F32)
            ones = sb('onesf', [128, 1], F32)
            P.dma('pool', wo[:], W['hy_wo_st'], W=['wo'])
            P.dma('sp', decL[:], K['decL'], W=['decL'])
            P.memset('dve', ones[:], 1.0, W=['onesf'])
            KAp = Pool_(ps, nc, 'KA', [128, 64, 128], BF16, 2)
            KFp = Pool_(ps, nc, 'KFs', [128, 2, 65, 64], BF16, 2)
            decRp = Pool_(ps, nc, 'decR', [3, 8, 64], F32, 3)
            decp = Pool_(ps, nc, 'dec', [128, 8, 64], F32, 2)
            tmpp = Pool_(ps, nc, 'ktmp', [128, 8, 64], F32, 2)
            partp = Pool_(ps, nc, 'kpart', [128, 64], F32, 2)
            accp = Pool_(ps, nc, 'kacc', [128, 64], F32, 2)
            rnp = Pool_(ps, nc, 'rn', [64, 1], F32, 2)
            pkp = Pool_(ps, nc, 'pk', [128, 8, 64], F32, 2, psum=True)
            pep = Pool_(ps, nc, 'pe', [128, 8, 64], F32, 1, psum=True)
            pnp = Pool_(ps, nc, 'pn', [64, 1], F32, 1, psum=True)
            for g in range(16):
                c0 = g * 64
                KA, KAk = KAp.next()
                acc, acck = accp.next()
                for nb in range(16):
                    dr, drk = decRp.next()
                    P.dma('sp', dr[:], K['decR'][:, nb * 8:(nb + 1) * 8, c0:c0 + 64], W=[drk])
                    pk, pkk = pkp.next()
                    for i in range(8):
                        n2 = nb * 8 + i
                        P.mm(pk[:, i, :], h3[:, n2 * 128:(n2 + 1) * 128], wo[:, c0:c0 + 64], True, True, R=['h3', 'wo'], W=[pkk])
                    pe_, pek = pep.next()
                    P.mm(pe_[:].rearrange('p a b -> p (a b)'), decL[:], dr[:].rearrange('p a b -> p (a b)'), True, True, R=['decL', drk], W=[pek])
                    dec, deck = decp.next()
                    P.act(dec[:], pe_[:], AF.Exp, R=[pek], W=[deck])
                    tmp, tmpk = tmpp.next()
                    P.tt('dve', tmp[:], pk[:], dec[:], ALU.mult, R=[pkk, deck], W=[tmpk])
                    tv = tmp[:].rearrange("p n c -> p c n")
                    P.copy('pool', KA[:, :, nb * 8:(nb + 1) * 8], tv, R=[tmpk], W=[KAk])
                    if nb == 0:
                        P.op('dve', lambda e, o=acc[:], i_=tv: e.tensor_reduce(out=o, in_=i_, axis=AX.X, op=ALU.add, apply_absolute_value=True),
                             R=[tmpk], W=[acck])
                    else:
                        part, partk = partp.next()
                        P.op('dve', lambda e, o=part[:], i_=tv: e.tensor_reduce(out=o, in_=i_, axis=AX.X, op=ALU.add, apply_absolute_value=True),
                             R=[tmpk], W=[partk])
                        P.tt('pool', acc[:], acc[:], part[:], ALU.add, R=[acck, partk], W=[acck])
                pn, pnk = pnp.next()
                P.mm(pn[:], acc[:], ones[:], True, True, R=[acck, 'onesf'], W=[pnk])
                rn, rnk = rnp.next()
                P.recip(rn[:], pn[:], R=[pnk], W=[rnk])
                P.dma('sp', RNd[(g % 2) * 64:(g % 2) * 64 + 64, g // 2:g // 2 + 1], rn[:], R=[rnk], W=['RNd'], allow_slow_non_contiguous=True)
                fc.stage_a(KA, KAk, 128)
                KFs, KFk = KFp.next()

                def consume(xp, xpk, k10, nk, KFs=KFs, KFk=KFk):
                    P.act(KFs[:, :, k10:k10 + nk, :], xp[:, :, 0:nk, :], AF.Identity, R=[xpk], W=[KFk])
                fc.stage_c(consume)
                P.dma('sp', KFd[g], KFs[:], R=[KFk], W=['KFd'])
            P.flush()

    def phase_z():
        with ExitStack() as ps:
            sb = lambda n, s, d: ps.enter_context(nc.sbuf_tensor(n, s, d))
            fc = FFTConsts(ps, True)
            IR = sb('IR', [128, 2, 256], BF16)
            TWI = sb('TWI', [65, 2, 128], F32)
            G = sb('G', [65, 2, 64], BF16)
            P.dma('pool', IR[:], K['IR'], W=['IR'])
            P.dma('sp', TWI[:], K['TWI'], W=['TWI'])
            P.dma('pool', G[:], K['G'], W=['G'])
            ZAp = Pool_(ps, nc, 'ZA', [64, 64, 128], BF16, 2)
            KFp = Pool_(ps, nc, 'KF', [128, 2, 65, 64], BF16, 1)
            Yp = Pool_(ps, nc, 'Y', [128, 2, 64, 65], BF16, 1)
            t1p = Pool_(ps, nc, 'zt1', [128, 4, 64], F32, 2)
            t2p = Pool_(ps, nc, 'zt2', [128, 4, 64], F32, 2)
            crp = Pool_(ps, nc, 'craw', [65, 2, 2, 128], F32, 2)
            m1p = Pool_(ps, nc, 'm1', [65, 2, 2, 128], F32, 2)
            m2p = Pool_(ps, nc, 'm2', [65, 2, 2, 128], F32, 2)
            C2p = Pool_(ps, nc, 'C2', [65, 2, 16, 128], BF16, 2)
            ybp = Pool_(ps, nc, 'ybuf', [64, 16, 128], F32, 2)
            cpp = Pool_(ps, nc, 'cp', [65, 2, 2, 128], F32, 2, psum=True)
            ypp = Pool_(ps, nc, 'yp', [64, 4, 128], F32, 2, psum=True)
            twc = TWI[:, 0:1, :].unsqueeze(1).broadcast_to([65, 2, 2, 128]) if False else None
            for g in range(16):
                c0 = g * 64
                ZA, ZAk = ZAp.next()
                P.dma('sp', ZA[:], Zd[c0:c0 + 64, :].rearrange("c (a b) -> a c b", b=128), W=[ZAk])
                KF, KFk = KFp.next()
                P.dma('sp', KF[:], KFd[g], W=[KFk])
                fc.stage_a(ZA, ZAk, 64)
                Y, Yk = Yp.next()

                def consume(xp, xpk, k10, nk, KF=KF, KFk=KFk, Y=Y, Yk=Yk):
                    t1, t1k = t1p.next()
                    t2, t2k = t2p.next()
                    yo = lambda ri: Y[:, ri, :, k10:k10 + nk].rearrange("p c k -> p k c")
                    P.tt('dve', t1[:, 0:nk, :], xp[:, 0, 0:nk, :], KF[:, 0, k10:k10 + nk, :], ALU.mult, R=[xpk, KFk], W=[t1k])
                    P.tt('dve', t2[:, 0:nk, :], xp[:, 1, 0:nk, :], KF[:, 1, k10:k10 + nk, :], ALU.mult, R=[xpk, KFk], W=[t2k])
                    P.tt('pool', yo(0), t1[:, 0:nk, :], t2[:, 0:nk, :], ALU.subtract, R=[t1k, t2k], W=[Yk])
                    t1, t1k = t1p.next()
                    t2, t2k = t2p.next()
                    P.tt('dve', t1[:, 0:nk, :], xp[:, 0, 0:nk, :], KF[:, 1, k10:k10 + nk, :], ALU.mult, R=[xpk, KFk], W=[t1k])
                    P.tt('dve', t2[:, 0:nk, :], xp[:, 1, 0:nk, :], KF[:, 0, k10:k10 + nk, :], ALU.mult, R=[xpk, KFk], W=[t2k])
                    P.tt('pool', yo(1), t1[:, 0:nk, :], t2[:, 0:nk, :], ALU.add, R=[t1k, t2k], W=[Yk])
                fc.stage_c(consume)
                for cs in range(4):
                    C2, C2k = C2p.next()
                    yb, ybk = ybp.next()
                    for cb in range(8):
                        cp, cpk = cpp.next()
                        for i in range(2):
                            c = cs * 16 + cb * 2 + i
                            o = cp[:, i, :, :].rearrange("p a b -> p (a b)")
                            P.mm(o, Y[:, 0, c, :], IR[:, 0, :], True, False, R=[Yk, 'IR'], W=[cpk])
                            P.mm(o, Y[:, 1, c, :], IR[:, 1, :], False, True, R=[Yk, 'IR'], W=[cpk])
                        cr, crk = crp.next()
                        P.copy('act', cr[:], cp[:], R=[cpk], W=[crk])
                        m1, m1k = m1p.next()
                        m2, m2k = m2p.next()
                        for i in range(2):
                            for r in range(2):
                                P.tt('dve' if r == 0 else 'pool', m1[:, i, r, :], cr[:, i, r, :], TWI[:, 0, :], ALU.mult, R=[crk, 'TWI'], W=[(m1k, i, r)])
                                P.tt('dve' if r == 1 else 'pool', m2[:, i, r, :], cr[:, i, r, :], TWI[:, 1, :], ALU.mult, R=[crk, 'TWI'], W=[(m2k, i, r)])
                        cc0 = cb * 2
                        P.tt('pool', C2[:, 0, cc0:cc0 + 2, :], m1[:, :, 0, :], m2[:, :, 1, :], ALU.subtract,
                             R=[(m1k, 0, 0), (m1k, 1, 0), (m2k, 0, 1), (m2k, 1, 1)], W=[(C2k, cb // 2)])
                        P.tt('dve', C2[:, 1, cc0:cc0 + 2, :], m2[:, :, 0, :], m1[:, :, 1, :], ALU.add,
                             R=[(m2k, 0, 0), (m2k, 1, 0), (m1k, 0, 1), (m1k, 1, 1)], W=[(C2k, cb // 2)])
                        if cb % 2 == 1:
                            q = cb // 2
                            yp, ypk = ypp.next()
                            o = yp[:].rearrange("p a b -> p (a b)")
                            P.mm(o, G[:, 0, :], C2[:, 0, q * 4:q * 4 + 4, :].rearrange("p a b -> p (a b)"), True, False, R=['G', (C2k, q)], W=[ypk])
                            P.mm(o, G[:, 1, :], C2[:, 1, q * 4:q * 4 + 4, :].rearrange("p a b -> p (a b)"), False, True, R=['G', (C2k, q)], W=[ypk])
                            P.copy('act', yb[:, q * 4:q * 4 + 4, :], yp[:], R=[ypk], W=[ybk])
                    cA = c0 + cs * 16
                    P.dma('sp', Ytd[cA:cA + 16, :].rearrange("c (a b) -> a c b", b=128), yb[:], R=[ybk], W=['Ytd'])
            P.flush()

    def load_row(sb_tile, key, src1d, q='sp'):
        P.dma(q, sb_tile, src1d.partition_broadcast(128), W=[key])

    def phase_h5(ysrc, zsrc, x0src, xin, xout, Tn, m, rnsrc):
        TT = 512 if Tn >= 512 else Tn
        nb = TT // 128
        with ExitStack() as ps:
            sb = lambda n, s, d: ps.enter_context(nc.sbuf_tensor(n, s, d))
            wout = sb('wout', [128, 8, D], BF16)
            P.dma('pool', wout[:], W['hy_w_out'].rearrange("(k p) n -> p k n", p=128), W=['wout'])
            rn = sb('rncol', [128, 8], F32)
            bias = sb('hbias', [128, 8], F32)
            P.dma('sp', rn[:], rnsrc, W=['rncol'])
            P.dma('sp', bias[:], W['hy_bias_col'], W=['hbias'])
            brow = sb('brow', [128, D], F32)
            grow = sb('grow', [128, D], F32)
            load_row(brow[:], 'brow', W['hy_b_out'])
            load_row(grow[:], 'grow', MODROW[0, m, 2048:3072])
            ytp = Pool_(ps, nc, 'yt', [128, 8, TT], F32, 2)
            ztp = Pool_(ps, nc, 'zt5', [128, 8, TT], BF16, 2)
            x0p = Pool_(ps, nc, 'x05', [128, 8, TT], BF16, 2)
            a1p = Pool_(ps, nc, 'a1', [128, TT], F32, 2)
            gtp = Pool_(ps, nc, 'gt', [128, 8, TT], BF16, 2)
            xtp = Pool_(ps, nc, 'xt5', [128, nb, D], F32, 2)
            tmp = Pool_(ps, nc, 'tmp5', [128, 512], F32, 3)
            pop = Pool_(ps, nc, 'po', [128, 512], F32, 4, psum=True)
            for ti in range(Tn // TT):
                t0 = ti * TT
                yt, ytk = ytp.next()
                zt, ztk = ztp.next()
                x0, x0k = x0p.next()
                xt, xtk = xtp.next()
                P.dma('sp', yt[:], ysrc[:, t0:t0 + TT].rearrange("(k p) t -> p k t", p=128), W=[ytk])
                P.dma('sp', zt[:], zsrc[:, t0:t0 + TT].rearrange("(k p) t -> p k t", p=128), W=[ztk])
                P.dma('sp', x0[:], x0src[:, t0:t0 + TT].rearrange("(k p) t -> p k t", p=128), W=[x0k])
                P.dma('sp', xt[:], xin[t0:t0 + TT, :].rearrange("(j p) d -> p j d", p=128), W=[xtk])
                gt, gtk = gtp.next()
                for k in range(8):
                    a1, a1k = a1p.next()
                    P.ts('pool', a1[:], zt[:, k, :], bias[:, k:k + 1], None, ALU.mult, None, R=[ztk, 'hbias'], W=[a1k])
                    P.stt(a1[:], yt[:, k, :], rn[:, k:k + 1], a1[:], ALU.mult, ALU.add, R=[ytk, 'rncol', a1k], W=[a1k])
                    P.tt('pool', gt[:, k, :], a1[:], x0[:, k, :], ALU.mult, R=[a1k, x0k], W=[(gtk, k)])
                for tb in range(nb):
                    for dh in range(2):
                        po, pok = pop.next()
                        for k in range(8):
                            P.mm(po[:], gt[:, k, tb * 128:(tb + 1) * 128], wout[:, k, dh * 512:(dh + 1) * 512], k == 0, k == 7,
                                 R=[(gtk, k), 'wout'], W=[pok])
                        tm, tmk = tmp.next()
                        dsl = slice(dh * 512, (dh + 1) * 512)
                        P.tt('dve', tm[:], po[:], brow[:, dsl], ALU.add, R=[pok, 'brow'], W=[tmk])
                        P.tt('pool', tm[:], tm[:], grow[:, dsl], ALU.mult, R=[tmk, 'grow'], W=[tmk])
                        P.tt('pool', xt[:, tb, dsl], tm[:], xt[:, tb, dsl], ALU.add, R=[tmk, xtk], W=[xtk])
                P.dma('sp', xout[t0:t0 + TT, :].rearrange("(j p) d -> p j d", p=128), xt[:], R=[xtk], W=['xout'])
            P.flush()

    def phase_ffn(l, xin, xout, Tn, m, final=False):
        lat = Tn == T
        halo = 64 if lat else 0
        CEN = 256
        NTOK = CEN + 2 * halo
        nblk = NTOK // 128
        gc = 64 if lat else 256
        R_ = CEN // gc
        RH = NTOK // gc
        ntile = Tn // CEN
        with ExitStack() as ps:
            sb = lambda n, s, d: ps.enter_context(nc.sbuf_tensor(n, s, d))
            wup = sb('wup', [128, 8, 2 * FH], BF16)
            wdn = sb('wdn', [128, NFC, D], BF16)
            for k in range(8):
                P.dma('pool', wup[:, k, :], W['ffn_w_up'][l, k * 128:(k + 1) * 128, :], W=['wup'])
            for fcc in range(NFC):
                P.dma('pool', wdn[:, fcc, :], W['ffn_w_down'][l, fcc * 128:(fcc + 1) * 128, :], W=['wdn'])
            cw = sb('cw', [128, 2, 9, NFC], F32)
            cb = sb('cb', [128, 2, NFC], F32)
            P.dma('sp', cw[:], W['ffn_conv_w_col'], W=['cw'])
            P.dma('sp', cb[:], W['ffn_conv_b_col'], W=['cw'])
            grow = sb('grow2', [128, D], F32)
            load_row(grow[:], 'grow2', MODROW[l, m, 5120:6144])
            if final:
                fgrow = sb('fgrow', [128, D], F32)
                load_row(fgrow[:], 'fgrow', W['final_g'][0])
                fss = Pool_(ps, nc, 'fss', [128, 2], F32, 2)
            fe = FrontEnd(ps, nblk)
            xhp = Pool_(ps, nc, 'xh', [128, nblk, D], BF16, 1)
            xcp = Pool_(ps, nc, 'xc', [128, 2, D], F32, 2)
            uTp = Pool_(ps, nc, 'uT', [128, 8, NTOK], BF16, 2)
            h2p = Pool_(ps, nc, 'h2', [128, NFC, CEN], BF16, 1)
            gsp = Pool_(ps, nc, 'gs', [128, NTOK], F32, 2)
            acp = Pool_(ps, nc, 'cacc', [128, CEN], F32, 2)
            sgp = Pool_(ps, nc, 'sg', [128, CEN], F32, 2)
            tmp = Pool_(ps, nc, 'tmpf', [128, 512], F32, 1 if final else 2)
            pap = Pool_(ps, nc, 'pa_f', [128, CEN], F32, 2, psum=True)
            pgp = Pool_(ps, nc, 'pg_f', [128, NTOK], F32, 2, psum=True)
            pop = Pool_(ps, nc, 'po_f', [128, 512], F32, 2, psum=True)
            taps = [(kr, kc) for kr in range(3) for kc in range(3)] if lat else [(1, 0), (1, 1), (1, 2)]
            for ti in range(ntile):
                t0 = ti * CEN
                xh, xhk = xhp.next()
                xc, xck = xcp.next()
                ts_ = t0 - halo
                if lat and ti == 0:
                    P.memset('pool', xh[:, 0, :], 0.0, W=[xhk])
                    P.dma('pool', xh[64:128, 0, :], xin[0:64, :], W=[xhk])
                    P.dma('pool', xh[:, 1:3, :], xin[64:320, :].rearrange("(j p) d -> p j d", p=128), W=[xhk])
                elif lat and ti == ntile - 1:
                    P.memset('pool', xh[:, 2, :], 0.0, W=[xhk])
                    P.dma('pool', xh[0:64, 2, :], xin[Tn - 64:Tn, :], W=[xhk])
                    P.dma('pool', xh[:, 0:2, :], xin[ts_:ts_ + 256, :].rearrange("(j p) d -> p j d", p=128), W=[xhk])
                else:
                    P.dma('pool', xh[:], xin[ts_:ts_ + NTOK, :].rearrange("(j p) d -> p j d", p=128), W=[xhk])
                P.dma('sp', xc[:], xin[t0:t0 + CEN, :].rearrange("(j p) d -> p j d", p=128), W=[xck])
                uT, uTk = uTp.next()
                fe.run(xh, xhk, uT, uTk, l, 1, m)
                h2, h2k = h2p.next()
                for fcc in range(NFC):
                    pa, pak = pap.next()
                    for k in range(8):
                        P.mm(pa[:], wup[:, k, fcc * 128:(fcc + 1) * 128], uT[:, k, halo:halo + CEN], k == 0, k == 7, R=['wup', uTk], W=[pak])
                    pg, pgk = pgp.next()
                    for k in range(8):
                        P.mm(pg[:], wup[:, k, FH + fcc * 128:FH + (fcc + 1) * 128], uT[:, k, :], k == 0, k == 7, R=['wup', uTk], W=[pgk])
                    gs, gsk = gsp.next()
                    P.copy('act', gs[:], pg[:], R=[pgk], W=[gsk])
                    if lat and ti == 0:
                        P.memset('pool', gs[:, 0:64], 0.0, W=[gsk])
                    if lat and ti == ntile - 1:
                        P.memset('pool', gs[:, NTOK - 64:NTOK], 0.0, W=[gsk])
                    gv = gs[:].rearrange("p (r c) -> p r c", c=gc)
                    acc, acck = acp.next()
                    av = acc[:].rearrange("p (r c) -> p r c", c=gc)
                    r0 = 1 if lat else 0
                    P.ts('dve', av, gv[:, r0:r0 + R_, :], cw[:, l, 4, fcc:fcc + 1], cb[:, l, fcc:fcc + 1], ALU.mult, ALU.add,
                         R=[gsk, 'cw'], W=[acck])
                    for (kr, kc) in taps:
                        if (kr, kc) == (1, 1):
                            continue
                        rr = r0 + kr - 1
                        clo = 1 if kc == 0 else 0
                        chi = gc - 1 if kc == 2 else gc
                        P.stt(av[:, :, clo:chi], gv[:, rr:rr + R_, clo + kc - 1:chi + kc - 1], cw[:, l, kr * 3 + kc, fcc:fcc + 1],
                              av[:, :, clo:chi], ALU.mult, ALU.add, R=[gsk, 'cw', acck], W=[acck])
                    sg, sgk = sgp.next()
                    P.act(sg[:], acc[:], AF.Silu, R=[acck], W=[sgk])
                    P.tt('dve', h2[:, fcc, :], sg[:], pa[:], ALU.mult, R=[sgk, pak], W=[(h2k, fcc)])
                if final:
                    ss, ssk = fss.next()
                for tb in range(2):
                    for dh in range(2):
                        po, pok = pop.next()
                        for fcc in range(NFC):
                            P.mm(po[:], h2[:, fcc, tb * 128:(tb + 1) * 128], wdn[:, fcc, dh * 512:(dh + 1) * 512], fcc == 0, fcc == NFC - 1,
                                 R=[(h2k, fcc), 'wdn'], W=[pok])
                        tm, tmk = tmp.next()
                        dsl = slice(dh * 512, (dh + 1) * 512)
                        P.tt('dve', tm[:], po[:], grow[:, dsl], ALU.mult, R=[pok, 'grow2'], W=[tmk])
                        P.tt('pool', xc[:, tb, dsl], tm[:], xc[:, tb, dsl], ALU.add, R=[tmk, xck], W=[xck])
                    if final:
                        P.act(fe.junk[:], xc[:, tb, :], AF.Square, R=[xck], W=['fe_junk', ssk], accum_out=ss[:, tb:tb + 1])
                if final:
                    P.ts('dve', ss[:], ss[:], 1.0 / D, EPS, ALU.mult, ALU.add, R=[ssk], W=[ssk])
                    P.act(ss[:], ss[:], AF.Sqrt, R=[ssk], W=[ssk])
                    P.recip(ss[:], ss[:], R=[ssk], W=[ssk])
                    for tb in range(2):
                        P.stt(xc[:, tb, :], xc[:, tb, :], ss[:, tb:tb + 1], fgrow[:], ALU.mult, ALU.mult, R=[xck, ssk, 'fgrow'], W=[xck])
                P.dma('sp', xout[t0:t0 + CEN, :].rearrange("(j p) d -> p j d", p=128), xc[:], R=[xck], W=['xout'])
            P.flush()

    def phase_kc(zsrc, ydst, rndst):
        with ExitStack() as ps:
            sb = lambda n, s, d: ps.enter_context(nc.sbuf_tensor(n, s, d))
            h3c = sb('h3c', [128, 512], F32)
            with ExitStack() as ps2:
                mlp = FilterMLP(ps2)
                mlp.run(K['posC'], 0, 512, h3c[:, 0:512], 'h3c')
                P.flush()
            wo = sb('wo_f', [128, 2, D], F32)
            P.memset('pool', wo[:], 0.0, W=['wo_f'])
            P.dma('sp', wo[0:64, 0, :], W['hy_wo_st'][0:64, :], W=['wo_f'])
            P.dma('sp', wo[64:128, 1, :], W['hy_wo_st'][64:128, :], W=['wo_f'])
            trow = sb('trow', [128, 511], F32)
            P.dma('sp', trow[:], K['tC'][0].partition_broadcast(128), W=['trow'])
            nd = sb('ndelta', [128, 8], F32)
            P.dma('sp', nd[:], K['ndelta'], W=['ndelta'])
            zcp = Pool_(ps, nc, 'zc', [128, TC], F32, 2)
            zbp = Pool_(ps, nc, 'zb', [128, TC], BF16, 2)
            decp = Pool_(ps, nc, 'decc', [128, 511], F32, 2)
            KLp = Pool_(ps, nc, 'KL', [128, 511], F32, 2)
            accp = Pool_(ps, nc, 'accc', [128, TC], F32, 2)
            nrm = sb('nrmc', [128, 8], F32)
            pkc = Pool_(ps, nc, 'pkc', [128, 512], F32, 2, psum=True)
            for k in range(8):
                pk, pkk = pkc.next()
                P.mm(pk[:, 0:256], wo[:, 1, k * 128:(k + 1) * 128], h3c[:, 0:256], True, True, R=['wo_f', 'h3c'], W=[pkk])
                P.mm(pk[:, 255:511], wo[:, 0, k * 128:(k + 1) * 128], h3c[:, 255:511], True, True, R=['wo_f', 'h3c'], W=[pkk])
                dec, deck = decp.next()
                P.act(dec[:], trow[:], AF.Exp, R=['trow', 'ndelta'], W=[deck], scale=nd[:, k:k + 1])
                KL, KLk = KLp.next()
                P.tt('dve', KL[:], pk[:, 0:511], dec[:], ALU.mult, R=[pkk, deck], W=[KLk])
                P.op('dve', lambda e, o=nrm[:, k:k + 1], i_=KL[:]: e.tensor_reduce(out=o, in_=i_, axis=AX.X, op=ALU.add, apply_absolute_value=True),
                     R=[KLk], W=[('nrmc', k)])
                zb, zbk = zbp.next()
                P.dma('sp', zb[:], zsrc[k * 128:(k + 1) * 128, :], W=[zbk])
                zc, zck = zcp.next()
                P.copy('pool', zc[:], zb[:], R=[zbk], W=[zck])
                acc, acck = accp.next()
                P.ts('dve', acc[:], KL[:, 255:511], zc[:, 0:1], None, ALU.mult, None, R=[KLk, zck], W=[acck])
                for s_ in range(1, TC):
                    P.stt(acc[:], KL[:, 255 - s_:511 - s_], zc[:, s_:s_ + 1], acc[:], ALU.mult, ALU.add, R=[KLk, zck, acck], W=[acck])
                P.dma('sp', ydst[k * 128:(k + 1) * 128, :], acc[:], R=[acck], W=['ydst'])
            P.recip(nrm[:], nrm[:], R=[('nrmc', k) for k in range(8)], W=['nrmc'])
            P.dma('sp', rndst, nrm[:], R=['nrmc'], W=['rndst'])
            P.flush()

    def phase_m1(src, Tn, xmdst, szdst):
        lat = Tn == T
        nblk = 4 if lat else Tn // 128
        TT = nblk * 128
        noc = 32 if lat else 16
        with ExitStack() as ps:
            sb = lambda n, s, d: ps.enter_context(nc.sbuf_tensor(n, s, d))
            win = sb('mwin', [128, 8, 2 * MI], BF16)
            fe = FrontEnd(ps, nblk)
            xtp = Pool_(ps, nc, 'mxt', [128, nblk, D], F32, 2)
            hTp = Pool_(ps, nc, 'mhT', [128, 8, TT], BF16, 2)
            pbp = Pool_(ps, nc, 'mpb', [128, 8, TT], BF16, 2)
            pp = Pool_(ps, nc, 'mpp', [128, TT], F32, 4, psum=True)
            for k in range(8):
                P.dma('pool', win[:, k, :], W['ml_w_in'][k * 128:(k + 1) * 128, :], W=['mwin'])
            m = 0 if lat else 1
            for ti in range(Tn // TT):
                t0 = ti * TT
                xt, xtk = xtp.next()
                P.dma('sp', xt[:], src[t0:t0 + TT, :].rearrange("(j p) d -> p j d", p=128), W=[xtk])
                hT, hTk = hTp.next()
                fe.run(xt, xtk, hT, hTk, 1, 0, m)
                for og in range(noc // 8):
                    pb, pbk = pbp.next()
                    for oi in range(8):
                        oc = og * 8 + oi
                        pt, ptk = pp.next()
                        for k in range(8):
                            P.mm(pt[:], win[:, k, oc * 128:(oc + 1) * 128], hT[:, k, :], k == 0, k == 7, R=['mwin', hTk], W=[ptk])
                        if oc >= 16:
                            P.act(pb[:, oi, :], pt[:], AF.Silu, R=[ptk], W=[pbk])
                        elif oi % 2 == 0:
                            P.copy('act', pb[:, oi, :], pt[:], R=[ptk], W=[pbk])
                        else:
                            P.copy('dve', pb[:, oi, :], pt[:], R=[ptk], W=[pbk])
                    if og < 2:
                        P.dma('sp', xmdst[og * 1024:(og + 1) * 1024, t0:t0 + TT].rearrange("(c p) t -> p c t", p=128), pb[:], R=[pbk], W=['xmdst'])
                    else:
                        o2 = og - 2
                        P.dma('sp', szdst[o2 * 1024:(o2 + 1) * 1024, t0:t0 + TT].rearrange("(c p) t -> p c t", p=128), pb[:], R=[pbk], W=['szdst'])
            P.flush()

    def phase_m2(xmsrc, Tn, qdst, kdst, ktdst, vtdst, sxdst, gpdst):
        lat = Tn == T
        TT = 512 if lat else Tn
        nblk = TT // 128
        DHS = 512.0 ** -0.5
        with ExitStack() as ps:
            sb = lambda n, s, d: ps.enter_context(nc.sbuf_tensor(n, s, d))
            bd = sb('bd', [128, 3, 16, 128], BF16)
            for j, nm in enumerate(('ml_bdq', 'ml_bdk', 'ml_bdv')):
                P.dma('pool', bd[:, j, :, :], W[nm].rearrange("c p m -> p c m"), W=['bd'])
            wg = sb('wg', [128, 48, 16], BF16)
            P.dma('pool', wg[:], W['ml_w_gate'].rearrange("(c p) n -> p c n", p=128), W=['wg'])
            bgrow = sb('bgrow', [128, 16], F32)
            load_row(bgrow[:], 'bgrow', W['ml_b_gate'])
            cwc = sb('mcw', [128, 3, 16], F32)
            cbc = sb('mcb', [128, 16], F32)
            skc = sb('mskip', [128, 16], F32)
            P.dma('sp', cwc[:], W['ml_conv_w_col'], W=['mcw'])
            P.dma('sp', cbc[:], W['ml_conv_b_col'], W=['mcw'])
            P.dma('sp', skc[:], W['ml_skip_col'], W=['mcw'])
            Um = sb('Um', [128, 3, 128], F32)
            P.dma('sp', Um[:, 0, :], K['U'], W=['Um'])
            P.dma('sp', Um[:, 1, :], K['UT'], W=['Um'])
            P.dma('sp', Um[:, 2, :], K['ones'], W=['Um'])
            xmp = Pool_(ps, nc, 'xmh', [128, 16, TT + 2], BF16, 2)
            xcp = Pool_(ps, nc, 'xcm', [128, 16, TT], BF16, 1)
            sxp = Pool_(ps, nc, 'sxc', [128, 16, TT], BF16, 1)
            accp = Pool_(ps, nc, 'macc', [128, TT], F32, 2)
            qp = Pool_(ps, nc, 'qfm', [128, 16, TT], BF16, 1)
            kp = Pool_(ps, nc, 'kfm', [128, 16, TT], BF16, 1)
            vp = Pool_(ps, nc, 'vfm', [128, 16, TT], BF16, 1)
            ktp = Pool_(ps, nc, 'ktm', [128, nblk, MI], BF16, 1)
            vtp = Pool_(ps, nc, 'vtm', [128, nblk, MI], BF16, 1)
            gpp = Pool_(ps, nc, 'gp', [128, nblk, 4, 8], F32, 2)
            gtp = Pool_(ps, nc, 'gates', [128, 16], F32, 2)
            spp = Pool_(ps, nc, 'spl', [128, 2, 4], F32, 2)
            t8p = Pool_(ps, nc, 'tmp8', [128, 8], F32, 2)
            pfm = Pool_(ps, nc, 'pfm', [128, TT], F32, 2, psum=True)
            ptm = Pool_(ps, nc, 'ptm', [128, 512], F32, 2, psum=True)
            pgp = Pool_(ps, nc, 'pgate', [128, 16], F32, 2, psum=True)
            pbp = Pool_(ps, nc, 'pbcum', [128, 2, 8], F32, 2, psum=True)
            for ti in range(Tn // TT):
                t0 = ti * TT
                xm, xmk = xmp.next()
                lo = 1 if ti == 0 else 0
                hi = TT + 1 if ti == Tn // TT - 1 else TT + 2
                if ti == 0:
                    P.memset('pool', xm[:, :, 0:1], 0.0, W=[xmk])
                if ti == Tn // TT - 1:
                    P.memset('pool', xm[:, :, TT + 1:TT + 2], 0.0, W=[xmk])
                P.dma('sp', xm[:, :, lo:hi], xmsrc[:, t0 - 1 + lo:t0 - 1 + hi].rearrange("(c p) t -> p c t", p=128), W=[xmk])
                xc, xck = xcp.next()
                sx, sxk = sxp.next()
                for cc in range(16):
                    acc, acck = accp.next()
                    P.act(acc[:], xm[:, cc, 1:TT + 1], AF.Identity, R=[xmk, 'mcw'], W=[acck], scale=cwc[:, 1, cc:cc + 1], bias=cbc[:, cc:cc + 1])
                    P.stt(acc[:], xm[:, cc, 0:TT], cwc[:, 0, cc:cc + 1], acc[:], ALU.mult, ALU.add, R=[xmk, 'mcw', acck], W=[acck])
                    P.stt(acc[:], xm[:, cc, 2:TT + 2], cwc[:, 2, cc:cc + 1], acc[:], ALU.mult, ALU.add, R=[xmk, 'mcw', acck], W=[acck])
                    P.act(xc[:, cc, :], acc[:], AF.Silu, R=[acck], W=[(xck, cc)])
                    if lat:
                        P.ts('pool', sx[:, cc, :], xc[:, cc, :], skc[:, cc:cc + 1], None, ALU.mult, None, R=[(xck, cc), 'mcw'], W=[sxk])
                if lat:
                    P.dma('sp', sxdst[:, t0:t0 + TT].rearrange("(c p) t -> p c t", p=128), sx[:], R=[sxk], W=['sxdst'])
                qf, qfk = qp.next()
                kf, kfk = kp.next()
                vf, vfk = vp.next()
                for cc in range(16):
                    for j, (dst_, dk, srct, srck) in enumerate(((qf, qfk, xc[:, cc, :], (xck, cc)), (kf, kfk, xc[:, cc, :], (xck, cc)),
                                                             (vf, vfk, xm[:, cc, 1:TT + 1], xmk))):
                        pf, pfk = pfm.next()
                        P.mm(pf[:], bd[:, j, cc, :], srct, True, True, R=['bd', srck], W=[pfk])
                        P.copy('act' if (cc + j) % 2 == 0 else 'dve', dst_[:, cc, :], pf[:], R=[pfk], W=[(dk, cc)])
                if lat:
                    P.dma('sp', qdst[:, t0:t0 + TT].rearrange("(c p) t -> p c t", p=128), qf[:], R=[(qfk, c_) for c_ in range(16)], W=['qdst'])
                    P.dma('sp', kdst[:, t0:t0 + TT].rearrange("(c p) t -> p c t", p=128), kf[:], R=[(kfk, c_) for c_ in range(16)], W=['kdst'])
                kt, ktk = ktp.next()
                vt, vtk = vtp.next()
                for blk in range(nblk):
                    bsl = slice(blk * 128, (blk + 1) * 128)
                    for j, (dst_, dk, which) in enumerate(((kt, ktk, 1), (vt, vtk, 2))):
                        for c4 in range(4):
                            pt, ptk = ptm.next()
                            for i in range(4):
                                cc = c4 * 4 + i
                                lhs = xc[:, cc, bsl] if which == 1 else xm[:, cc, 1 + blk * 128:1 + (blk + 1) * 128]
                                P.mm(pt[:, i * 128:(i + 1) * 128], lhs, bd[:, which, cc, :], True, True,
                                     R=['bd', (xck, cc) if which == 1 else xmk], W=[ptk])
                            P.copy('act' if (c4 + j) % 2 == 0 else 'dve', dst_[:, blk, c4 * 512:(c4 + 1) * 512], pt[:], R=[ptk], W=[dk])
                P.dma('sp', ktdst[t0:t0 + TT, :].rearrange("(j p) n -> p j n", p=128), kt[:], R=[ktk], W=['ktdst'])
                P.dma('sp', vtdst[t0:t0 + TT, :].rearrange("(j p) n -> p j n", p=128), vt[:], R=[vtk], W=['vtdst'])
                gp, gpk = gpp.next()
                for blk in range(nblk):
                    bsl = slice(blk * 128, (blk + 1) * 128)
                    pg, pgk = pgp.next()
                    n_ = 0
                    for j, (src_, sk) in enumerate(((qf, qfk), (kf, kfk), (vf, vfk))):
                        for cc in range(16):
                            P.mm(pg[:], src_[:, cc, bsl], wg[:, j * 16 + cc, :], n_ == 0, n_ == 47, R=[(sk, cc), 'wg'], W=[pgk])
                            n_ += 1
                    gt, gtk = gtp.next()
                    P.tt('dve', gt[:], pg[:], bgrow[:], ALU.add, R=[pgk, 'bgrow'], W=[gtk])
                    gv = gt[:].rearrange("p (d g h) -> p d g h", d=2, g=2)
                    sp_, spk = spp.next()
                    P.act(sp_[:], gv[:, :, 1, :], AF.Exp, R=[gtk], W=[spk], scale=-1.0)
                    P.act(sp_[:], sp_[:], AF.Ln, R=[spk], W=[spk], bias=1.0)
                    pb, pbk = pbp.next()
                    P.mm(pb[:, 0, 0:4], Um[:, 0, :], sp_[:, 0, :], True, True, R=['Um', spk], W=[pbk])
                    P.mm(pb[:, 0, 4:8], Um[:, 1, :], sp_[:, 1, :], True, True, R=['Um', spk], W=[pbk])
                    P.mm(pb[:, 1, :], Um[:, 2, :], sp_[:].rearrange("p d h -> p (d h)"), True, True, R=['Um', spk], W=[pbk])
                    P.act(gp[:, blk, 0:2, :], pb[:], AF.Exp, R=[pbk], W=[gpk], scale=-1.0)
                    t8, t8k = t8p.next()
                    P.tt('dve', t8[:].rearrange("p (d h) -> p d h", d=2), gv[:, :, 0, :], pb[:, 0, :].rearrange("p (d h) -> p d h", d=2), ALU.add,
                         R=[gtk, pbk], W=[t8k])
                    P.act(gp[:, blk, 2, :], t8[:], AF.Exp, R=[t8k], W=[gpk], bias=float(math.log(DHS)))
                    P.tt('dve', gp[:, blk, 3, :], gp[:, blk, 2, :], gp[:, blk, 1, :], ALU.mult, R=[gpk], W=[gpk])
                P.dma('sp', gpdst[t0:t0 + TT, :, :].rearrange("(j p) a b -> p j a b", p=128), gp[:], R=[gpk], W=['gpdst'])
            P.flush()

    def phase_m3():
        with ExitStack() as ps:
            sb = lambda n, s, d: ps.enter_context(nc.sbuf_tensor(n, s, d))
            pdC = ps.enter_context(nc.psum_tensor('pdC', [128, 4, 512], F32))
            Ct = [sb(f'Ct{c}', [128, 4, 512], F32) for c in range(8)]
            Cb = [sb(f'Cb{c}', [128, 4, 512], BF16) for c in range(8)]
            nt = sb('nt', [128, 8, 4], F32)
            nb = sb('nb', [128, 8, 4, 2], BF16)
            maskf = sb('maskf', [128, 2, 128], F32)
            onesb = sb('onesb', [128, 2], BF16)
            P.dma('sp', maskf[:, 0, :], K['U'], W=['maskf'])
            P.dma('sp', maskf[:, 1, :], K['UT'], W=['maskf'])
            P.memset('dve', onesb[:], 1.0, W=['onesb'])
            for c in range(8):
                P.memset('pool', Ct[c][:], 0.0, W=[('Ct', c)])
                P.memset('dve', Cb[c][:], 0.0, W=[('Cb', c)])
            P.memset('dve', nt[:], 0.0, W=[('nt', c) for c in range(8)])
            P.memset('dve', nb[:], 0.0, W=[('nb', c) for c in range(8)])
            qTp = Pool_(ps, nc, 'qT', [128, 4, 128], BF16, 4)
            kTp = Pool_(ps, nc, 'kT', [128, 4, 128], BF16, 4)
            ktp = Pool_(ps, nc, 'ktm3', [128, 512], BF16, 4)
            vtp = Pool_(ps, nc, 'vtm3', [128, 512], BF16, 4)
            gpp = Pool_(ps, nc, 'gp3', [128, 4, 8], F32, 6)
            Stp = Pool_(ps, nc, 'St', [128, 128], BF16, 3)
            k2p = Pool_(ps, nc, 'k2', [128, 512], BF16, 3)
            hop = Pool_(ps, nc, 'hout', [128, 512], F32, 3)
            smp = Pool_(ps, nc, 'sm3', [128, 4], F32, 4)
            pmp = Pool_(ps, nc, 'pmisc', [128, 512], F32, 2, psum=True)
            pnp = Pool_(ps, nc, 'pnum', [128, 512], F32, 2, psum=True)
            gpcache = {}

            def part_a(h, dr, c, srcs, full):
                ktsrc, vtsrc, gpsrc = srcs
                t0 = c * 128
                col = dr * 4 + h
                key = (id(gpsrc), dr, c)
                if key not in gpcache:
                    gp, gpk = gpp.next()
                    P.dma('sp', gp[:], gpsrc[t0:t0 + 128, :, :], W=[gpk])
                    gpcache.clear()
                    gpcache[key] = (gp, gpk)
                    gpcache[('other', dr)] = None
                gp, gpk = gpcache[key]
                kt, ktk = ktp.next()
                vt, vtk = vtp.next()
                P.dma('sp', kt[:], ktsrc[t0:t0 + 128, h * 512:(h + 1) * 512], W=[ktk])
                P.dma('sp', vt[:], vtsrc[t0:t0 + 128, h * 512:(h + 1) * 512], W=[vtk])
                pm, pmk = pmp.next()
                st = dict(h=h, dr=dr, c=c, gp=gp, gpk=gpk, kt=kt, ktk=ktk, vt=vt, vtk=vtk, col=col, full=full, pm=pm, pmk=pmk)
                k2, k2k = k2p.next()
                P.ts('pool', k2[:], kt[:], gp[:, 3, col:col + 1], None, ALU.mult, None, R=[ktk, gpk], W=[k2k])
                st.update(k2=k2, k2k=k2k)
                if full:
                    qT, qTk = qTp.next()
                    kT, kTk = kTp.next()
                    P.dma('sp', qT[:], Qfm[h * 512:(h + 1) * 512, t0:t0 + 128].rearrange("(dc p) t -> p dc t", p=128), W=[qTk])
                    P.dma('sp', kT[:], Kfm[h * 512:(h + 1) * 512, t0:t0 + 128].rearrange("(dc p) t -> p dc t", p=128), W=[kTk])
                    pS, pSk = pm[:, 0:128], (pmk, 'S')
                    for dc in range(4):
                        P.mm(pS, kT[:, dc, :], qT[:, dc, :], dc == 0, dc == 3, R=[kTk, qTk], W=[pSk])
                    St, Stk = Stp.next()
                    P.stt(St[:], pS, gp[:, 2, col:col + 1], maskf[:, dr, :], ALU.mult, ALU.mult, R=[pSk, gpk, 'maskf'], W=[Stk])
                    st.update(qT=qT, qTk=qTk, St=St, Stk=Stk)
                return st

            def part_b(st):
                h, dr, c, col = st['h'], st['dr'], st['c'], st['col']
                ch = h * 2 + dr
                gp, gpk = st['gp'], st['gpk']
                t0 = c * 128
                if st['full']:
                    qT, qTk, St, Stk, vt, vtk = st['qT'], st['qTk'], st['St'], st['Stk'], st['vt'], st['vtk']
                    pn, pnk = pnp.next()
                    P.mm(pn[:], St[:], vt[:], True, False, R=[Stk, vtk], W=[pnk])
                    for dc in range(4):
                        P.mm(pn[:], qT[:, dc, :], Cb[ch][:, dc, :], False, dc == 3, R=[qTk, ('Cb', ch)], W=[pnk])
                    pd, pdk = st['pm'][:, 128:130], (st['pmk'], 'den')
                    P.mm(pd, St[:], onesb[:], True, False, R=[Stk, 'onesb'], W=[pdk])
                    for dc in range(4):
                        P.mm(pd, qT[:, dc, :], nb[:, ch, dc, :], False, dc == 3, R=[qTk, ('nb', ch)], W=[pdk])
                    sm, smk = smp.next()
                    P.act(sm[:, 0:1], st['pm'][:, 128:129], AF.Abs, R=[pdk, gpk], W=[smk], scale=gp[:, 0, col:col + 1])
                    P.ts('dve', sm[:, 0:1], sm[:, 0:1], 1.0, None, ALU.max, None, R=[smk], W=[smk])
                    P.recip(sm[:, 1:2], sm[:, 0:1], R=[smk], W=[smk])
                    P.tt('dve', sm[:, 2:3], sm[:, 1:2], gp[:, 0, col:col + 1], ALU.mult, R=[smk, gpk], W=[smk])
                    ho, hok = hop.next()
                    P.act(ho[:], pn[:], AF.Identity, R=[pnk, smk], W=[hok], scale=sm[:, 2:3])
                    P.dma('sp', HFB[dr, t0:t0 + 128, h * 512:(h + 1) * 512], ho[:], R=[hok], W=['HFB'])
                k2, k2k, vt, vtk = st['k2'], st['k2k'], st['vt'], st['vtk']
                for dc in range(4):
                    P.mm(pdC[:, dc, :], k2[:, dc * 128:(dc + 1) * 128], vt[:], True, True, R=[k2k, vtk], W=[('pdC', dc)])
                pdn, pdnk = st['pm'][:, 256:264].rearrange("p (a b) -> p a b", b=2), (st['pmk'], 'dn')
                for dc in range(4):
                    P.mm(pdn[:, dc, :], k2[:, dc * 128:(dc + 1) * 128], onesb[:], True, True, R=[k2k, 'onesb'], W=[pdnk])
                glc = gp[:, 1, col:col + 1]
                for dc in range(4):
                    P.stt(Ct[ch][:, dc, :], Ct[ch][:, dc, :], glc, pdC[:, dc, :], ALU.mult, ALU.add, R=[('Ct', ch), gpk, ('pdC', dc)], W=[('Ct', ch)])
                P.copy('act', Cb[ch][:], Ct[ch][:], R=[('Ct', ch)], W=[('Cb', ch)])
                P.stt(nt[:, ch, :], nt[:, ch, :], glc, pdn[:, :, 0], ALU.mult, ALU.add, R=[('nt', ch), gpk, pdnk], W=[('nt', ch)])
                P.copy('pool', nb[:, ch, :, 0], nt[:, ch, :], R=[('nt', ch)], W=[('nb', ch)])
                P.copy('pool', nb[:, ch, :, 1], nt[:, ch, :], R=[('nt', ch)], W=[('nb', ch)])

            sched = []
            for s_ in range(2):
                for h in range(4):
                    for dr in range(2):
                        sched.append((h, dr, s_ if dr == 0 else 1 - s_, (KtmC, VtmC, GPdC), False))
            for s_ in range(64):
                for h in range(4):
                    for dr in range(2):
                        sched.append((h, dr, s_ if dr == 0 else 63 - s_, (Ktm, Vtm, GPd), True))
            gp_tiles = {}

            def get_gp(gpsrc, c):
                key = (id(gpsrc), c)
                if key not in gp_tiles:
                    if len(gp_tiles) >= 4:
                        gp_tiles.pop(next(iter(gp_tiles)))
                    gp, gpk = gpp.next()
                    P.dma('sp', gp[:], gpsrc[c * 128:(c + 1) * 128, :, :], W=[gpk])
                    gp_tiles[key] = (gp, gpk)
                return gp_tiles[key]

            def part_a2(h, dr, c, srcs, full):
                gp, gpk = get_gp(srcs[2], c)
                gpcache.clear()
                gpcache[(id(srcs[2]), dr, c)] = (gp, gpk)
                return part_a(h, dr, c, srcs, full)

            prev = None
            for item in sched:
                cur = part_a2(*item)
                if prev is not None:
                    part_b(prev)
                prev = cur
            part_b(prev)
            P.flush()

    def phase_m4(xin, xout):
        with ExitStack() as ps:
            sb = lambda n, s, d: ps.enter_context(nc.sbuf_tensor(n, s, d))
            wdn = sb('mwdn', [128, 16, D], BF16)
            P.dma('pool', wdn[:], W['ml_w_down'].rearrange("(c p) n -> p c n", p=128), W=['mwdn'])
            nwc = sb('nwc', [128, 16], F32)
            P.dma('sp', nwc[:], W['ml_norm_w_col'], W=['nwc'])
            grow = sb('grow4', [128, D], F32)
            load_row(grow[:], 'grow4', MODROW[1, 0, 2048:3072])
            hfp = Pool_(ps, nc, 'hf', [128, MI], F32, 2)
            hbp = Pool_(ps, nc, 'hb', [128, MI], F32, 2)
            hnp = Pool_(ps, nc, 'hn', [128, MI], BF16, 2)
            stp = Pool_(ps, nc, 'bst', [128, 4, 6], F32, 2)
            mvp = Pool_(ps, nc, 'bmv', [128, 4, 2], F32, 2)
            sxp = Pool_(ps, nc, 'sx4', [128, 16, 128], BF16, 2)
            szp = Pool_(ps, nc, 'sz4', [128, 16, 128], BF16, 2)
            m1p = Pool_(ps, nc, 'm14', [128, 128], F32, 3)
            mfp = Pool_(ps, nc, 'mfm', [128, 16, 128], BF16, 2)
            xtp = Pool_(ps, nc, 'xt4', [128, D], F32, 2)
            tmp = Pool_(ps, nc, 'tmp4', [128, 512], F32, 2)
            pTp = Pool_(ps, nc, 'pT4', [128, 4, 128], BF16, 2, psum=True)
            pop = Pool_(ps, nc, 'po4', [128, 512], F32, 2, psum=True)
            for blk in range(T // 128):
                t0 = blk * 128
                hf, hfk = hfp.next()
                hb, hbk = hbp.next()
                sx, sxk = sxp.next()
                sz, szk = szp.next()
                xt, xtk = xtp.next()
                P.dma('sp', hf[:], HFB[0, t0:t0 + 128, :], W=[hfk])
                P.dma('sp', hb[:], HFB[1, t0:t0 + 128, :], W=[hbk])
                P.dma('sp', sx[:], SXC[:, t0:t0 + 128].rearrange("(c p) t -> p c t", p=128), W=[sxk])
                P.dma('sp', sz[:], SZd[:, t0:t0 + 128].rearrange("(c p) t -> p c t", p=128), W=[szk])
                P.dma('sp', xt[:], xin[t0:t0 + 128, :], W=[xtk])
                P.tt('pool', hf[:], hf[:], hb[:], ALU.add, R=[hfk, hbk], W=[hfk])
                bst, bstk = stp.next()
                mv, mvk = mvp.next()
                for h in range(4):
                    P.op('dve', lambda e, o=bst[:, h, :], i_=hf[:, h * 512:(h + 1) * 512]: e.bn_stats(out=o, in_=i_), R=[hfk], W=[(bstk, h)])
                    P.op('dve', lambda e, o=mv[:, h, :], i_=bst[:, h, :]: e.bn_aggr(out=o, in_=i_), R=[(bstk, h)], W=[(mvk, h)])
                mvall = [(mvk, h) for h in range(4)]
                P.ts('dve', mv[:, :, 1], mv[:, :, 1], 1e-5, None, ALU.add, None, R=mvall, W=mvall)
                P.act(mv[:, :, 1], mv[:, :, 1], AF.Sqrt, R=mvall, W=mvall)
                P.recip(mv[:, :, 1], mv[:, :, 1], R=mvall, W=mvall)
                hn, hnk = hnp.next()
                for h in range(4):
                    P.ts('dve', hn[:, h * 512:(h + 1) * 512], hf[:, h * 512:(h + 1) * 512], mv[:, h, 0:1], mv[:, h, 1:2], ALU.subtract, ALU.mult,
                         R=[hfk] + mvall, W=[(hnk, h)])
                mf, mfk = mfp.next()
                for c4 in range(4):
                    pT, pTk = pTp.next()
                    for i in range(4):
                        cc = c4 * 4 + i
                        P.tr(pT[:, i, :], hn[:, cc * 128:(cc + 1) * 128], identb[:], R=[(hnk, cc // 4), 'identb'], W=[pTk])
                    for i in range(4):
                        cc = c4 * 4 + i
                        m1, m1k = m1p.next()
                        P.stt(m1[:], pT[:, i, :], nwc[:, cc:cc + 1], sx[:, cc, :], ALU.mult, ALU.add, R=[pTk, 'nwc', sxk], W=[m1k])
                        P.tt('pool', mf[:, cc, :], m1[:], sz[:, cc, :], ALU.mult, R=[m1k, szk], W=[(mfk, cc)])
                for dh in range(2):
                    po, pok = pop.next()
                    for cc in range(16):
                        P.mm(po[:], mf[:, cc, :], wdn[:, cc, dh * 512:(dh + 1) * 512], cc == 0, cc == 15, R=[(mfk, cc), 'mwdn'], W=[pok])
                    tm, tmk = tmp.next()
                    dsl = slice(dh * 512, (dh + 1) * 512)
                    P.tt('dve', tm[:], po[:], grow[:, dsl], ALU.mult, R=[pok, 'grow4'], W=[tmk])
                    P.tt('pool', xt[:, dsl], tm[:], xt[:, dsl], ALU.add, R=[tmk, xtk], W=[xtk])
                P.dma('sp', xout[t0:t0 + 128, :], xt[:], R=[xtk], W=['xout'])
            P.flush()
    only = stop_after
    run = lambda name: (only is None) or (name in only)
    phase_adaln()
    if run('lat0'):
        phase_h1(x_d, T, P0)
        phase_h2(P0, T, Zd, X0d)
        phase_k()
        phase_z()
        phase_h5(Ytd, Zd, X0d, x_d, XA0, T, 0, RNd)
        phase_ffn(0, XA0, XB0, T, 0)
    if run('ctx0') or run('c1'):
        phase_h1(ctx_d, TC, P0C)
    if run('ctx0') or run('c2'):
        phase_h2(P0C, TC, ZdC, X0dC)
    if run('ctx0') or run('c3'):
        phase_kc(ZdC, YtC, RNdC)
    if run('ctx0') or run('c4'):
        phase_h5(YtC, ZdC, X0dC, ctx_d, CA0, TC, 1, RNdC)
    if run('ctx0') or run('c5'):
        phase_ffn(0, CA0, CB0, TC, 1)
    if run('m1'):
        phase_m1(XB0, T, XMd, SZd)
        phase_m1(CB0, TC, XMC, None)
    if run('m2'):
        phase_m2(XMd, T, Qfm, Kfm, Ktm, Vtm, SXC, GPd)
        phase_m2(XMC, TC, None, None, KtmC, VtmC, None, GPdC)
    if run('m3'):
        phase_m3()
    if run('m4'):
        phase_m4(XB0, XA1)
    if run('f1'):
        phase_ffn(1, XA1, out_d, T, 0, final=True)
    return nc_real, cx


def _blockdiag(w):
    out = np.zeros((16, 128, 128), dtype=np.float32)
    for ch in range(16):
        for n in range(32):
            out[ch, 4 * n:4 * n + 4, 4 * n:4 * n + 4] = w[32 * ch + n]
    return out


def prep_shared(inputs):
    f = lambda k: np.asarray(inputs[k], dtype=np.float32)
    S = {}
    for k in ('mod_w', 'mod_b', 'norm_g', 'ffn_w_up', 'ffn_conv_w', 'ffn_conv_b', 'ffn_w_down'):
        S[k] = f(k)
    S['final_g'] = f('final_g').reshape(1, D)
    for k in ('hy_w_in', 'hy_b_in', 'hy_sc_w', 'hy_sc_b', 'hy_bias', 'hy_w_out', 'hy_b_out', 'ml_w_in', 'ml_conv_w', 'ml_conv_b',
              'ml_w_gate', 'ml_b_gate', 'ml_norm_w', 'ml_skip', 'ml_w_down'):
        S[k] = f(k)[0]
    w1, w2, w3 = f('hy_f_w1')[0], f('hy_f_w2')[0], f('hy_f_w3')[0]
    S['hy_w1d'] = np.concatenate([w1, w1], axis=1)
    z = np.zeros((64, 64), np.float32)
    S['hy_w2bd'] = np.block([[w2, z], [z, w2]])
    S['hy_w3bd'] = np.block([[w3, z], [z, w3]])
    dup = lambda v: np.concatenate([v, v]).reshape(128, 1)
    S['hy_b1d'] = dup(f('hy_f_b1')[0])
    S['hy_b2d'] = dup(f('hy_f_b2')[0])
    S['hy_b3d'] = dup(f('hy_f_b3')[0])
    S['hy_freqd'] = dup(f('hy_freq')[0])
    wo = f('hy_f_wout')[0]
    S['hy_wo_st'] = np.concatenate([wo[:, :D], wo[:, D:]], axis=0)
    def col(v):
        v = np.asarray(v, dtype=np.float32)
        n = v.shape[-1] // 128
        v = v.reshape(v.shape[:-1] + (n, 128))
        return np.moveaxis(v, -1, 0)
    S['mod_b_col'] = col(S['mod_b'])
    S['norm_g_col'] = col(S['norm_g'])
    S['hy_b_in_col'] = col(S['hy_b_in'])
    S['hy_sc_w_col'] = col(S['hy_sc_w'])
    S['hy_sc_b_col'] = col(S['hy_sc_b'])
    S['hy_bias_col'] = col(S['hy_bias'])
    S['ml_conv_w_col'] = col(S['ml_conv_w'])
    S['ml_conv_b_col'] = col(S['ml_conv_b'])
    S['ml_norm_w_col'] = col(S['ml_norm_w'])
    S['ml_skip_col'] = col(S['ml_skip'])
    S['ffn_conv_w_col'] = col(S['ffn_conv_w'].reshape(2, 9, FH))
    S['ffn_conv_b_col'] = col(S['ffn_conv_b'])
    S['ml_bdq'] = _blockdiag(f('ml_wq')[0])
    S['ml_bdk'] = _blockdiag(f('ml_wk')[0])
    S['ml_bdv'] = _blockdiag(f('ml_wv')[0])
    return {k: np.ascontiguousarray(v, dtype=np.float32) for k, v in S.items()}


def prep_core(inputs, S, b):
    d = dict(S)
    d['x'] = np.ascontiguousarray(inputs['x'][b], dtype=np.float32)
    d['ctx'] = np.ascontiguousarray(inputs['ctx'][b], dtype=np.float32)
    d['cvec'] = np.ascontiguousarray(np.stack([inputs['c'][b], inputs['c_ctx']], axis=1), dtype=np.float32)
    return d


def kernel(**inputs):
    S = prep_shared(inputs)
    cores = [prep_core(inputs, S, b % 4) for b in range(8)]
    nc, cx = build(cores[0])
    HC = host_consts()
    in_maps = []
    for c in cores:
        m = dict(c)
        for k, v in HC.items():
            m['c_' + k] = v
        in_maps.append(m)
    res = run_bass_kernel_spmd(nc, in_maps, core_ids=list(range(8)))
    out = np.stack([np.asarray(res.results[b]['out'], dtype=np.float32) for b in range(4)], axis=0)
    return out
```

```python
import math
import numpy as np
import concourse.bass as bass
import concourse.mybir as mybir
from concourse.bass_utils import run_bass_kernel_spmd
from contextlib import ExitStack

F32 = mybir.dt.float32
BF16 = mybir.dt.bfloat16
ALU = mybir.AluOpType
AF = mybir.ActivationFunctionType
AX = mybir.AxisListType

CENG = ('pe', 'dve', 'act', 'pool')
DQ = ('sp', 'pool', 'act')

T = 8192
D = 1024
TC = 256
FH = 2816
NFC = 22
MI = 2048
EPS = 1e-6
MAGIC = 12582912.0
TWO_PI = 2.0 * math.pi


class Prog:
    NDS = 6
    SAME_ENGINE_RELAX = True
    LONG = 200

    def __init__(self, nc, es):
        self.toklen = {}
        self.nc = nc
        self.eng = dict(pe=nc.tensor, dve=nc.vector, act=nc.scalar, pool=nc.gpsimd, sp=nc.sync)
        self.streams = {e: [] for e in self.eng}
        self.ninstr = {e: 0 for e in self.eng}
        self.csem = {e: es.enter_context(nc.semaphore(f"c{e}")) for e in CENG}
        self.ccount = {e: 0 for e in CENG}
        self.dsem = {q: [es.enter_context(nc.semaphore(f"d{q}_{i}")) for i in range(self.NDS)] for q in DQ}
        self.dcount = {q: 0 for q in DQ}
        self.seen = {e: {} for e in self.eng}
        self.lastw = {}
        self.readers = {}

    def _deps(self, R, W):
        deps = []
        for k in R:
            t = self.lastw.get(k)
            if t is not None:
                deps.append((t, 'raw'))
        for k in W:
            t = self.lastw.get(k)
            if t is not None:
                deps.append((t, 'waw'))
            deps.extend((r, 'war') for r in self.readers.get(k, ()))
        return deps

    def _emit_waits(self, e, deps, skip_pe_pe=False):
        seen = self.seen[e]
        need = {}
        for t, kind in deps:
            if t[0] == 'c':
                _, pe_, n = t
                if pe_ == e:
                    if skip_pe_pe and e == 'pe':
                        continue
                    if self.SAME_ENGINE_RELAX and (kind != 'raw' or self.toklen.get((pe_, n), 0) >= self.LONG):
                        continue
                key = ('c', pe_)
                val = n
            else:
                _, q, slot, v = t
                key = ('d', q, slot)
                val = v
            if seen.get(key, 0) >= val:
                continue
            if need.get(key, 0) < val:
                need[key] = val
        for key, val in need.items():
            seen[key] = val
            if key[0] == 'c':
                sem = self.csem[key[1]]
                self.streams[e].append(lambda eng, sem=sem, val=val: eng.wait_ge(sem, val))
            else:
                sem = self.dsem[key[1]][key[2]]
                self.streams[e].append(lambda eng, sem=sem, val=val: eng.wait_ge(sem, 16 * val))

    def _record(self, tok, R, W):
        for k in W:
            self.lastw[k] = tok
            self.readers[k] = []
        for k in R:
            self.readers.setdefault(k, []).append(tok)

    def op(self, e, fn, R=(), W=(), ln=0):
        deps = self._deps(R, W)
        self._emit_waits(e, deps, skip_pe_pe=True)
        self.ccount[e] += 1
        n = self.ccount[e]
        self.toklen[(e, n)] = ln
        sem = self.csem[e]
        self.streams[e].append(lambda eng, fn=fn, sem=sem: fn(eng).then_inc(sem, 1))
        self._record(('c', e, n), R, W)
        self.ninstr[e] += 1

    def dma(self, q, out, in_, R=(), W=(), **kw):
        deps = self._deps(R, W)
        self._emit_waits(q, deps)
        i = self.dcount[q]
        self.dcount[q] += 1
        slot = i % self.NDS
        v = i // self.NDS + 1
        sem = self.dsem[q][slot]
        self.streams[q].append(lambda eng, out=out, in_=in_, sem=sem, kw=kw: eng.dma_start(out=out, in_=in_, **kw).then_inc(sem, 16))
        self._record(('d', q, slot, v), R, W)
        self.ninstr[q] += 1

    def uq(self, name):
        self._uq = getattr(self, '_uq', 0) + 1
        return (name, 'u', self._uq)

    def barrier(self):
        toks = []
        for e in CENG:
            if self.ccount[e] > 0:
                toks.append(('c', e, self.ccount[e]))
        for q in DQ:
            n = self.dcount[q]
            for slot in range(self.NDS):
                if n > slot:
                    toks.append(('d', q, slot, (n - 1 - slot) // self.NDS + 1))
        for e in self.eng:
            self._emit_waits(e, [(t, 'bar') for t in toks])
        self.lastw = {}
        self.readers = {}

    def flush(self):
        self.barrier()
        nc = self.nc
        st = self.streams
        with nc.Block() as block:
            @block.tensor
            def _(eng):
                for f in st['pe']:
                    f(eng)

            @block.vector
            def _(eng):
                for f in st['dve']:
                    f(eng)

            @block.scalar
            def _(eng):
                for f in st['act']:
                    f(eng)

            @block.gpsimd
            def _(eng):
                for f in st['pool']:
                    f(eng)

            @block.sync
            def _(eng):
                for f in st['sp']:
                    f(eng)
        self.streams = {e: [] for e in self.eng}

    def mm(self, out, lhsT, rhs, start, stop, R, W):
        self.op('pe', lambda e: e.matmul(out=out, lhsT=lhsT, rhs=rhs, start=start, stop=stop), R, W)

    def tr(self, out, in_, ident, R, W):
        self.op('pe', lambda e: e.transpose(out=out, in_=in_, identity=ident), R, W)

    @staticmethod
    def _ln(ap):
        n = 1
        for d in ap.shape[1:]:
            n *= int(d)
        return n

    def act(self, out, in_, func, R, W, scale=None, bias=None, accum_out=None):
        kw = {}
        if scale is not None:
            kw['scale'] = scale
        if bias is not None:
            kw['bias'] = bias
        if accum_out is not None:
            kw['accum_out'] = accum_out
        self.op('act', lambda e: e.activation(out=out, in_=in_, func=func, **kw), R, W, ln=0 if accum_out is not None else self._ln(out))

    def ts(self, eng, out, in0, s1, s2, op0, op1, R, W):
        if op1 is None:
            self.op(eng, lambda e: e.tensor_scalar(out=out, in0=in0, scalar1=s1, scalar2=None, op0=op0), R, W, ln=self._ln(out))
        else:
            self.op(eng, lambda e: e.tensor_scalar(out=out, in0=in0, scalar1=s1, scalar2=s2, op0=op0, op1=op1), R, W, ln=self._ln(out))

    def tt(self, eng, out, in0, in1, op, R, W):
        self.op(eng, lambda e: e.tensor_tensor(out=out, in0=in0, in1=in1, op=op), R, W, ln=self._ln(out))

    def stt(self, out, in0, scalar, in1, op0, op1, R, W):
        self.op('dve', lambda e: e.scalar_tensor_tensor(out=out, in0=in0, scalar=scalar, in1=in1, op0=op0, op1=op1), R, W, ln=self._ln(out))

    def copy(self, eng, out, in_, R, W):
        if eng == 'act':
            self.op('act', lambda e: e.copy(out=out, in_=in_), R, W, ln=self._ln(out))
        else:
            self.op(eng, lambda e: e.tensor_copy(out=out, in_=in_), R, W, ln=self._ln(out))

    def memset(self, eng, ap, val, W):
        self.op(eng, lambda e: e.memset(ap, val), (), W, ln=self._ln(ap))

    def recip(self, out, in_, R, W):
        self.op('dve', lambda e: e.reciprocal(out=out, in_=in_), R, W)


class Ctx:
    def __init__(self, nc, debug):
        self.nc = nc
        self.debug = debug
        self.inputs = {}
        self.feed = None
        self.dbg_names = []

    def din(self, name, arr):
        arr = np.ascontiguousarray(arr, dtype=np.float32)
        self.inputs[name] = arr
        return self.nc.dram_tensor(name, list(arr.shape), F32, kind="ExternalInput").ap()

    def dscr(self, name, shape, dt):
        if self.feed and name in self.feed:
            return self.nc.dram_tensor(name, list(shape), dt, kind="ExternalInput").ap()
        if self.debug and name in self.debug:
            self.dbg_names.append(name)
            return self.nc.dram_tensor(name, list(shape), dt, kind="ExternalOutput").ap()
        return self.nc.dram_tensor(name, list(shape), dt, kind="Internal").ap()


class Pool_:
    def __init__(self, es, nc, name, shape, dt, n, psum=False):
        mk = nc.psum_tensor if psum else nc.sbuf_tensor
        self.tiles = [es.enter_context(mk(f"{name}{i}", shape, dt)) for i in range(n)]
        self.name = name
        self.i = -1

    def next(self):
        self.i = (self.i + 1) % len(self.tiles)
        return self.tiles[self.i], (self.name, self.i)


def _pos_feats(idx, L):
    idx = np.asarray(idx, dtype=np.float64)
    t = (idx / (L - 1)).astype(np.float32).astype(np.float64)
    w = (2.0 * math.pi * idx.astype(np.float32) / np.float32(L)).astype(np.float64)
    bands = np.linspace(1e-4, 15.0, 16, dtype=np.float32).astype(np.float64)
    arg = bands[None, :] * w[:, None]
    return np.concatenate([t[:, None], np.cos(arg), -np.sin(arg)], axis=1).astype(np.float32)


def host_consts():
    C = {}
    C['ident'] = np.eye(128, dtype=np.float32)
    n = np.arange(128)
    k1 = np.arange(65)
    ang = 2 * np.pi * np.outer(n, k1) / 128.0
    C['FA'] = np.concatenate([np.cos(ang), -np.sin(ang[:, 1:64])], axis=1)
    n2 = n[:, None, None]
    kk = (k1[None, :, None] + 128 * n[None, None, :])
    th = 2 * np.pi * (n2 * kk % 16384) / 16384.0
    C['TC'] = np.stack([np.cos(th), np.sin(th)], axis=2)
    ph = 2 * np.pi * np.outer(n, n) / 128.0
    C['IR'] = np.stack([np.concatenate([np.cos(ph), np.sin(ph)], 1), np.concatenate([-np.sin(ph), np.cos(ph)], 1)], axis=1)
    wk = np.where((k1 == 0) | (k1 == 64), 1.0, 2.0) / 16384.0
    tw = 2 * np.pi * np.outer(k1, n) / 16384.0
    C['TWI'] = np.stack([wk[:, None] * np.cos(tw), wk[:, None] * np.sin(tw)], axis=1)
    n1 = np.arange(64)
    g = 2 * np.pi * np.outer(k1, n1) / 128.0
    C['G'] = np.stack([np.cos(g), -np.sin(g)], axis=1)
    N2, N1 = np.meshgrid(np.arange(128), np.arange(128), indexing='ij')
    fwd = N1 < 64
    idx = np.where(fwd, 128 * N1 + N2, 16384 - 128 * N1 - N2)
    idx = np.where(idx >= T, 0, idx)
    C['posR'] = _pos_feats(idx.reshape(-1), T).T.copy()
    deltas = np.abs(np.linspace(math.log(1e-2) / 1.5, math.log(1e-2) / 0.3, D, dtype=np.float32)).astype(np.float64)
    C['absdelta'] = deltas
    a_n1 = np.where(np.arange(128) < 64, 128.0 * np.arange(128), 16384.0 - 128.0 * np.arange(128)) / (T - 1)
    C['decL'] = np.stack([a_n1, (np.arange(128) < 64).astype(np.float64), (np.arange(128) >= 64).astype(np.float64)], axis=0)
    r0 = np.broadcast_to(-deltas[None, :], (128, D))
    r1 = -np.outer(np.arange(128) / (T - 1), deltas)
    C['decR'] = np.stack([r0, r1, -r1], axis=0)
    dch = np.concatenate([np.arange(255, 0, -1), np.arange(0, 256)])
    C['posC'] = _pos_feats(np.concatenate([dch, [0]]), TC).T.copy()
    C['tC'] = (dch / (TC - 1.0)).astype(np.float32)[None, :]
    C['ndelta'] = (-deltas).reshape(8, 128).T.copy()
    i = np.arange(128)
    C['U'] = (i[:, None] <= i[None, :]).astype(np.float32)
    C['UT'] = (i[:, None] >= i[None, :]).astype(np.float32)
    C['ones'] = np.ones((128, 128), dtype=np.float32)
    return {k: np.ascontiguousarray(v, dtype=np.float32) for k, v in C.items()}


class _NC:
    def __init__(self, nc):
        self._nc = nc
        self._uid = 0

    def __getattr__(self, k):
        return getattr(self._nc, k)

    def sbuf_tensor(self, name, shape, dt):
        self._uid += 1
        return self._nc.sbuf_tensor(f"{name}_{self._uid}", shape, dt)

    def psum_tensor(self, name, shape, dt):
        self._uid += 1
        return self._nc.psum_tensor(f"{name}_{self._uid}", shape, dt)


def build(inp, debug=False, stop_after=None, feed=None):
    nc_real = bass.Bass("TRN2", target_bir_lowering=False)
    nc = _NC(nc_real)
    cx = Ctx(nc, debug)
    cx.feed = feed
    HC = host_consts()
    es = ExitStack()
    P = Prog(nc, es)

    def sbp(name, shape, dt):
        return es.enter_context(nc.sbuf_tensor(name, shape, dt))

    x_d = cx.din('x', inp['x'])
    ctx_d = cx.din('ctx', inp['ctx'])
    cvec_d = cx.din('cvec', inp['cvec'])
    W = {}
    for k in ('mod_w', 'mod_b', 'norm_g', 'final_g', 'hy_w_in', 'hy_b_in', 'hy_sc_w', 'hy_sc_b', 'hy_w1d', 'hy_b1d',
              'hy_w2bd', 'hy_b2d', 'hy_w3bd', 'hy_b3d', 'hy_wo_st', 'hy_freqd', 'hy_bias', 'hy_w_out', 'hy_b_out',
              'ml_w_in', 'ml_conv_w', 'ml_conv_b', 'ml_bdq', 'ml_bdk', 'ml_bdv', 'ml_w_gate', 'ml_b_gate', 'ml_norm_w',
              'ml_skip', 'ml_w_down', 'ffn_w_up', 'ffn_conv_w', 'ffn_conv_b', 'ffn_w_down',
              'mod_b_col', 'norm_g_col', 'hy_b_in_col', 'hy_sc_w_col', 'hy_sc_b_col', 'hy_bias_col', 'ml_conv_w_col',
              'ml_conv_b_col', 'ml_norm_w_col', 'ml_skip_col', 'ffn_conv_w_col', 'ffn_conv_b_col'):
        W[k] = cx.din(k, inp[k])
    K = {k: cx.din('c_' + k, v) for k, v in HC.items()}
    out_d = nc.dram_tensor('out', [T, D], F32, kind="ExternalOutput").ap()

    MODROW = cx.dscr('MODROW', [2, 2, 6144], F32)
    P0 = cx.dscr('P0', [3 * D, T], BF16)
    P0C = cx.dscr('P0C', [3 * D, TC], BF16)
    Zd = cx.dscr('Zd', [D, T], BF16)
    X0d = cx.dscr('X0d', [D, T], BF16)
    KFd = cx.dscr('KFd', [16, 128, 2, 65, 64], BF16)
    RNd = cx.dscr('RNd', [128, 8], F32)
    Ytd = cx.dscr('Ytd', [D, T], F32)
    XA0 = cx.dscr('XA0', [T, D], F32)
    XB0 = cx.dscr('XB0', [T, D], F32)
    CA0 = cx.dscr('CA0', [TC, D], F32)
    CB0 = cx.dscr('CB0', [TC, D], F32)
    XA1 = cx.dscr('XA1', [T, D], F32)
    ZdC = cx.dscr('ZdC', [D, TC], BF16)
    XMd = cx.dscr('XMd', [MI, T], BF16)
    SZd = cx.dscr('SZd', [MI, T], BF16)
    XMC = cx.dscr('XMC', [MI, TC], BF16)
    Qfm = cx.dscr('Qfm', [MI, T], BF16)
    Kfm = cx.dscr('Kfm', [MI, T], BF16)
    Ktm = cx.dscr('Ktm', [T, MI], BF16)
    Vtm = cx.dscr('Vtm', [T, MI], BF16)
    SXC = cx.dscr('SXC', [MI, T], BF16)
    GPd = cx.dscr('GPd', [T, 4, 8], F32)
    KtmC = cx.dscr('KtmC', [TC, MI], BF16)
    VtmC = cx.dscr('VtmC', [TC, MI], BF16)
    GPdC = cx.dscr('GPdC', [TC, 4, 8], F32)
    HFB = cx.dscr('HFB', [2, T, MI], F32)
    X0dC = cx.dscr('X0dC', [D, TC], BF16)
    YtC = cx.dscr('YtC', [D, TC], F32)
    RNdC = cx.dscr('RNdC', [128, 8], F32)

    identb = sbp('identb', [128, 128], BF16)
    identf = sbp('identf', [128, 128], F32)
    modcol = sbp('modcol', [128, 2, 48, 2], F32)
    effs = sbp('effs', [128, 2, 2, 2, 8], F32)
    P.dma('sp', identf[:], K['ident'], W=['identf'])
    P.copy('dve', identb[:], identf[:], R=['identf'], W=['identb'])

    def phase_adaln():
        with ExitStack() as ps:
            sb = lambda n, s, d: ps.enter_context(nc.sbuf_tensor(n, s, d))
            cv = sb('cv', [128, 8, 2], F32)
            scv = sb('scv', [128, 8, 2], F32)
            mbcol = sb('mbcol', [128, 2, 48], F32)
            mbrow = sb('mbrow', [2, 2, 6144], F32)
            rowbuf = sb('rowbuf', [2, 2, 6144], F32)
            ngcol = sb('ngcol', [128, 2, 2, 8], F32)
            mwp = Pool_(ps, nc, 'mw', [128, 8, 1024], F32, 2)
            pcol = ps.enter_context(nc.psum_tensor('pcol', [128, 48, 2], F32))
            prow = Pool_(ps, nc, 'prow', [2, 512], F32, 2, psum=True)
            P.dma('sp', cv[:], cvec_d.rearrange("(k p) m -> p k m", p=128), W=['cv'])
            P.dma('sp', mbcol[:], W['mod_b_col'], W=['mbcol'])
            for l in range(2):
                P.dma('sp', mbrow[:, l, :], W['mod_b'][l:l + 1, :].partition_broadcast(2) if False else W['mod_b'][l:l + 1, :].broadcast_to([2, 6144]), W=['mbrow'])
            P.dma('sp', ngcol[:], W['norm_g_col'], W=['ngcol'])
            P.act(scv[:], cv[:], AF.Silu, R=['cv'], W=['scv'])
            for l in range(2):
                for j in range(6):
                    mw, mwk = mwp.next()
                    P.dma('sp', mw[:], W['mod_w'][l, :, j * 1024:(j + 1) * 1024].rearrange("(k p) n -> p k n", p=128), W=[mwk])
                    for m in range(8):
                        for k in range(8):
                            P.mm(pcol[:, j * 8 + m, :], mw[:, k, m * 128:(m + 1) * 128], scv[:, k, :], k == 0, k == 7,
                                 R=[mwk, 'scv'], W=['pcol'])
                    for half in range(2):
                        pr, prk = prow.next()
                        for k in range(8):
                            P.mm(pr[:], scv[:, k, :], mw[:, k, half * 512:(half + 1) * 512], k == 0, k == 7, R=[mwk, 'scv'], W=[prk])
                        o = j * 1024 + half * 512
                        P.tt('dve', rowbuf[:, l, o:o + 512], pr[:], mbrow[:, l, o:o + 512], ALU.add, R=[prk, 'mbrow'], W=['rowbuf'])
                for m in range(2):
                    P.tt('dve', modcol[:, l, :, m], pcol[:, :, m], mbcol[:, l, :], ALU.add, R=['pcol', 'mbcol'], W=['modcol'])
                for i in range(2):
                    for m in range(2):
                        sc_ap = modcol[:, l, 8 + 24 * i:16 + 24 * i, m]
                        P.ts('dve', effs[:, l, i, m, :], sc_ap, 1.0, None, ALU.add, None, R=['modcol'], W=['effs'])
                        P.tt('dve', effs[:, l, i, m, :], effs[:, l, i, m, :], ngcol[:, l, i, :], ALU.mult, R=['effs', 'ngcol'], W=['effs'])
            P.dma('sp', MODROW.rearrange("l m n -> m l n"), rowbuf[:], R=['rowbuf'], W=['MODROW'])
            P.flush()

    def shift_col(l, i, m):
        return modcol[:, l, 24 * i:24 * i + 8, m]

    class FrontEnd:
        def __init__(self, ps, nblk, inplace=False, junk=None):
            self.nblk = nblk
            self.inplace = inplace
            self.junk_ap = junk
            if not inplace:
                self.xn = Pool_(ps, nc, 'fe_xn', [128, nblk, D], BF16, 1)
            if junk is None:
                self.junk = ps.enter_context(nc.sbuf_tensor('fe_junk', [128, D], BF16))
                self.junk_ap = self.junk[:]
            self.ss = Pool_(ps, nc, 'fe_ss', [128, nblk], F32, 2)
            self.pT = Pool_(ps, nc, 'fe_pT', [128, nblk * 128], BF16, 2, psum=True)

        def run(self, xt, xtk, hT, hTk, l, i, m):
            nblk = self.nblk
            ss, ssk = self.ss.next()
            if self.inplace:
                xn, xnk = xt, xtk
            else:
                xn, xnk = self.xn.next()
            for j in range(nblk):
                P.act(self.junk_ap, xt[:, j, :], AF.Square, R=[xtk], W=['fe_junk', ssk], accum_out=ss[:, j:j + 1])
            P.ts('dve', ss[:], ss[:], 1.0 / D, EPS, ALU.mult, ALU.add, R=[ssk], W=[ssk])
            P.act(ss[:], ss[:], AF.Sqrt, R=[ssk], W=[ssk])
            P.recip(ss[:], ss[:], R=[ssk], W=[ssk])
            for j in range(nblk):
                P.ts('pool', xn[:, j, :], xt[:, j, :], ss[:, j:j + 1], None, ALU.mult, None, R=[xtk, ssk], W=[xnk])
            for k in range(8):
                pT, pTk = self.pT.next()
                for j in range(nblk):
                    P.tr(pT[:, j * 128:(j + 1) * 128], xn[:, j, k * 128:(k + 1) * 128], identb[:], R=[xnk, 'identb'], W=[pTk])
                P.act(hT[:, k, :], pT[:], AF.Identity, R=[pTk, 'effs', 'modcol'], W=[hTk],
                      scale=effs[:, l, i, m, k:k + 1], bias=shift_col(l, i, m)[:, k:k + 1])

    def phase_h1(src, Tn, dst):
        nblk = 4 if Tn >= 512 else Tn // 128
        TT = nblk * 128
        with ExitStack() as ps:
            sb = lambda n, s, d: ps.enter_context(nc.sbuf_tensor(n, s, d))
            win = sb('win', [128, 8, 3 * D], BF16)
            bcol = sb('bcol', [128, 24], F32)
            fe = FrontEnd(ps, nblk)
            xtp = Pool_(ps, nc, 'xt', [128, nblk, D], F32, 2)
            hTp = Pool_(ps, nc, 'hT', [128, 8, TT], BF16, 2)
            pbp = Pool_(ps, nc, 'pb', [128, 6, TT], BF16, 2)
            pp = Pool_(ps, nc, 'pp', [128, TT], F32, 4, psum=True)
            for k in range(8):
                P.dma('pool', win[:, k, :], W['hy_w_in'][k * 128:(k + 1) * 128, :], W=['win'])
            P.dma('sp', bcol[:], W['hy_b_in_col'], W=['bcol'])
            m = 0 if Tn == T else 1

            def load(ti):
                xt, xtk = xtp.next()
                P.dma('sp', xt[:], src[ti * TT:(ti + 1) * TT, :].rearrange("(j p) d -> p j d", p=128), W=[xtk])
                return xt, xtk
            ntile = Tn // TT
            ld = load(0)
            for ti in range(ntile):
                t0 = ti * TT
                xt, xtk = ld
                if ti + 1 < ntile:
                    ld = load(ti + 1)
                hT, hTk = hTp.next()
                fe.run(xt, xtk, hT, hTk, 0, 0, m)
                for og in range(4):
                    pb, pbk = pbp.next()
                    for oi in range(6):
                        oc = og * 6 + oi
                        pt, ptk = pp.next()
                        for k in range(8):
                            P.mm(pt[:], win[:, k, oc * 128:(oc + 1) * 128], hT[:, k, :], k == 0, k == 7, R=['win', hTk], W=[ptk])
                        if oi % 2 == 0:
                            P.act(pb[:, oi, :], pt[:], AF.Identity, R=[ptk, 'bcol'], W=[pbk], bias=bcol[:, oc:oc + 1])
                        else:
                            P.ts('dve', pb[:, oi, :], pt[:], bcol[:, oc:oc + 1], None, ALU.add, None, R=[ptk, 'bcol'], W=[pbk])
                    P.dma('sp', dst[og * 768:(og + 1) * 768, t0:t0 + TT].rearrange("(c p) t -> p c t", p=128), pb[:], R=[pbk], W=[P.uq('P0')])
            P.flush()

    def phase_h2(src, Tn, zdst, x0dst):
        PW = 2048 if Tn >= 2048 else Tn
        npc = Tn // PW
        with ExitStack() as ps:
            sb = lambda n, s, d: ps.enter_context(nc.sbuf_tensor(n, s, d))
            scw = sb('scw', [128, 3, 24], F32)
            scb = sb('scb', [128, 24], F32)
            pinp = Pool_(ps, nc, 'pin', [128, 3, PW + 2], BF16, 2)
            accp = Pool_(ps, nc, 'acc', [128, 3, PW], F32, 2)
            zp = Pool_(ps, nc, 'zt', [128, PW], BF16, 2)
            x0p = Pool_(ps, nc, 'x0t', [128, PW], BF16, 2)
            P.dma('sp', scw[:], W['hy_sc_w_col'], W=['scw'])
            P.dma('sp', scb[:], W['hy_sc_b_col'], W=['scb'])
            srcv = src.rearrange("(j c p) t -> c p j t", j=3, c=8, p=128)
            def load(it):
                cc, pi = it // npc, it % npc
                t0 = pi * PW
                pin, pink = pinp.next()
                lo = 1 if pi == 0 else 0
                hi = PW + 1 if pi == npc - 1 else PW + 2
                if pi == 0:
                    P.memset('pool', pin[:, :, 0:1], 0.0, W=[pink])
                if pi == npc - 1:
                    P.memset('pool', pin[:, :, PW + 1:PW + 2], 0.0, W=[pink])
                P.dma('sp', pin[:, :, lo:hi], srcv[cc][:, :, t0 - 1 + lo:t0 - 1 + hi], W=[pink])
                return pin, pink
            ld = load(0)
            for cc in range(8):
                for pi in range(npc):
                    t0 = pi * PW
                    pin, pink = ld
                    if cc * npc + pi + 1 < 8 * npc:
                        ld = load(cc * npc + pi + 1)
                    acc, acck = accp.next()
                    zt, ztk = zp.next()
                    x0t, x0k = x0p.next()
                    for j in range(3):
                        oc = j * 8 + cc
                        P.act(acc[:, j, :], pin[:, j, 1:PW + 1], AF.Identity, R=[pink, 'scw', 'scb'], W=[(acck, j)],
                              scale=scw[:, 1, oc:oc + 1], bias=scb[:, oc:oc + 1])
                        P.stt(acc[:, j, :], pin[:, j, 0:PW], scw[:, 0, oc:oc + 1], acc[:, j, :], ALU.mult, ALU.add,
                              R=[pink, 'scw', (acck, j)], W=[(acck, j)])
                        if j == 0:
                            P.stt(x0t[:], pin[:, j, 2:PW + 2], scw[:, 2, oc:oc + 1], acc[:, j, :], ALU.mult, ALU.add,
                                  R=[pink, 'scw', (acck, j)], W=[x0k])
                        else:
                            P.stt(acc[:, j, :], pin[:, j, 2:PW + 2], scw[:, 2, oc:oc + 1], acc[:, j, :], ALU.mult, ALU.add,
                                  R=[pink, 'scw', (acck, j)], W=[(acck, j)])
                    P.tt('pool', zt[:], acc[:, 1, :], acc[:, 2, :], ALU.mult, R=[(acck, 1), (acck, 2)], W=[ztk])
                    P.dma('sp', zdst[cc * 128:(cc + 1) * 128, t0:t0 + PW], zt[:], R=[ztk], W=[P.uq('Zd')])
                    P.dma('sp', x0dst[cc * 128:(cc + 1) * 128, t0:t0 + PW], x0t[:], R=[x0k], W=[P.uq('X0d')])
            P.flush()

    def sin_layer(pre, prek, frc, frb, tmpa, tmpk, tmpb, tmpbk, out, outk):
        P.ts('dve', tmpa, pre, frc, frb, ALU.mult, ALU.add, R=[prek, 'mlpc'], W=[tmpk])
        P.ts('dve', tmpb, tmpa, 1.0 / TWO_PI, MAGIC, ALU.mult, ALU.add, R=[tmpk], W=[tmpbk])
        P.ts('dve', tmpb, tmpb, -MAGIC, -TWO_PI, ALU.add, ALU.mult, R=[tmpbk], W=[tmpbk])
        P.tt('dve', tmpa, tmpa, tmpb, ALU.add, R=[tmpk, tmpbk], W=[tmpk])
        P.act(out, tmpa, AF.Sin, R=[tmpk], W=[outk])

    class FilterMLP:
        def __init__(self, ps):
            sb = lambda n, s, d: ps.enter_context(nc.sbuf_tensor(n, s, d))
            self.w1 = sb('mlp_w1', [33, 128], F32)
            self.w2 = sb('mlp_w2', [128, 128], F32)
            self.w3 = sb('mlp_w3', [128, 128], F32)
            self.cols = sb('mlp_cols', [128, 8], F32)
            P.dma('sp', self.w1[:], W['hy_w1d'], W=['mlpw'])
            P.dma('sp', self.w2[:], W['hy_w2bd'], W=['mlpw'])
            P.dma('sp', self.w3[:], W['hy_w3bd'], W=['mlpw'])
            P.dma('sp', self.cols[:, 0:1], W['hy_freqd'], W=['mlpc'])
            P.dma('sp', self.cols[:, 1:2], W['hy_b1d'], W=['mlpc'])
            P.dma('sp', self.cols[:, 2:3], W['hy_b2d'], W=['mlpc'])
            P.dma('sp', self.cols[:, 3:4], W['hy_b3d'], W=['mlpc'])
            P.ts('dve', self.cols[:, 4:7], self.cols[:, 1:4], self.cols[:, 0:1], None, ALU.mult, None, R=['mlpc'], W=['mlpc'])
            self.posp = Pool_(ps, nc, 'mlp_pos', [33, 512], F32, 2)
            self.ta = Pool_(ps, nc, 'mlp_ta', [128, 512], F32, 2)
            self.tb = Pool_(ps, nc, 'mlp_tb', [128, 512], F32, 2)
            self.h = Pool_(ps, nc, 'mlp_h', [128, 512], F32, 2)
            self.pm = Pool_(ps, nc, 'mlp_pm', [128, 512], F32, 2, psum=True)

        def run(self, pos_d, c0, n, out, outk):
            pos, posk = self.posp.next()
            P.dma('sp', pos[:, 0:n], pos_d[:, c0:c0 + n], W=[posk])
            cur, curk, ws = pos[:, 0:n], posk, [self.w1, self.w2, self.w3]
            for li in range(3):
                pm, pmk = self.pm.next()
                P.mm(pm[:, 0:n], ws[li][:], cur, True, True, R=['mlpw', curk], W=[pmk])
                ta, tak = self.ta.next()
                tb, tbk = self.tb.next()
                if li < 2:
                    h, hk = self.h.next()
                    o, ok = h[:, 0:n], hk
                else:
                    o, ok = out, outk
                sin_layer(pm[:, 0:n], pmk, self.cols[:, 0:1], self.cols[:, 4 + li:5 + li], ta[:, 0:n], tak, tb[:, 0:n], tbk, o, ok)
                cur, curk = o, ok

    class FFTConsts:
        def __init__(self, ps, inverse):
            sb = lambda n, s, d: ps.enter_context(nc.sbuf_tensor(n, s, d))
            self.FA = sb('FA', [128, 128], BF16)
            self.TC = sb('TC', [128, 65, 2, 128], BF16)
            P.dma('pool', self.FA[:], K['FA'], W=['fftc'])
            P.dma('pool', self.TC[:], K['TC'], W=['fftc'])
            self.B = sb('Bbuf', [128, 3, 65, 64], BF16)
            P.memset('dve', self.B[:, 1, 0:1, :], 0.0, W=['Bbuf'])
            P.memset('dve', self.B[:, 1, 64:65, :], 0.0, W=['Bbuf'])
            self.pa = Pool_(ps, nc, 'pa', [128, 4, 128], F32, 2, psum=True)
            self.xp = Pool_(ps, nc, 'xp', [128, 2, 4, 64], F32, 2, psum=True)

        def stage_a(self, src, srck, Kp):
            B = self.B
            for c4 in range(16):
                pa, pak = self.pa.next()
                for i in range(4):
                    c = c4 * 4 + i
                    P.mm(pa[:, i, :], src[0:Kp, c, :], self.FA[0:Kp, :], True, True, R=[srck, 'fftc'], W=[pak])
                o0 = B[:, 0, :, c4 * 4:c4 * 4 + 4].rearrange("p k c -> p c k")
                o2 = B[:, 2, :, c4 * 4:c4 * 4 + 4].rearrange("p k c -> p c k")
                o1 = B[:, 1, 1:64, c4 * 4:c4 * 4 + 4].rearrange("p k c -> p c k")
                P.act(o0, pa[:, :, 0:65], AF.Identity, R=[pak], W=['Bbuf'])
                P.act(o2, pa[:, :, 0:65], AF.Identity, R=[pak], W=['Bbuf'], scale=-1.0)
                P.copy('dve', o1, pa[:, :, 65:128], R=[pak], W=['Bbuf'])

        def stage_c(self, consume):
            B, TC = self.B, self.TC
            for kq in range(17):
                k10 = kq * 4
                nk = min(4, 65 - k10)
                xp, xpk = self.xp.next()
                for i in range(nk):
                    k1 = k10 + i
                    P.mm(xp[:, 0, i, :], TC[:, k1, 0, :], B[:, 0, k1, :], True, False, R=['fftc', 'Bbuf'], W=[xpk])
                    P.mm(xp[:, 0, i, :], TC[:, k1, 1, :], B[:, 1, k1, :], False, True, R=['fftc', 'Bbuf'], W=[xpk])
                    P.mm(xp[:, 1, i, :], TC[:, k1, 0, :], B[:, 1, k1, :], True, False, R=['fftc', 'Bbuf'], W=[xpk])
                    P.mm(xp[:, 1, i, :], TC[:, k1, 1, :], B[:, 2, k1, :], False, True, R=['fftc', 'Bbuf'], W=[xpk])
                consume(xp, xpk, k10, nk)

    def phase_k():
        with ExitStack() as ps:
            sb = lambda n, s, d: ps.enter_context(nc.sbuf_tensor(n, s, d))
            h3 = sb('h3', [128, 16384], BF16)
            with ExitStack() as ps2:
                mlp = FilterMLP(ps2)
                for ct in range(32):
                    mlp.run(K['posR'], ct * 512, 512, h3[:, ct * 512:(ct + 1) * 512], 'h3')
                h3v = h3[:].rearrange("p (a b) -> p a b", b=128)
                P.memset('pool', h3v[0:64, :, 64:128], 0.0, W=['h3'])
                P.memset('pool', h3v[64:128, :, 0:64], 0.0, W=['h3'])
                P.memset('pool', h3v[64:128, 0:1, 64:65], 0.0, W=['h3'])
                P.flush()
            fc = FFTConsts(ps, False)
            wo = sb('wo', [128, D], BF16)
            decL = sb('decL', [3, 128], F32)
            ones = sb('onesf', [128, 1], F32)
            P.dma('pool', wo[:], W['hy_wo_st'], W=['wo'])
            P.dma('sp', decL[:], K['decL'], W=['decL'])
            P.memset('dve', ones[:], 1.0, W=['onesf'])
            KAp = Pool_(ps, nc, 'KA', [128, 64, 128], BF16, 2)
            KFp = Pool_(ps, nc, 'KFs', [128, 2, 65, 64], BF16, 2)
            decRp = Pool_(ps, nc, 'decR', [3, 8, 64], F32, 3)
            decp = Pool_(ps, nc, 'dec', [128, 8, 64], F32, 2)
            tmpp = Pool_(ps, nc, 'ktmp', [128, 8, 64], F32, 2)
            partp = Pool_(ps, nc, 'kpart', [128, 64], F32, 2)
            accp = Pool_(ps, nc, 'kacc', [128, 64], F32, 2)
            rnp = Pool_(ps, nc, 'rn', [64, 1], F32, 2)
            pkp = Pool_(ps, nc, 'pk', [128, 8, 64], F32, 2, psum=True)
            pep = Pool_(ps, nc, 'pe', [128, 8, 64], F32, 1, psum=True)
            pnp = Pool_(ps, nc, 'pn', [64, 1], F32, 1, psum=True)
            for g in range(16):
                c0 = g * 64
                KA, KAk = KAp.next()
                acc, acck = accp.next()
                for nb in range(16):
                    dr, drk = decRp.next()
                    P.dma('sp', dr[:], K['decR'][:, nb * 8:(nb + 1) * 8, c0:c0 + 64], W=[drk])
                    pk, pkk = pkp.next()
                    for i in range(8):
                        n2 = nb * 8 + i
                        P.mm(pk[:, i, :], h3[:, n2 * 128:(n2 + 1) * 128], wo[:, c0:c0 + 64], True, True, R=['h3', 'wo'], W=[pkk])
                    pe_, pek = pep.next()
                    P.mm(pe_[:].rearrange('p a b -> p (a b)'), decL[:], dr[:].rearrange('p a b -> p (a b)'), True, True, R=['decL', drk], W=[pek])
                    dec, deck = decp.next()
                    P.act(dec[:], pe_[:], AF.Exp, R=[pek], W=[deck])
                    tmp, tmpk = tmpp.next()
                    P.tt('dve', tmp[:], pk[:], dec[:], ALU.mult, R=[pkk, deck], W=[tmpk])
                    tv = tmp[:].rearrange("p n c -> p c n")
                    P.copy('pool', KA[:, :, nb * 8:(nb + 1) * 8], tv, R=[tmpk], W=[KAk])
                    if nb == 0:
                        P.op('dve', lambda e, o=acc[:], i_=tv: e.tensor_reduce(out=o, in_=i_, axis=AX.X, op=ALU.add, apply_absolute_value=True),
                             R=[tmpk], W=[acck])
                    else:
                        part, partk = partp.next()
                        P.op('dve', lambda e, o=part[:], i_=tv: e.tensor_reduce(out=o, in_=i_, axis=AX.X, op=ALU.add, apply_absolute_value=True),
                             R=[tmpk], W=[partk])
                        P.tt('pool', acc[:], acc[:], part[:], ALU.add, R=[acck, partk], W=[acck])
                pn, pnk = pnp.next()
                P.mm(pn[:], acc[:], ones[:], True, True, R=[acck, 'onesf'], W=[pnk])
                rn, rnk = rnp.next()
                P.recip(rn[:], pn[:], R=[pnk], W=[rnk])
                P.dma('sp', RNd[(g % 2) * 64:(g % 2) * 64 + 64, g // 2:g // 2 + 1], rn[:], R=[rnk], W=[P.uq('RNd')], allow_slow_non_contiguous=True)
                fc.stage_a(KA, KAk, 128)
                KFs, KFk = KFp.next()

                def consume(xp, xpk, k10, nk, KFs=KFs, KFk=KFk):
                    P.act(KFs[:, :, k10:k10 + nk, :], xp[:, :, 0:nk, :], AF.Identity, R=[xpk], W=[KFk])
                fc.stage_c(consume)
                P.dma('sp', KFd[g], KFs[:], R=[KFk], W=[P.uq('KFd')])
            P.flush()

    def phase_z():
        with ExitStack() as ps:
            sb = lambda n, s, d: ps.enter_context(nc.sbuf_tensor(n, s, d))
            fc = FFTConsts(ps, True)
            IR = sb('IR', [128, 2, 256], BF16)
            TWI = sb('TWI', [65, 2, 128], F32)
            G = sb('G', [65, 2, 64], BF16)
            P.dma('pool', IR[:], K['IR'], W=['IR'])
            P.dma('sp', TWI[:], K['TWI'], W=['TWI'])
            P.dma('pool', G[:], K['G'], W=['G'])
            ZAp = Pool_(ps, nc, 'ZA', [64, 64, 128], BF16, 2)
            KFp = Pool_(ps, nc, 'KF', [128, 2, 65, 64], BF16, 1)
            Yp = Pool_(ps, nc, 'Y', [128, 2, 64, 65], BF16, 1)
            t1p = Pool_(ps, nc, 'zt1', [128, 4, 64], F32, 2)
            t2p = Pool_(ps, nc, 'zt2', [128, 4, 64], F32, 2)
            crp = Pool_(ps, nc, 'craw', [65, 2, 2, 128], F32, 2)
            m1p = Pool_(ps, nc, 'm1', [65, 2, 2, 128], F32, 2)
            m2p = Pool_(ps, nc, 'm2', [65, 2, 2, 128], F32, 2)
            C2p = Pool_(ps, nc, 'C2', [65, 2, 16, 128], BF16, 2)
            ybp = Pool_(ps, nc, 'ybuf', [64, 16, 128], F32, 2)
            cpp = Pool_(ps, nc, 'cp', [65, 2, 2, 128], F32, 2, psum=True)
            ypp = Pool_(ps, nc, 'yp', [64, 4, 128], F32, 2, psum=True)
            twc = TWI[:, 0:1, :].unsqueeze(1).broadcast_to([65, 2, 2, 128]) if False else None
            for g in range(16):
                c0 = g * 64
                ZA, ZAk = ZAp.next()
                P.dma('sp', ZA[:], Zd[c0:c0 + 64, :].rearrange("c (a b) -> a c b", b=128), W=[ZAk])
                KF, KFk = KFp.next()
                P.dma('sp', KF[:], KFd[g], W=[KFk])
                fc.stage_a(ZA, ZAk, 64)
                Y, Yk = Yp.next()

                def consume(xp, xpk, k10, nk, KF=KF, KFk=KFk, Y=Y, Yk=Yk):
                    t1, t1k = t1p.next()
                    t2, t2k = t2p.next()
                    yo = lambda ri: Y[:, ri, :, k10:k10 + nk].rearrange("p c k -> p k c")
                    P.tt('dve', t1[:, 0:nk, :], xp[:, 0, 0:nk, :], KF[:, 0, k10:k10 + nk, :], ALU.mult, R=[xpk, KFk], W=[t1k])
                    P.tt('dve', t2[:, 0:nk, :], xp[:, 1, 0:nk, :], KF[:, 1, k10:k10 + nk, :], ALU.mult, R=[xpk, KFk], W=[t2k])
                    P.tt('pool', yo(0), t1[:, 0:nk, :], t2[:, 0:nk, :], ALU.subtract, R=[t1k, t2k], W=[Yk])
                    t1, t1k = t1p.next()
                    t2, t2k = t2p.next()
                    P.tt('dve', t1[:, 0:nk, :], xp[:, 0, 0:nk, :], KF[:, 1, k10:k10 + nk, :], ALU.mult, R=[xpk, KFk], W=[t1k])
                    P.tt('dve', t2[:, 0:nk, :], xp[:, 1, 0:nk, :], KF[:, 0, k10:k10 + nk, :], ALU.mult, R=[xpk, KFk], W=[t2k])
                    P.tt('pool', yo(1), t1[:, 0:nk, :], t2[:, 0:nk, :], ALU.add, R=[t1k, t2k], W=[Yk])
                fc.stage_c(consume)
                for cs in range(4):
                    C2, C2k = C2p.next()
                    yb, ybk = ybp.next()
                    for cb in range(8):
                        cp, cpk = cpp.next()
                        for i in range(2):
                            c = cs * 16 + cb * 2 + i
                            o = cp[:, i, :, :].rearrange("p a b -> p (a b)")
                            P.mm(o, Y[:, 0, c, :], IR[:, 0, :], True, False, R=[Yk, 'IR'], W=[cpk])
                            P.mm(o, Y[:, 1, c, :], IR[:, 1, :], False, True, R=[Yk, 'IR'], W=[cpk])
                        cr, crk = crp.next()
                        P.copy('act', cr[:], cp[:], R=[cpk], W=[crk])
                        m1, m1k = m1p.next()
                        m2, m2k = m2p.next()
                        for i in range(2):
                            for r in range(2):
                                P.tt('dve' if r == 0 else 'pool', m1[:, i, r, :], cr[:, i, r, :], TWI[:, 0, :], ALU.mult, R=[crk, 'TWI'], W=[(m1k, i, r)])
                                P.tt('dve' if r == 1 else 'pool', m2[:, i, r, :], cr[:, i, r, :], TWI[:, 1, :], ALU.mult, R=[crk, 'TWI'], W=[(m2k, i, r)])
                        cc0 = cb * 2
                        P.tt('pool', C2[:, 0, cc0:cc0 + 2, :], m1[:, :, 0, :], m2[:, :, 1, :], ALU.subtract,
                             R=[(m1k, 0, 0), (m1k, 1, 0), (m2k, 0, 1), (m2k, 1, 1)], W=[(C2k, cb // 2)])
                        P.tt('dve', C2[:, 1, cc0:cc0 + 2, :], m2[:, :, 0, :], m1[:, :, 1, :], ALU.add,
                             R=[(m2k, 0, 0), (m2k, 1, 0), (m1k, 0, 1), (m1k, 1, 1)], W=[(C2k, cb // 2)])
                        if cb % 2 == 1:
                            q = cb // 2
                            yp, ypk = ypp.next()
                            o = yp[:].rearrange("p a b -> p (a b)")
                            P.mm(o, G[:, 0, :], C2[:, 0, q * 4:q * 4 + 4, :].rearrange("p a b -> p (a b)"), True, False, R=['G', (C2k, q)], W=[ypk])
                            P.mm(o, G[:, 1, :], C2[:, 1, q * 4:q * 4 + 4, :].rearrange("p a b -> p (a b)"), False, True, R=['G', (C2k, q)], W=[ypk])
                            P.copy('act', yb[:, q * 4:q * 4 + 4, :], yp[:], R=[ypk], W=[ybk])
                    cA = c0 + cs * 16
                    P.dma('sp', Ytd[cA:cA + 16, :].rearrange("c (a b) -> a c b", b=128), yb[:], R=[ybk], W=[P.uq('Ytd')])
            P.flush()

    def load_row(sb_tile, key, src1d, q='sp'):
        P.dma(q, sb_tile, src1d.partition_broadcast(128), W=[key])

    def phase_h5(ysrc, zsrc, x0src, xin, xout, Tn, m, rnsrc):
        TT = 512 if Tn >= 512 else Tn
        nb = TT // 128
        with ExitStack() as ps:
            sb = lambda n, s, d: ps.enter_context(nc.sbuf_tensor(n, s, d))
            wout = sb('wout', [128, 8, D], BF16)
            P.dma('pool', wout[:], W['hy_w_out'].rearrange("(k p) n -> p k n", p=128), W=['wout'])
            rn = sb('rncol', [128, 8], F32)
            bias = sb('hbias', [128, 8], F32)
            P.dma('sp', rn[:], rnsrc, W=['rncol'])
            P.dma('sp', bias[:], W['hy_bias_col'], W=['hbias'])
            brow = sb('brow', [128, D], F32)
            grow = sb('grow', [128, D], F32)
            load_row(brow[:], 'brow', W['hy_b_out'])
            load_row(grow[:], 'grow', MODROW[0, m, 2048:3072])
            ytp = Pool_(ps, nc, 'yt', [128, 8, TT], F32, 2)
            ztp = Pool_(ps, nc, 'zt5', [128, 8, TT], BF16, 2)
            x0p = Pool_(ps, nc, 'x05', [128, 8, TT], BF16, 2)
            a1p = Pool_(ps, nc, 'a1', [128, TT], F32, 2)
            gtp = Pool_(ps, nc, 'gt', [128, 8, TT], BF16, 2)
            xtp = Pool_(ps, nc, 'xt5', [128, nb, D], F32, 2)
            tmp = Pool_(ps, nc, 'tmp5', [128, 512], F32, 3)
            pop = Pool_(ps, nc, 'po', [128, 512], F32, 4, psum=True)
            def load(ti):
                t0 = ti * TT
                yt, ytk = ytp.next()
                zt, ztk = ztp.next()
                x0, x0k = x0p.next()
                xt, xtk = xtp.next()
                P.dma('sp', yt[:], ysrc[:, t0:t0 + TT].rearrange("(k p) t -> p k t", p=128), W=[ytk])
                P.dma('sp', zt[:], zsrc[:, t0:t0 + TT].rearrange("(k p) t -> p k t", p=128), W=[ztk])
                P.dma('sp', x0[:], x0src[:, t0:t0 + TT].rearrange("(k p) t -> p k t", p=128), W=[x0k])
                P.dma('sp', xt[:], xin[t0:t0 + TT, :].rearrange("(j p) d -> p j d", p=128), W=[xtk])
                return yt, ytk, zt, ztk, x0, x0k, xt, xtk
            ntile = Tn // TT
            ld = load(0)
            for ti in range(ntile):
                t0 = ti * TT
                yt, ytk, zt, ztk, x0, x0k, xt, xtk = ld
                if ti + 1 < ntile:
                    ld = load(ti + 1)
                gt, gtk = gtp.next()
                for k in range(8):
                    a1, a1k = a1p.next()
                    P.ts('pool', a1[:], zt[:, k, :], bias[:, k:k + 1], None, ALU.mult, None, R=[ztk, 'hbias'], W=[a1k])
                    P.stt(a1[:], yt[:, k, :], rn[:, k:k + 1], a1[:], ALU.mult, ALU.add, R=[ytk, 'rncol', a1k], W=[a1k])
                    P.tt('pool', gt[:, k, :], a1[:], x0[:, k, :], ALU.mult, R=[a1k, x0k], W=[(gtk, k)])
                for tb in range(nb):
                    for dh in range(2):
                        po, pok = pop.next()
                        for k in range(8):
                            P.mm(po[:], gt[:, k, tb * 128:(tb + 1) * 128], wout[:, k, dh * 512:(dh + 1) * 512], k == 0, k == 7,
                                 R=[(gtk, k), 'wout'], W=[pok])
                        tm, tmk = tmp.next()
                        dsl = slice(dh * 512, (dh + 1) * 512)
                        P.tt('dve', tm[:], po[:], brow[:, dsl], ALU.add, R=[pok, 'brow'], W=[tmk])
                        P.tt('pool', tm[:], tm[:], grow[:, dsl], ALU.mult, R=[tmk, 'grow'], W=[tmk])
                        P.tt('pool', xt[:, tb, dsl], tm[:], xt[:, tb, dsl], ALU.add, R=[tmk, xtk], W=[xtk])
                P.dma('sp', xout[t0:t0 + TT, :].rearrange("(j p) d -> p j d", p=128), xt[:], R=[xtk], W=[P.uq('xout')])
            P.flush()

    def phase_ffn(l, xin, xout, Tn, m, final=False):
        lat = Tn == T
        halo = 64 if lat else 0
        CEN = 256
        NTOK = CEN + 2 * halo
        nblk = NTOK // 128
        gc = 64 if lat else 256
        R_ = CEN // gc
        RH = NTOK // gc
        ntile = Tn // CEN
        with ExitStack() as ps:
            sb = lambda n, s, d: ps.enter_context(nc.sbuf_tensor(n, s, d))
            wup = sb('wup', [128, 8, 2 * FH], BF16)
            wdn = sb('wdn', [128, NFC, D], BF16)
            for k in range(8):
                P.dma('pool', wup[:, k, :], W['ffn_w_up'][l, k * 128:(k + 1) * 128, :], W=['wup'])
            for fcc in range(NFC):
                P.dma('pool', wdn[:, fcc, :], W['ffn_w_down'][l, fcc * 128:(fcc + 1) * 128, :], W=['wdn'])
            cw = sb('cw', [128, 2, 9, NFC], F32)
            cb = sb('cb', [128, 2, NFC], F32)
            P.dma('sp', cw[:], W['ffn_conv_w_col'], W=['cw'])
            P.dma('sp', cb[:], W['ffn_conv_b_col'], W=['cw'])
            grow = sb('grow2', [128, D], F32)
            load_row(grow[:], 'grow2', MODROW[l, m, 5120:6144])
            if final:
                fgrow = sb('fgrow', [128, D], F32)
                load_row(fgrow[:], 'fgrow', W['final_g'][0])
                fss = Pool_(ps, nc, 'fss', [128, 2], F32, 2)
            tmpt = sb('tmpf', [128, 512], F32)
            fe = FrontEnd(ps, nblk, inplace=True, junk=tmpt[:].bitcast(BF16))
            xhp = Pool_(ps, nc, 'xh', [128, nblk, D], BF16, 1)
            xcp = Pool_(ps, nc, 'xc', [128, 2, D], F32, 2)
            uTp = Pool_(ps, nc, 'uT', [128, 8, NTOK], BF16, 2)
            h2p = Pool_(ps, nc, 'h2', [128, NFC, CEN], BF16, 2)
            gsp = Pool_(ps, nc, 'gs', [128, NTOK], F32, 2)
            acp = Pool_(ps, nc, 'cacc', [128, CEN], F32, 3)
            pap = Pool_(ps, nc, 'pa_f', [128, CEN], F32, 3, psum=True)
            pgp = Pool_(ps, nc, 'pg_f', [128, NTOK], F32, 2, psum=True)
            pop = Pool_(ps, nc, 'po_f', [128, 512], F32, 1, psum=True)
            taps = [(kr, kc) for kr in range(3) for kc in range(3)] if lat else [(1, 0), (1, 1), (1, 2)]

            def load(ti):
                t0 = ti * CEN
                xh, xhk = xhp.next()
                xc, xck = xcp.next()
                ts_ = t0 - halo
                if lat and ti == 0:
                    P.memset('pool', xh[:, 0, :], 0.0, W=[xhk])
                    P.dma('pool', xh[64:128, 0, :], xin[0:64, :], W=[xhk])
                    P.dma('pool', xh[:, 1:3, :], xin[64:320, :].rearrange("(j p) d -> p j d", p=128), W=[xhk])
                elif lat and ti == ntile - 1:
                    P.memset('pool', xh[:, 2, :], 0.0, W=[xhk])
                    P.dma('pool', xh[0:64, 2, :], xin[Tn - 64:Tn, :], W=[xhk])
                    P.dma('pool', xh[:, 0:2, :], xin[ts_:ts_ + 256, :].rearrange("(j p) d -> p j d", p=128), W=[xhk])
                else:
                    P.dma('pool', xh[:], xin[ts_:ts_ + NTOK, :].rearrange("(j p) d -> p j d", p=128), W=[xhk])
                P.dma('sp', xc[:], xin[t0:t0 + CEN, :].rearrange("(j p) d -> p j d", p=128), W=[xck])
                return xh, xhk, xc, xck

            def front(ld):
                uT, uTk = uTp.next()
                fe.run(ld[0], ld[1], uT, uTk, l, 1, m)
                return uT, uTk

            def h2_op(pend):
                acc, acck, pa, pak, h2, h2k, fcc = pend
                P.act(acc[:], acc[:], AF.Silu, R=[acck], W=[acck])
                return pend

            def h2_fin(pend):
                acc, acck, pa, pak, h2, h2k, fcc = pend
                P.tt('dve', h2[:, fcc, :], acc[:], pa[:], ALU.mult, R=[acck, pak], W=[(h2k, fcc)])

            ld = load(0)
            fr = front(ld)
            for ti in range(ntile):
                t0 = ti * CEN
                xh, xhk, xc, xck = ld
                uT, uTk = fr
                if ti + 1 < ntile:
                    ld_n = load(ti + 1)
                h2, h2k = h2p.next()
                pend = None
                for fcc in range(NFC):
                    pa, pak = pap.next()
                    for k in range(8):
                        P.mm(pa[:], wup[:, k, fcc * 128:(fcc + 1) * 128], uT[:, k, halo:halo + CEN], k == 0, k == 7, R=['wup', uTk], W=[pak])
                    pg, pgk = pgp.next()
                    for k in range(8):
                        P.mm(pg[:], wup[:, k, FH + fcc * 128:FH + (fcc + 1) * 128], uT[:, k, :], k == 0, k == 7, R=['wup', uTk], W=[pgk])
                    gs, gsk = gsp.next()
                    P.copy('act', gs[:], pg[:], R=[pgk], W=[gsk])
                    if lat and ti == 0:
                        P.memset('pool', gs[:, 0:64], 0.0, W=[gsk])
                    if lat and ti == ntile - 1:
                        P.memset('pool', gs[:, NTOK - 64:NTOK], 0.0, W=[gsk])
                    gv = gs[:].rearrange("p (r c) -> p r c", c=gc)
                    acc, acck = acp.next()
                    av = acc[:].rearrange("p (r c) -> p r c", c=gc)
                    r0 = 1 if lat else 0
                    P.ts('dve', av, gv[:, r0:r0 + R_, :], cw[:, l, 4, fcc:fcc + 1], cb[:, l, fcc:fcc + 1], ALU.mult, ALU.add,
                         R=[gsk, 'cw'], W=[acck])
                    for (kr, kc) in taps:
                        if (kr, kc) == (1, 1):
                            continue
                        rr = r0 + kr - 1
                        clo = 1 if kc == 0 else 0
                        chi = gc - 1 if kc == 2 else gc
                        P.stt(av[:, :, clo:chi], gv[:, rr:rr + R_, clo + kc - 1:chi + kc - 1], cw[:, l, kr * 3 + kc, fcc:fcc + 1],
                              av[:, :, clo:chi], ALU.mult, ALU.add, R=[gsk, 'cw', acck], W=[acck])
                    cur = h2_op((acc, acck, pa, pak, h2, h2k, fcc))
                    if pend is not None:
                        h2_fin(pend)
                    pend = cur
                    if fcc == 12 and ti + 1 < ntile:
                        fr_n = front(ld_n)
                h2_fin(pend)
                if final:
                    ss, ssk = fss.next()
                for tb in range(2):
                    for dh in range(2):
                        po, pok = pop.next()
                        for fcc in range(NFC):
                            P.mm(po[:], h2[:, fcc, tb * 128:(tb + 1) * 128], wdn[:, fcc, dh * 512:(dh + 1) * 512], fcc == 0, fcc == NFC - 1,
                                 R=[(h2k, fcc), 'wdn'], W=[pok])
                        tm, tmk = tmpt, 'fe_junk'
                        dsl = slice(dh * 512, (dh + 1) * 512)
                        P.tt('dve', tm[:], po[:], grow[:, dsl], ALU.mult, R=[pok, 'grow2'], W=[tmk])
                        P.tt('pool', xc[:, tb, dsl], tm[:], xc[:, tb, dsl], ALU.add, R=[tmk, xck], W=[xck])
                    if final:
                        P.act(fe.junk_ap, xc[:, tb, :], AF.Square, R=[xck], W=['fe_junk', ssk], accum_out=ss[:, tb:tb + 1])
                if final:
                    P.ts('dve', ss[:], ss[:], 1.0 / D, EPS, ALU.mult, ALU.add, R=[ssk], W=[ssk])
                    P.act(ss[:], ss[:], AF.Sqrt, R=[ssk], W=[ssk])
                    P.recip(ss[:], ss[:], R=[ssk], W=[ssk])
                    for tb in range(2):
                        P.stt(xc[:, tb, :], xc[:, tb, :], ss[:, tb:tb + 1], fgrow[:], ALU.mult, ALU.mult, R=[xck, ssk, 'fgrow'], W=[xck])
                P.dma('sp', xout[t0:t0 + CEN, :].rearrange("(j p) d -> p j d", p=128), xc[:], R=[xck], W=[P.uq('xout')])
                if ti + 1 < ntile:
                    ld, fr = ld_n, fr_n
            P.flush()

    def phase_kc(zsrc, ydst, rndst):
        with ExitStack() as ps:
            sb = lambda n, s, d: ps.enter_context(nc.sbuf_tensor(n, s, d))
            h3c = sb('h3c', [128, 512], F32)
            with ExitStack() as ps2:
                mlp = FilterMLP(ps2)
                mlp.run(K['posC'], 0, 512, h3c[:, 0:512], 'h3c')
                P.flush()
            wo = sb('wo_f', [128, 2, D], F32)
            P.memset('pool', wo[:], 0.0, W=['wo_f'])
            P.dma('sp', wo[0:64, 0, :], W['hy_wo_st'][0:64, :], W=['wo_f'])
            P.dma('sp', wo[64:128, 1, :], W['hy_wo_st'][64:128, :], W=['wo_f'])
            trow = sb('trow', [128, 511], F32)
            P.dma('sp', trow[:], K['tC'][0].partition_broadcast(128), W=['trow'])
            nd = sb('ndelta', [128, 8], F32)
            P.dma('sp', nd[:], K['ndelta'], W=['ndelta'])
            zcp = Pool_(ps, nc, 'zc', [128, TC], F32, 2)
            zbp = Pool_(ps, nc, 'zb', [128, TC], BF16, 2)
            decp = Pool_(ps, nc, 'decc', [128, 511], F32, 2)
            KLp = Pool_(ps, nc, 'KL', [128, 511], F32, 2)
            accp = Pool_(ps, nc, 'accc', [128, TC], F32, 2)
            nrm = sb('nrmc', [128, 8], F32)
            pkc = Pool_(ps, nc, 'pkc', [128, 512], F32, 2, psum=True)
            for k in range(8):
                pk, pkk = pkc.next()
                P.mm(pk[:, 0:256], wo[:, 1, k * 128:(k + 1) * 128], h3c[:, 0:256], True, True, R=['wo_f', 'h3c'], W=[pkk])
                P.mm(pk[:, 255:511], wo[:, 0, k * 128:(k + 1) * 128], h3c[:, 255:511], True, True, R=['wo_f', 'h3c'], W=[pkk])
                dec, deck = decp.next()
                P.act(dec[:], trow[:], AF.Exp, R=['trow', 'ndelta'], W=[deck], scale=nd[:, k:k + 1])
                KL, KLk = KLp.next()
                P.tt('dve', KL[:], pk[:, 0:511], dec[:], ALU.mult, R=[pkk, deck], W=[KLk])
                P.op('dve', lambda e, o=nrm[:, k:k + 1], i_=KL[:]: e.tensor_reduce(out=o, in_=i_, axis=AX.X, op=ALU.add, apply_absolute_value=True),
                     R=[KLk], W=[('nrmc', k)])
                zb, zbk = zbp.next()
                P.dma('sp', zb[:], zsrc[k * 128:(k + 1) * 128, :], W=[zbk])
                zc, zck = zcp.next()
                P.copy('pool', zc[:], zb[:], R=[zbk], W=[zck])
                acc, acck = accp.next()
                P.ts('dve', acc[:], KL[:, 255:511], zc[:, 0:1], None, ALU.mult, None, R=[KLk, zck], W=[acck])
                for s_ in range(1, TC):
                    P.stt(acc[:], KL[:, 255 - s_:511 - s_], zc[:, s_:s_ + 1], acc[:], ALU.mult, ALU.add, R=[KLk, zck, acck], W=[acck])
                P.dma('sp', ydst[k * 128:(k + 1) * 128, :], acc[:], R=[acck], W=[P.uq('ydst')])
            P.recip(nrm[:], nrm[:], R=[('nrmc', k) for k in range(8)], W=['nrmc'])
            P.dma('sp', rndst, nrm[:], R=['nrmc'], W=[P.uq('rndst')])
            P.flush()

    def phase_m1(src, Tn, xmdst, szdst):
        lat = Tn == T
        nblk = 4 if lat else Tn // 128
        TT = nblk * 128
        noc = 32 if lat else 16
        with ExitStack() as ps:
            sb = lambda n, s, d: ps.enter_context(nc.sbuf_tensor(n, s, d))
            win = sb('mwin', [128, 8, 2 * MI], BF16)
            fe = FrontEnd(ps, nblk)
            xtp = Pool_(ps, nc, 'mxt', [128, nblk, D], F32, 2)
            hTp = Pool_(ps, nc, 'mhT', [128, 8, TT], BF16, 2)
            pbp = Pool_(ps, nc, 'mpb', [128, 8, TT], BF16, 2)
            pp = Pool_(ps, nc, 'mpp', [128, TT], F32, 4, psum=True)
            for k in range(8):
                P.dma('pool', win[:, k, :], W['ml_w_in'][k * 128:(k + 1) * 128, :], W=['mwin'])
            m = 0 if lat else 1

            def load(ti):
                xt, xtk = xtp.next()
                P.dma('sp', xt[:], src[ti * TT:(ti + 1) * TT, :].rearrange("(j p) d -> p j d", p=128), W=[xtk])
                return xt, xtk
            ntile = Tn // TT
            ld = load(0)
            for ti in range(ntile):
                t0 = ti * TT
                xt, xtk = ld
                if ti + 1 < ntile:
                    ld = load(ti + 1)
                hT, hTk = hTp.next()
                fe.run(xt, xtk, hT, hTk, 1, 0, m)
                for og in range(noc // 8):
                    pb, pbk = pbp.next()
                    for oi in range(8):
                        oc = og * 8 + oi
                        pt, ptk = pp.next()
                        for k in range(8):
                            P.mm(pt[:], win[:, k, oc * 128:(oc + 1) * 128], hT[:, k, :], k == 0, k == 7, R=['mwin', hTk], W=[ptk])
                        if oc >= 16:
                            P.act(pb[:, oi, :], pt[:], AF.Silu, R=[ptk], W=[pbk])
                        elif oi % 2 == 0:
                            P.copy('act', pb[:, oi, :], pt[:], R=[ptk], W=[pbk])
                        else:
                            P.copy('dve', pb[:, oi, :], pt[:], R=[ptk], W=[pbk])
                    if og < 2:
                        P.dma('sp', xmdst[og * 1024:(og + 1) * 1024, t0:t0 + TT].rearrange("(c p) t -> p c t", p=128), pb[:], R=[pbk], W=[P.uq('xmdst')])
                    else:
                        o2 = og - 2
                        P.dma('sp', szdst[o2 * 1024:(o2 + 1) * 1024, t0:t0 + TT].rearrange("(c p) t -> p c t", p=128), pb[:], R=[pbk], W=[P.uq('szdst')])
            P.flush()

    def phase_m2(xmsrc, Tn, qdst, kdst, ktdst, vtdst, sxdst, gpdst):
        lat = Tn == T
        TT = 512 if lat else Tn
        nblk = TT // 128
        DHS = 512.0 ** -0.5
        with ExitStack() as ps:
            sb = lambda n, s, d: ps.enter_context(nc.sbuf_tensor(n, s, d))
            bd = sb('bd', [128, 3, 16, 128], BF16)
            for j, nm in enumerate(('ml_bdq', 'ml_bdk', 'ml_bdv')):
                P.dma('pool', bd[:, j, :, :], W[nm].rearrange("c p m -> p c m"), W=['bd'])
            wg = sb('wg', [128, 48, 16], BF16)
            P.dma('pool', wg[:], W['ml_w_gate'].rearrange("(c p) n -> p c n", p=128), W=['wg'])
            bgrow = sb('bgrow', [128, 16], F32)
            load_row(bgrow[:], 'bgrow', W['ml_b_gate'])
            cwc = sb('mcw', [128, 3, 16], F32)
            cbc = sb('mcb', [128, 16], F32)
            skc = sb('mskip', [128, 16], F32)
            P.dma('sp', cwc[:], W['ml_conv_w_col'], W=['mcw'])
            P.dma('sp', cbc[:], W['ml_conv_b_col'], W=['mcw'])
            P.dma('sp', skc[:], W['ml_skip_col'], W=['mcw'])
            Um = sb('Um', [128, 3, 128], F32)
            P.dma('sp', Um[:, 0, :], K['U'], W=['Um'])
            P.dma('sp', Um[:, 1, :], K['UT'], W=['Um'])
            P.dma('sp', Um[:, 2, :], K['ones'], W=['Um'])
            xmp = Pool_(ps, nc, 'xmh', [128, 16, TT + 2], BF16, 2)
            xcp = Pool_(ps, nc, 'xcm', [128, 16, TT], BF16, 1)
            sxp = Pool_(ps, nc, 'sxc', [128, 16, TT], BF16, 1)
            accp = Pool_(ps, nc, 'macc', [128, TT], F32, 2)
            qp = Pool_(ps, nc, 'qfm', [128, 16, TT], BF16, 1)
            kp = Pool_(ps, nc, 'kfm', [128, 16, TT], BF16, 1)
            vp = Pool_(ps, nc, 'vfm', [128, 16, TT], BF16, 1)
            ktp = Pool_(ps, nc, 'ktm', [128, nblk, MI], BF16, 1)
            vtp = Pool_(ps, nc, 'vtm', [128, nblk, MI], BF16, 1)
            gpp = Pool_(ps, nc, 'gp', [128, nblk, 4, 8], F32, 2)
            gtp = Pool_(ps, nc, 'gates', [128, 16], F32, 2)
            spp = Pool_(ps, nc, 'spl', [128, 2, 4], F32, 2)
            t8p = Pool_(ps, nc, 'tmp8', [128, 8], F32, 2)
            pfm = Pool_(ps, nc, 'pfm', [128, TT], F32, 2, psum=True)
            ptm = Pool_(ps, nc, 'ptm', [128, 512], F32, 2, psum=True)
            pgp = Pool_(ps, nc, 'pgate', [128, 16], F32, 2, psum=True)
            pbp = Pool_(ps, nc, 'pbcum', [128, 2, 8], F32, 2, psum=True)
            def load(ti):
                t0 = ti * TT
                xm, xmk = xmp.next()
                lo = 1 if ti == 0 else 0
                hi = TT + 1 if ti == Tn // TT - 1 else TT + 2
                if ti == 0:
                    P.memset('pool', xm[:, :, 0:1], 0.0, W=[xmk])
                if ti == Tn // TT - 1:
                    P.memset('pool', xm[:, :, TT + 1:TT + 2], 0.0, W=[xmk])
                P.dma('sp', xm[:, :, lo:hi], xmsrc[:, t0 - 1 + lo:t0 - 1 + hi].rearrange("(c p) t -> p c t", p=128), W=[xmk])
                return xm, xmk
            ld = load(0)
            for ti in range(Tn // TT):
                t0 = ti * TT
                xm, xmk = ld
                if ti + 1 < Tn // TT:
                    ld = load(ti + 1)
                xc, xck = xcp.next()
                sx, sxk = sxp.next()
                for cc in range(16):
                    acc, acck = accp.next()
                    P.act(acc[:], xm[:, cc, 1:TT + 1], AF.Identity, R=[xmk, 'mcw'], W=[acck], scale=cwc[:, 1, cc:cc + 1], bias=cbc[:, cc:cc + 1])
                    P.stt(acc[:], xm[:, cc, 0:TT], cwc[:, 0, cc:cc + 1], acc[:], ALU.mult, ALU.add, R=[xmk, 'mcw', acck], W=[acck])
                    P.stt(acc[:], xm[:, cc, 2:TT + 2], cwc[:, 2, cc:cc + 1], acc[:], ALU.mult, ALU.add, R=[xmk, 'mcw', acck], W=[acck])
                    P.act(xc[:, cc, :], acc[:], AF.Silu, R=[acck], W=[(xck, cc)])
                    if lat:
                        P.ts('pool', sx[:, cc, :], xc[:, cc, :], skc[:, cc:cc + 1], None, ALU.mult, None, R=[(xck, cc), 'mcw'], W=[sxk])
                if lat:
                    P.dma('sp', sxdst[:, t0:t0 + TT].rearrange("(c p) t -> p c t", p=128), sx[:], R=[sxk], W=[P.uq('sxdst')])
                qf, qfk = qp.next()
                kf, kfk = kp.next()
                vf, vfk = vp.next()
                for cc in range(16):
                    for j, (dst_, dk, srct, srck) in enumerate(((qf, qfk, xc[:, cc, :], (xck, cc)), (kf, kfk, xc[:, cc, :], (xck, cc)),
                                                             (vf, vfk, xm[:, cc, 1:TT + 1], xmk))):
                        pf, pfk = pfm.next()
                        P.mm(pf[:], bd[:, j, cc, :], srct, True, True, R=['bd', srck], W=[pfk])
                        P.copy('act' if (cc + j) % 2 == 0 else 'dve', dst_[:, cc, :], pf[:], R=[pfk], W=[(dk, cc)])
                if lat:
                    P.dma('sp', qdst[:, t0:t0 + TT].rearrange("(c p) t -> p c t", p=128), qf[:], R=[(qfk, c_) for c_ in range(16)], W=[P.uq('qdst')])
                    P.dma('sp', kdst[:, t0:t0 + TT].rearrange("(c p) t -> p c t", p=128), kf[:], R=[(kfk, c_) for c_ in range(16)], W=[P.uq('kdst')])
                kt, ktk = ktp.next()
                vt, vtk = vtp.next()
                for blk in range(nblk):
                    bsl = slice(blk * 128, (blk + 1) * 128)
                    for j, (dst_, dk, which) in enumerate(((kt, ktk, 1), (vt, vtk, 2))):
                        for c4 in range(4):
                            pt, ptk = ptm.next()
                            for i in range(4):
                                cc = c4 * 4 + i
                                lhs = xc[:, cc, bsl] if which == 1 else xm[:, cc, 1 + blk * 128:1 + (blk + 1) * 128]
                                P.mm(pt[:, i * 128:(i + 1) * 128], lhs, bd[:, which, cc, :], True, True,
                                     R=['bd', (xck, cc) if which == 1 else xmk], W=[ptk])
                            P.copy('act' if (c4 + j) % 2 == 0 else 'dve', dst_[:, blk, c4 * 512:(c4 + 1) * 512], pt[:], R=[ptk], W=[dk])
                P.dma('sp', ktdst[t0:t0 + TT, :].rearrange("(j p) n -> p j n", p=128), kt[:], R=[ktk], W=[P.uq('ktdst')])
                P.dma('sp', vtdst[t0:t0 + TT, :].rearrange("(j p) n -> p j n", p=128), vt[:], R=[vtk], W=[P.uq('vtdst')])
                gp, gpk = gpp.next()
                for blk in range(nblk):
                    bsl = slice(blk * 128, (blk + 1) * 128)
                    pg, pgk = pgp.next()
                    n_ = 0
                    for j, (src_, sk) in enumerate(((qf, qfk), (kf, kfk), (vf, vfk))):
                        for cc in range(16):
                            P.mm(pg[:], src_[:, cc, bsl], wg[:, j * 16 + cc, :], n_ == 0, n_ == 47, R=[(sk, cc), 'wg'], W=[pgk])
                            n_ += 1
                    gt, gtk = gtp.next()
                    P.tt('dve', gt[:], pg[:], bgrow[:], ALU.add, R=[pgk, 'bgrow'], W=[gtk])
                    gv = gt[:].rearrange("p (d g h) -> p d g h", d=2, g=2)
                    sp_, spk = spp.next()
                    P.act(sp_[:], gv[:, :, 1, :], AF.Exp, R=[gtk], W=[spk], scale=-1.0)
                    P.act(sp_[:], sp_[:], AF.Ln, R=[spk], W=[spk], bias=1.0)
                    pb, pbk = pbp.next()
                    P.mm(pb[:, 0, 0:4], Um[:, 0, :], sp_[:, 0, :], True, True, R=['Um', spk], W=[pbk])
                    P.mm(pb[:, 0, 4:8], Um[:, 1, :], sp_[:, 1, :], True, True, R=['Um', spk], W=[pbk])
                    P.mm(pb[:, 1, :], Um[:, 2, :], sp_[:].rearrange("p d h -> p (d h)"), True, True, R=['Um', spk], W=[pbk])
                    P.act(gp[:, blk, 0:2, :], pb[:], AF.Exp, R=[pbk], W=[gpk], scale=-1.0)
                    t8, t8k = t8p.next()
                    P.tt('dve', t8[:].rearrange("p (d h) -> p d h", d=2), gv[:, :, 0, :], pb[:, 0, :].rearrange("p (d h) -> p d h", d=2), ALU.add,
                         R=[gtk, pbk], W=[t8k])
                    P.act(gp[:, blk, 2, :], t8[:], AF.Exp, R=[t8k], W=[gpk], bias=float(math.log(DHS)))
                    P.tt('dve', gp[:, blk, 3, :], gp[:, blk, 2, :], gp[:, blk, 1, :], ALU.mult, R=[gpk], W=[gpk])
                P.dma('sp', gpdst[t0:t0 + TT, :, :].rearrange("(j p) a b -> p j a b", p=128), gp[:], R=[gpk], W=[P.uq('gpdst')])
            P.flush()

    def phase_m3():
        with ExitStack() as ps:
            sb = lambda n, s, d: ps.enter_context(nc.sbuf_tensor(n, s, d))
            pdC = ps.enter_context(nc.psum_tensor('pdC', [128, 4, 512], F32))
            Ct = [sb(f'Ct{c}', [128, 4, 512], F32) for c in range(8)]
            Cb = [sb(f'Cb{c}', [128, 4, 512], BF16) for c in range(8)]
            nt = sb('nt', [128, 8, 4], F32)
            nb = sb('nb', [128, 8, 4, 2], BF16)
            maskf = sb('maskf', [128, 2, 128], F32)
            onesb = sb('onesb', [128, 2], BF16)
            P.dma('sp', maskf[:, 0, :], K['U'], W=['maskf'])
            P.dma('sp', maskf[:, 1, :], K['UT'], W=['maskf'])
            P.memset('dve', onesb[:], 1.0, W=['onesb'])
            for c in range(8):
                P.memset('pool', Ct[c][:], 0.0, W=[('Ct', c)])
                P.memset('dve', Cb[c][:], 0.0, W=[('Cb', c)])
            P.memset('dve', nt[:], 0.0, W=[('nt', c) for c in range(8)])
            P.memset('dve', nb[:], 0.0, W=[('nb', c) for c in range(8)])
            qTp = Pool_(ps, nc, 'qT', [128, 4, 128], BF16, 6)
            kTp = Pool_(ps, nc, 'kT', [128, 4, 128], BF16, 6)
            ktp = Pool_(ps, nc, 'ktm3', [128, 512], BF16, 6)
            vtp = Pool_(ps, nc, 'vtm3', [128, 512], BF16, 6)
            gpp = Pool_(ps, nc, 'gp3', [128, 4, 8], F32, 10)
            Stp = Pool_(ps, nc, 'St', [128, 128], BF16, 3)
            k2p = Pool_(ps, nc, 'k2', [128, 512], BF16, 3)
            hop = Pool_(ps, nc, 'hout', [128, 512], F32, 3)
            smp = Pool_(ps, nc, 'sm3', [128, 4], F32, 4)
            pmp = Pool_(ps, nc, 'pmisc', [128, 512], F32, 2, psum=True)
            pnp = Pool_(ps, nc, 'pnum', [128, 512], F32, 2, psum=True)

            gp_tiles = {}

            def get_gp(gpsrc, c):
                key = (id(gpsrc), c)
                if key not in gp_tiles:
                    if len(gp_tiles) >= 6:
                        gp_tiles.pop(next(iter(gp_tiles)))
                    gp, gpk = gpp.next()
                    P.dma('sp', gp[:], gpsrc[c * 128:(c + 1) * 128, :, :], W=[gpk])
                    gp_tiles[key] = (gp, gpk)
                return gp_tiles[key]

            def part_l(h, dr, c, srcs, full):
                ktsrc, vtsrc, gpsrc = srcs
                t0 = c * 128
                col = dr * 4 + h
                gp, gpk = get_gp(gpsrc, c)
                kt, ktk = ktp.next()
                vt, vtk = vtp.next()
                P.dma('sp', kt[:], ktsrc[t0:t0 + 128, h * 512:(h + 1) * 512], W=[ktk])
                P.dma('sp', vt[:], vtsrc[t0:t0 + 128, h * 512:(h + 1) * 512], W=[vtk])
                st = dict(h=h, dr=dr, c=c, gp=gp, gpk=gpk, kt=kt, ktk=ktk, vt=vt, vtk=vtk, col=col, full=full)
                if full:
                    qT, qTk = qTp.next()
                    kT, kTk = kTp.next()
                    P.dma('sp', qT[:], Qfm[h * 512:(h + 1) * 512, t0:t0 + 128].rearrange("(dc p) t -> p dc t", p=128), W=[qTk])
                    P.dma('sp', kT[:], Kfm[h * 512:(h + 1) * 512, t0:t0 + 128].rearrange("(dc p) t -> p dc t", p=128), W=[kTk])
                    st.update(qT=qT, qTk=qTk, kT=kT, kTk=kTk)
                return st

            def part_a(st):
                h, dr, c, col, full = st['h'], st['dr'], st['c'], st['col'], st['full']
                gp, gpk, kt, ktk = st['gp'], st['gpk'], st['kt'], st['ktk']
                pm, pmk = pmp.next()
                st.update(pm=pm, pmk=pmk)
                k2, k2k = k2p.next()
                P.ts('pool', k2[:], kt[:], gp[:, 3, col:col + 1], None, ALU.mult, None, R=[ktk, gpk], W=[k2k])
                st.update(k2=k2, k2k=k2k)
                if full:
                    qT, qTk, kT, kTk = st['qT'], st['qTk'], st['kT'], st['kTk']
                    pS, pSk = pm[:, 0:128], (pmk, 'S')
                    for dc in range(4):
                        P.mm(pS, kT[:, dc, :], qT[:, dc, :], dc == 0, dc == 3, R=[kTk, qTk], W=[pSk])
                    St, Stk = Stp.next()
                    P.stt(St[:], pS, gp[:, 2, col:col + 1], maskf[:, dr, :], ALU.mult, ALU.mult, R=[pSk, gpk, 'maskf'], W=[Stk])
                    st.update(St=St, Stk=Stk)
                return st

            def part_b(st):
                h, dr, c, col = st['h'], st['dr'], st['c'], st['col']
                ch = h * 2 + dr
                gp, gpk = st['gp'], st['gpk']
                t0 = c * 128
                if st['full']:
                    qT, qTk, St, Stk, vt, vtk = st['qT'], st['qTk'], st['St'], st['Stk'], st['vt'], st['vtk']
                    pn, pnk = pnp.next()
                    P.mm(pn[:], St[:], vt[:], True, False, R=[Stk, vtk], W=[pnk])
                    for dc in range(4):
                        P.mm(pn[:], qT[:, dc, :], Cb[ch][:, dc, :], False, dc == 3, R=[qTk, ('Cb', ch)], W=[pnk])
                    pd, pdk = st['pm'][:, 128:130], (st['pmk'], 'den')
                    P.mm(pd, St[:], onesb[:], True, False, R=[Stk, 'onesb'], W=[pdk])
                    for dc in range(4):
                        P.mm(pd, qT[:, dc, :], nb[:, ch, dc, :], False, dc == 3, R=[qTk, ('nb', ch)], W=[pdk])
                    sm, smk = smp.next()
                    P.act(sm[:, 0:1], st['pm'][:, 128:129], AF.Abs, R=[pdk, gpk], W=[smk], scale=gp[:, 0, col:col + 1])
                    P.ts('dve', sm[:, 0:1], sm[:, 0:1], 1.0, None, ALU.max, None, R=[smk], W=[smk])
                    P.recip(sm[:, 1:2], sm[:, 0:1], R=[smk], W=[smk])
                    P.tt('dve', sm[:, 2:3], sm[:, 1:2], gp[:, 0, col:col + 1], ALU.mult, R=[smk, gpk], W=[smk])
                    ho, hok = hop.next()
                    P.act(ho[:], pn[:], AF.Identity, R=[pnk, smk], W=[hok], scale=sm[:, 2:3])
                    P.dma('act', HFB[dr, t0:t0 + 128, h * 512:(h + 1) * 512], ho[:], R=[hok], W=[P.uq('HFB')])
                k2, k2k, vt, vtk = st['k2'], st['k2k'], st['vt'], st['vtk']
                for dc in range(4):
                    P.mm(pdC[:, dc, :], k2[:, dc * 128:(dc + 1) * 128], vt[:], True, True, R=[k2k, vtk], W=[('pdC', dc)])
                pdn, pdnk = st['pm'][:, 256:264].rearrange("p (a b) -> p a b", b=2), (st['pmk'], 'dn')
                for dc in range(4):
                    P.mm(pdn[:, dc, :], k2[:, dc * 128:(dc + 1) * 128], onesb[:], True, True, R=[k2k, 'onesb'], W=[pdnk])
                glc = gp[:, 1, col:col + 1]
                for dc in range(4):
                    P.stt(Ct[ch][:, dc, :], Ct[ch][:, dc, :], glc, pdC[:, dc, :], ALU.mult, ALU.add, R=[('Ct', ch), gpk, ('pdC', dc)], W=[('Ct', ch)])
                P.copy('act', Cb[ch][:], Ct[ch][:], R=[('Ct', ch)], W=[('Cb', ch)])
                P.stt(nt[:, ch, :], nt[:, ch, :], glc, pdn[:, :, 0], ALU.mult, ALU.add, R=[('nt', ch), gpk, pdnk], W=[('nt', ch)])
                P.copy('pool', nb[:, ch, :, 0], nt[:, ch, :], R=[('nt', ch)], W=[('nb', ch)])
                P.copy('pool', nb[:, ch, :, 1], nt[:, ch, :], R=[('nt', ch)], W=[('nb', ch)])

            sched = []
            for s_ in range(2):
                for h in range(4):
                    for dr in range(2):
                        sched.append((h, dr, s_ if dr == 0 else 1 - s_, (KtmC, VtmC, GPdC), False))
            for s_ in range(64):
                for h in range(4):
                    for dr in range(2):
                        sched.append((h, dr, s_ if dr == 0 else 63 - s_, (Ktm, Vtm, GPd), True))
            LOOK = 3
            loaded = []
            nxt = 0
            prev = None
            for i in range(len(sched)):
                while nxt < len(sched) and nxt <= i + LOOK:
                    loaded.append(part_l(*sched[nxt]))
                    nxt += 1
                cur = part_a(loaded.pop(0))
                if prev is not None:
                    part_b(prev)
                prev = cur
            part_b(prev)
            P.flush()

    def phase_m4(xin, xout):
        with ExitStack() as ps:
            sb = lambda n, s, d: ps.enter_context(nc.sbuf_tensor(n, s, d))
            wdn = sb('mwdn', [128, 16, D], BF16)
            P.dma('pool', wdn[:], W['ml_w_down'].rearrange("(c p) n -> p c n", p=128), W=['mwdn'])
            nwc = sb('nwc', [128, 16], F32)
            P.dma('sp', nwc[:], W['ml_norm_w_col'], W=['nwc'])
            grow = sb('grow4', [128, D], F32)
            load_row(grow[:], 'grow4', MODROW[1, 0, 2048:3072])
            hfp = Pool_(ps, nc, 'hf', [128, MI], F32, 2)
            hbp = Pool_(ps, nc, 'hb', [128, MI], F32, 2)
            hnp = Pool_(ps, nc, 'hn', [128, MI], BF16, 2)
            stp = Pool_(ps, nc, 'bst', [128, 4, 6], F32, 2)
            mvp = Pool_(ps, nc, 'bmv', [128, 4, 2], F32, 2)
            sxp = Pool_(ps, nc, 'sx4', [128, 16, 128], BF16, 2)
            szp = Pool_(ps, nc, 'sz4', [128, 16, 128], BF16, 2)
            m1p = Pool_(ps, nc, 'm14', [128, 128], F32, 3)
            mfp = Pool_(ps, nc, 'mfm', [128, 16, 128], BF16, 2)
            xtp = Pool_(ps, nc, 'xt4', [128, D], F32, 2)
            tmp = Pool_(ps, nc, 'tmp4', [128, 512], F32, 2)
            pTp = Pool_(ps, nc, 'pT4', [128, 4, 128], BF16, 2, psum=True)
            pop = Pool_(ps, nc, 'po4', [128, 512], F32, 2, psum=True)
            def load(blk):
                t0 = blk * 128
                hf, hfk = hfp.next()
                hb, hbk = hbp.next()
                sx, sxk = sxp.next()
                sz, szk = szp.next()
                xt, xtk = xtp.next()
                P.dma('sp', hf[:], HFB[0, t0:t0 + 128, :], W=[hfk])
                P.dma('sp', hb[:], HFB[1, t0:t0 + 128, :], W=[hbk])
                P.dma('sp', sx[:], SXC[:, t0:t0 + 128].rearrange("(c p) t -> p c t", p=128), W=[sxk])
                P.dma('sp', sz[:], SZd[:, t0:t0 + 128].rearrange("(c p) t -> p c t", p=128), W=[szk])
                P.dma('sp', xt[:], xin[t0:t0 + 128, :], W=[xtk])
                return hf, hfk, hb, hbk, sx, sxk, sz, szk, xt, xtk
            ld = load(0)
            for blk in range(T // 128):
                t0 = blk * 128
                hf, hfk, hb, hbk, sx, sxk, sz, szk, xt, xtk = ld
                if blk + 1 < T // 128:
                    ld = load(blk + 1)
                P.tt('pool', hf[:], hf[:], hb[:], ALU.add, R=[hfk, hbk], W=[hfk])
                bst, bstk = stp.next()
                mv, mvk = mvp.next()
                for h in range(4):
                    P.op('dve', lambda e, o=bst[:, h, :], i_=hf[:, h * 512:(h + 1) * 512]: e.bn_stats(out=o, in_=i_), R=[hfk], W=[(bstk, h)])
                    P.op('dve', lambda e, o=mv[:, h, :], i_=bst[:, h, :]: e.bn_aggr(out=o, in_=i_), R=[(bstk, h)], W=[(mvk, h)])
                mvall = [(mvk, h) for h in range(4)]
                P.ts('dve', mv[:, :, 1], mv[:, :, 1], 1e-5, None, ALU.add, None, R=mvall, W=mvall)
                P.act(mv[:, :, 1], mv[:, :, 1], AF.Sqrt, R=mvall, W=mvall)
                P.recip(mv[:, :, 1], mv[:, :, 1], R=mvall, W=mvall)
                hn, hnk = hnp.next()
                for h in range(4):
                    P.ts('dve', hn[:, h * 512:(h + 1) * 512], hf[:, h * 512:(h + 1) * 512], mv[:, h, 0:1], mv[:, h, 1:2], ALU.subtract, ALU.mult,
                         R=[hfk] + mvall, W=[(hnk, h)])
                mf, mfk = mfp.next()
                for c4 in range(4):
                    pT, pTk = pTp.next()
                    for i in range(4):
                        cc = c4 * 4 + i
                        P.tr(pT[:, i, :], hn[:, cc * 128:(cc + 1) * 128], identb[:], R=[(hnk, cc // 4), 'identb'], W=[pTk])
                    for i in range(4):
                        cc = c4 * 4 + i
                        m1, m1k = m1p.next()
                        P.stt(m1[:], pT[:, i, :], nwc[:, cc:cc + 1], sx[:, cc, :], ALU.mult, ALU.add, R=[pTk, 'nwc', sxk], W=[m1k])
                        P.tt('pool', mf[:, cc, :], m1[:], sz[:, cc, :], ALU.mult, R=[m1k, szk], W=[(mfk, cc)])
                for dh in range(2):
                    po, pok = pop.next()
                    for cc in range(16):
                        P.mm(po[:], mf[:, cc, :], wdn[:, cc, dh * 512:(dh + 1) * 512], cc == 0, cc == 15, R=[(mfk, cc), 'mwdn'], W=[pok])
                    tm, tmk = tmp.next()
                    dsl = slice(dh * 512, (dh + 1) * 512)
                    P.tt('dve', tm[:], po[:], grow[:, dsl], ALU.mult, R=[pok, 'grow4'], W=[tmk])
                    P.tt('pool', xt[:, dsl], tm[:], xt[:, dsl], ALU.add, R=[tmk, xtk], W=[xtk])
                P.dma('sp', xout[t0:t0 + 128, :], xt[:], R=[xtk], W=[P.uq('xout')])
            P.flush()
    only = stop_after
    run = lambda name: (only is None) or (name in only)
    phase_adaln()
    if run('lat0'):
        phase_h1(x_d, T, P0)
        phase_h2(P0, T, Zd, X0d)
        phase_k()
        phase_z()
        phase_h5(Ytd, Zd, X0d, x_d, XA0, T, 0, RNd)
        phase_ffn(0, XA0, XB0, T, 0)
    if run('ctx0') or run('c1'):
        phase_h1(ctx_d, TC, P0C)
    if run('ctx0') or run('c2'):
        phase_h2(P0C, TC, ZdC, X0dC)
    if run('ctx0') or run('c3'):
        phase_kc(ZdC, YtC, RNdC)
    if run('ctx0') or run('c4'):
        phase_h5(YtC, ZdC, X0dC, ctx_d, CA0, TC, 1, RNdC)
    if run('ctx0') or run('c5'):
        phase_ffn(0, CA0, CB0, TC, 1)
    if run('m1'):
        phase_m1(XB0, T, XMd, SZd)
        phase_m1(CB0, TC, XMC, None)
    if run('m2'):
        phase_m2(XMd, T, Qfm, Kfm, Ktm, Vtm, SXC, GPd)
        phase_m2(XMC, TC, None, None, KtmC, VtmC, None, GPdC)
    if run('m3'):
        phase_m3()
    if run('m4'):
        phase_m4(XB0, XA1)
    if run('f1'):
        phase_ffn(1, XA1, out_d, T, 0, final=True)
    return nc_real, cx


def _blockdiag(w):
    out = np.zeros((16, 128, 128), dtype=np.float32)
    for ch in range(16):
        for n in range(32):
            out[ch, 4 * n:4 * n + 4, 4 * n:4 * n + 4] = w[32 * ch + n]
    return out


def prep_shared(inputs):
    f = lambda k: np.asarray(inputs[k], dtype=np.float32)
    S = {}
    for k in ('mod_w', 'mod_b', 'norm_g', 'ffn_w_up', 'ffn_conv_w', 'ffn_conv_b', 'ffn_w_down'):
        S[k] = f(k)
    S['final_g'] = f('final_g').reshape(1, D)
    for k in ('hy_w_in', 'hy_b_in', 'hy_sc_w', 'hy_sc_b', 'hy_bias', 'hy_w_out', 'hy_b_out', 'ml_w_in', 'ml_conv_w', 'ml_conv_b',
              'ml_w_gate', 'ml_b_gate', 'ml_norm_w', 'ml_skip', 'ml_w_down'):
        S[k] = f(k)[0]
    w1, w2, w3 = f('hy_f_w1')[0], f('hy_f_w2')[0], f('hy_f_w3')[0]
    S['hy_w1d'] = np.concatenate([w1, w1], axis=1)
    z = np.zeros((64, 64), np.float32)
    S['hy_w2bd'] = np.block([[w2, z], [z, w2]])
    S['hy_w3bd'] = np.block([[w3, z], [z, w3]])
    dup = lambda v: np.concatenate([v, v]).reshape(128, 1)
    S['hy_b1d'] = dup(f('hy_f_b1')[0])
    S['hy_b2d'] = dup(f('hy_f_b2')[0])
    S['hy_b3d'] = dup(f('hy_f_b3')[0])
    S['hy_freqd'] = dup(f('hy_freq')[0])
    wo = f('hy_f_wout')[0]
    S['hy_wo_st'] = np.concatenate([wo[:, :D], wo[:, D:]], axis=0)
    def col(v):
        v = np.asarray(v, dtype=np.float32)
        n = v.shape[-1] // 128
        v = v.reshape(v.shape[:-1] + (n, 128))
        return np.moveaxis(v, -1, 0)
    S['mod_b_col'] = col(S['mod_b'])
    S['norm_g_col'] = col(S['norm_g'])
    S['hy_b_in_col'] = col(S['hy_b_in'])
    S['hy_sc_w_col'] = col(S['hy_sc_w'])
    S['hy_sc_b_col'] = col(S['hy_sc_b'])
    S['hy_bias_col'] = col(S['hy_bias'])
    S['ml_conv_w_col'] = col(S['ml_conv_w'])
    S['ml_conv_b_col'] = col(S['ml_conv_b'])
    S['ml_norm_w_col'] = col(S['ml_norm_w'])
    S['ml_skip_col'] = col(S['ml_skip'])
    S['ffn_conv_w_col'] = col(S['ffn_conv_w'].reshape(2, 9, FH))
    S['ffn_conv_b_col'] = col(S['ffn_conv_b'])
    S['ml_bdq'] = _blockdiag(f('ml_wq')[0])
    S['ml_bdk'] = _blockdiag(f('ml_wk')[0])
    S['ml_bdv'] = _blockdiag(f('ml_wv')[0])
    return {k: np.ascontiguousarray(v, dtype=np.float32) for k, v in S.items()}


def prep_core(inputs, S, b):
    d = dict(S)
    d['x'] = np.ascontiguousarray(inputs['x'][b], dtype=np.float32)
    d['ctx'] = np.ascontiguousarray(inputs['ctx'][b], dtype=np.float32)
    d['cvec'] = np.ascontiguousarray(np.stack([inputs['c'][b], inputs['c_ctx']], axis=1), dtype=np.float32)
    return d


def kernel(**inputs):
    S = prep_shared(inputs)
    cores = [prep_core(inputs, S, b % 4) for b in range(8)]
    nc, cx = build(cores[0])
    HC = host_consts()
    in_maps = []
    for c in cores:
        m = dict(c)
        for k, v in HC.items():
            m['c_' + k] = v
        in_maps.append(m)
    res = run_bass_kernel_spmd(nc, in_maps, core_ids=list(range(8)))
    out = np.stack([np.asarray(res.results[b]['out'], dtype=np.float32) for b in range(4)], axis=0)
    return out
```

```python
import math
import numpy as np
import concourse.bass as bass
import concourse.mybir as mybir
from concourse.bass_utils import run_bass_kernel_spmd
from contextlib import ExitStack

F32 = mybir.dt.float32
BF16 = mybir.dt.bfloat16
ALU = mybir.AluOpType
AF = mybir.ActivationFunctionType
AX = mybir.AxisListType

CENG = ('pe', 'dve', 'act', 'pool')
DQ = ('sp', 'pool', 'act')

T = 8192
D = 1024
TC = 256
FH = 2816
NFC = 22
MI = 2048
EPS = 1e-6
MAGIC = 12582912.0
TWO_PI = 2.0 * math.pi


class Prog:
    NDS = 6
    SAME_ENGINE_RELAX = True
    LONG = 200

    def __init__(self, nc, es):
        self.toklen = {}
        self.nc = nc
        self.eng = dict(pe=nc.tensor, dve=nc.vector, act=nc.scalar, pool=nc.gpsimd, sp=nc.sync)
        self.streams = {e: [] for e in self.eng}
        self.ninstr = {e: 0 for e in self.eng}
        self.csem = {e: es.enter_context(nc.semaphore(f"c{e}")) for e in CENG}
        self.ccount = {e: 0 for e in CENG}
        self.dsem = {q: [es.enter_context(nc.semaphore(f"d{q}_{i}")) for i in range(self.NDS)] for q in DQ}
        self.dcount = {q: 0 for q in DQ}
        self.seen = {e: {} for e in self.eng}
        self.lastw = {}
        self.readers = {}

    def _deps(self, R, W):
        deps = []
        for k in R:
            t = self.lastw.get(k)
            if t is not None:
                deps.append((t, 'raw'))
        for k in W:
            t = self.lastw.get(k)
            if t is not None:
                deps.append((t, 'waw'))
            deps.extend((r, 'war') for r in self.readers.get(k, ()))
        return deps

    def _emit_waits(self, e, deps, skip_pe_pe=False):
        seen = self.seen[e]
        need = {}
        for t, kind in deps:
            if t[0] == 'c':
                _, pe_, n = t
                if pe_ == e:
                    if skip_pe_pe and e == 'pe':
                        continue
                    if self.SAME_ENGINE_RELAX and (kind != 'raw' or self.toklen.get((pe_, n), 0) >= self.LONG):
                        continue
                key = ('c', pe_)
                val = n
            else:
                _, q, slot, v = t
                key = ('d', q, slot)
                val = v
            if seen.get(key, 0) >= val:
                continue
            if need.get(key, 0) < val:
                need[key] = val
        for key, val in need.items():
            seen[key] = val
            if key[0] == 'c':
                sem = self.csem[key[1]]
                self.streams[e].append(lambda eng, sem=sem, val=val: eng.wait_ge(sem, val))
            else:
                sem = self.dsem[key[1]][key[2]]
                self.streams[e].append(lambda eng, sem=sem, val=val: eng.wait_ge(sem, 16 * val))

    def _record(self, tok, R, W):
        for k in W:
            self.lastw[k] = tok
            self.readers[k] = []
        for k in R:
            self.readers.setdefault(k, []).append(tok)

    def op(self, e, fn, R=(), W=(), ln=0):
        deps = self._deps(R, W)
        self._emit_waits(e, deps, skip_pe_pe=True)
        self.ccount[e] += 1
        n = self.ccount[e]
        self.toklen[(e, n)] = ln
        sem = self.csem[e]
        self.streams[e].append(lambda eng, fn=fn, sem=sem: fn(eng).then_inc(sem, 1))
        self._record(('c', e, n), R, W)
        self.ninstr[e] += 1

    def dma(self, q, out, in_, R=(), W=(), **kw):
        deps = self._deps(R, W)
        self._emit_waits(q, deps)
        i = self.dcount[q]
        self.dcount[q] += 1
        slot = i % self.NDS
        v = i // self.NDS + 1
        sem = self.dsem[q][slot]
        self.streams[q].append(lambda eng, out=out, in_=in_, sem=sem, kw=kw: eng.dma_start(out=out, in_=in_, **kw).then_inc(sem, 16))
        self._record(('d', q, slot, v), R, W)
        self.ninstr[q] += 1

    def uq(self, name):
        self._uq = getattr(self, '_uq', 0) + 1
        return (name, 'u', self._uq)

    def barrier(self):
        toks = []
        for e in CENG:
            if self.ccount[e] > 0:
                toks.append(('c', e, self.ccount[e]))
        for q in DQ:
            n = self.dcount[q]
            for slot in range(self.NDS):
                if n > slot:
                    toks.append(('d', q, slot, (n - 1 - slot) // self.NDS + 1))
        for e in self.eng:
            self._emit_waits(e, [(t, 'bar') for t in toks])
        self.lastw = {}
        self.readers = {}

    def flush(self):
        self.barrier()
        nc = self.nc
        st = self.streams
        with nc.Block() as block:
            @block.tensor
            def _(eng):
                for f in st['pe']:
                    f(eng)

            @block.vector
            def _(eng):
                for f in st['dve']:
                    f(eng)

            @block.scalar
            def _(eng):
                for f in st['act']:
                    f(eng)

            @block.gpsimd
            def _(eng):
                for f in st['pool']:
                    f(eng)

            @block.sync
            def _(eng):
                for f in st['sp']:
                    f(eng)
        self.streams = {e: [] for e in self.eng}

    def mm(self, out, lhsT, rhs, start, stop, R, W):
        self.op('pe', lambda e: e.matmul(out=out, lhsT=lhsT, rhs=rhs, start=start, stop=stop), R, W)

    def tr(self, out, in_, ident, R, W):
        self.op('pe', lambda e: e.transpose(out=out, in_=in_, identity=ident), R, W)

    @staticmethod
    def _ln(ap):
        n = 1
        for d in ap.shape[1:]:
            n *= int(d)
        return n

    def act(self, out, in_, func, R, W, scale=None, bias=None, accum_out=None):
        kw = {}
        if scale is not None:
            kw['scale'] = scale
        if bias is not None:
            kw['bias'] = bias
        if accum_out is not None:
            kw['accum_out'] = accum_out
        self.op('act', lambda e: e.activation(out=out, in_=in_, func=func, **kw), R, W, ln=0 if accum_out is not None else self._ln(out))

    def ts(self, eng, out, in0, s1, s2, op0, op1, R, W):
        if op1 is None:
            self.op(eng, lambda e: e.tensor_scalar(out=out, in0=in0, scalar1=s1, scalar2=None, op0=op0), R, W, ln=self._ln(out))
        else:
            self.op(eng, lambda e: e.tensor_scalar(out=out, in0=in0, scalar1=s1, scalar2=s2, op0=op0, op1=op1), R, W, ln=self._ln(out))

    def tt(self, eng, out, in0, in1, op, R, W):
        self.op(eng, lambda e: e.tensor_tensor(out=out, in0=in0, in1=in1, op=op), R, W, ln=self._ln(out))

    def stt(self, out, in0, scalar, in1, op0, op1, R, W):
        self.op('dve', lambda e: e.scalar_tensor_tensor(out=out, in0=in0, scalar=scalar, in1=in1, op0=op0, op1=op1), R, W, ln=self._ln(out))

    def copy(self, eng, out, in_, R, W):
        if eng == 'act':
            self.op('act', lambda e: e.copy(out=out, in_=in_), R, W, ln=self._ln(out))
        else:
            self.op(eng, lambda e: e.tensor_copy(out=out, in_=in_), R, W, ln=self._ln(out))

    def memset(self, eng, ap, val, W):
        self.op(eng, lambda e: e.memset(ap, val), (), W, ln=self._ln(ap))

    def recip(self, out, in_, R, W):
        self.op('dve', lambda e: e.reciprocal(out=out, in_=in_), R, W)


class Ctx:
    def __init__(self, nc, debug):
        self.nc = nc
        self.debug = debug
        self.inputs = {}
        self.feed = None
        self.dbg_names = []

    def din(self, name, arr):
        arr = np.ascontiguousarray(arr, dtype=np.float32)
        self.inputs[name] = arr
        return self.nc.dram_tensor(name, list(arr.shape), F32, kind="ExternalInput").ap()

    def dscr(self, name, shape, dt):
        if self.feed and name in self.feed:
            return self.nc.dram_tensor(name, list(shape), dt, kind="ExternalInput").ap()
        if self.debug and name in self.debug:
            self.dbg_names.append(name)
            return self.nc.dram_tensor(name, list(shape), dt, kind="ExternalOutput").ap()
        return self.nc.dram_tensor(name, list(shape), dt, kind="Internal").ap()


class Pool_:
    def __init__(self, es, nc, name, shape, dt, n, psum=False):
        mk = nc.psum_tensor if psum else nc.sbuf_tensor
        self.tiles = [es.enter_context(mk(f"{name}{i}", shape, dt)) for i in range(n)]
        self.name = name
        self.i = -1

    def next(self):
        self.i = (self.i + 1) % len(self.tiles)
        return self.tiles[self.i], (self.name, self.i)


def _pos_feats(idx, L):
    idx = np.asarray(idx, dtype=np.float64)
    t = (idx / (L - 1)).astype(np.float32).astype(np.float64)
    w = (2.0 * math.pi * idx.astype(np.float32) / np.float32(L)).astype(np.float64)
    bands = np.linspace(1e-4, 15.0, 16, dtype=np.float32).astype(np.float64)
    arg = bands[None, :] * w[:, None]
    return np.concatenate([t[:, None], np.cos(arg), -np.sin(arg)], axis=1).astype(np.float32)


def host_consts():
    C = {}
    C['ident'] = np.eye(128, dtype=np.float32)
    n = np.arange(128)
    k1 = np.arange(65)
    ang = 2 * np.pi * np.outer(n, k1) / 128.0
    C['FA'] = np.concatenate([np.cos(ang), -np.sin(ang[:, 1:64])], axis=1)
    n2 = n[:, None, None]
    kk = (k1[None, :, None] + 128 * n[None, None, :])
    th = 2 * np.pi * (n2 * kk % 16384) / 16384.0
    C['TC'] = np.stack([np.cos(th), np.sin(th)], axis=2)
    ph = 2 * np.pi * np.outer(n, n) / 128.0
    C['IR'] = np.stack([np.concatenate([np.cos(ph), np.sin(ph)], 1), np.concatenate([-np.sin(ph), np.cos(ph)], 1)], axis=1)
    wk = np.where((k1 == 0) | (k1 == 64), 1.0, 2.0) / 16384.0
    tw = 2 * np.pi * np.outer(k1, n) / 16384.0
    C['TWI'] = np.stack([wk[:, None] * np.cos(tw), wk[:, None] * np.sin(tw)], axis=1)
    n1 = np.arange(64)
    g = 2 * np.pi * np.outer(k1, n1) / 128.0
    C['G'] = np.stack([np.cos(g), -np.sin(g)], axis=1)
    N2, N1 = np.meshgrid(np.arange(128), np.arange(128), indexing='ij')
    fwd = N1 < 64
    idx = np.where(fwd, 128 * N1 + N2, 16384 - 128 * N1 - N2)
    idx = np.where(idx >= T, 0, idx)
    C['posR'] = _pos_feats(idx.reshape(-1), T).T.copy()
    deltas = np.abs(np.linspace(math.log(1e-2) / 1.5, math.log(1e-2) / 0.3, D, dtype=np.float32)).astype(np.float64)
    C['absdelta'] = deltas
    a_n1 = np.where(np.arange(128) < 64, 128.0 * np.arange(128), 16384.0 - 128.0 * np.arange(128)) / (T - 1)
    C['decL'] = np.stack([a_n1, (np.arange(128) < 64).astype(np.float64), (np.arange(128) >= 64).astype(np.float64)], axis=0)
    r0 = np.broadcast_to(-deltas[None, :], (128, D))
    r1 = -np.outer(np.arange(128) / (T - 1), deltas)
    C['decR'] = np.stack([r0, r1, -r1], axis=0)
    dch = np.concatenate([np.arange(255, 0, -1), np.arange(0, 256)])
    C['posC'] = _pos_feats(np.concatenate([dch, [0]]), TC).T.copy()
    C['tC'] = (dch / (TC - 1.0)).astype(np.float32)[None, :]
    C['ndelta'] = (-deltas).reshape(8, 128).T.copy()
    i = np.arange(128)
    C['U'] = (i[:, None] <= i[None, :]).astype(np.float32)
    C['UT'] = (i[:, None] >= i[None, :]).astype(np.float32)
    C['ones'] = np.ones((128, 128), dtype=np.float32)
    return {k: np.ascontiguousarray(v, dtype=np.float32) for k, v in C.items()}


class _NC:
    def __init__(self, nc):
        self._nc = nc
        self._uid = 0

    def __getattr__(self, k):
        return getattr(self._nc, k)

    def sbuf_tensor(self, name, shape, dt):
        self._uid += 1
        return self._nc.sbuf_tensor(f"{name}_{self._uid}", shape, dt)

    def psum_tensor(self, name, shape, dt):
        self._uid += 1
        return self._nc.psum_tensor(f"{name}_{self._uid}", shape, dt)


def build(inp, debug=False, stop_after=None, feed=None):
    nc_real = bass.Bass("TRN2", target_bir_lowering=False)
    nc = _NC(nc_real)
    cx = Ctx(nc, debug)
    cx.feed = feed
    HC = host_consts()
    es = ExitStack()
    P = Prog(nc, es)

    def sbp(name, shape, dt):
        return es.enter_context(nc.sbuf_tensor(name, shape, dt))

    x_d = cx.din('x', inp['x'])
    ctx_d = cx.din('ctx', inp['ctx'])
    cvec_d = cx.din('cvec', inp['cvec'])
    W = {}
    for k in ('mod_w', 'mod_b', 'norm_g', 'final_g', 'hy_w_in', 'hy_b_in', 'hy_sc_w', 'hy_sc_b', 'hy_w1d', 'hy_b1d',
              'hy_w2bd', 'hy_b2d', 'hy_w3bd', 'hy_b3d', 'hy_wo_st', 'hy_freqd', 'hy_bias', 'hy_w_out', 'hy_b_out',
              'ml_w_in', 'ml_conv_w', 'ml_conv_b', 'ml_bdq', 'ml_bdk', 'ml_bdv', 'ml_w_gate', 'ml_b_gate', 'ml_norm_w',
              'ml_skip', 'ml_w_down', 'ffn_w_up', 'ffn_conv_w', 'ffn_conv_b', 'ffn_w_down',
              'mod_b_col', 'norm_g_col', 'hy_b_in_col', 'hy_sc_w_col', 'hy_sc_b_col', 'hy_bias_col', 'ml_conv_w_col',
              'ml_conv_b_col', 'ml_norm_w_col', 'ml_skip_col', 'ffn_conv_w_col', 'ffn_conv_b_col'):
        W[k] = cx.din(k, inp[k])
    K = {k: cx.din('c_' + k, v) for k, v in HC.items()}
    out_d = nc.dram_tensor('out', [T, D], F32, kind="ExternalOutput").ap()

    MODROW = cx.dscr('MODROW', [2, 2, 6144], F32)
    P0 = cx.dscr('P0', [3 * D, T], BF16)
    P0C = cx.dscr('P0C', [3 * D, TC], BF16)
    Zd = cx.dscr('Zd', [D, T], BF16)
    X0d = cx.dscr('X0d', [D, T], BF16)
    KFd = cx.dscr('KFd', [16, 128, 2, 65, 64], BF16)
    RNd = cx.dscr('RNd', [128, 8], F32)
    Ytd = cx.dscr('Ytd', [D, T], F32)
    XA0 = cx.dscr('XA0', [T, D], F32)
    XB0 = cx.dscr('XB0', [T, D], F32)
    CA0 = cx.dscr('CA0', [TC, D], F32)
    CB0 = cx.dscr('CB0', [TC, D], F32)
    XA1 = cx.dscr('XA1', [T, D], F32)
    ZdC = cx.dscr('ZdC', [D, TC], BF16)
    XMd = cx.dscr('XMd', [MI, T], BF16)
    SZd = cx.dscr('SZd', [MI, T], BF16)
    XMC = cx.dscr('XMC', [MI, TC], BF16)
    Qfm = cx.dscr('Qfm', [MI, T], BF16)
    Kfm = cx.dscr('Kfm', [MI, T], BF16)
    Ktm = cx.dscr('Ktm', [T, MI], BF16)
    Vtm = cx.dscr('Vtm', [T, MI], BF16)
    SXC = cx.dscr('SXC', [MI, T], BF16)
    GPd = cx.dscr('GPd', [T, 4, 8], F32)
    KtmC = cx.dscr('KtmC', [TC, MI], BF16)
    VtmC = cx.dscr('VtmC', [TC, MI], BF16)
    GPdC = cx.dscr('GPdC', [TC, 4, 8], F32)
    HFB = cx.dscr('HFB', [2, T, MI], F32)
    X0dC = cx.dscr('X0dC', [D, TC], BF16)
    YtC = cx.dscr('YtC', [D, TC], F32)
    RNdC = cx.dscr('RNdC', [128, 8], F32)

    identb = sbp('identb', [128, 128], BF16)
    identf = sbp('identf', [128, 128], F32)
    modcol = sbp('modcol', [128, 2, 48, 2], F32)
    effs = sbp('effs', [128, 2, 2, 2, 8], F32)
    P.dma('sp', identf[:], K['ident'], W=['identf'])
    P.copy('dve', identb[:], identf[:], R=['identf'], W=['identb'])

    def phase_adaln():
        with ExitStack() as ps:
            sb = lambda n, s, d: ps.enter_context(nc.sbuf_tensor(n, s, d))
            cv = sb('cv', [128, 8, 2], F32)
            scv = sb('scv', [128, 8, 2], F32)
            mbcol = sb('mbcol', [128, 2, 48], F32)
            mbrow = sb('mbrow', [2, 2, 6144], F32)
            rowbuf = sb('rowbuf', [2, 2, 6144], F32)
            ngcol = sb('ngcol', [128, 2, 2, 8], F32)
            mwp = Pool_(ps, nc, 'mw', [128, 8, 1024], F32, 2)
            pcol = ps.enter_context(nc.psum_tensor('pcol', [128, 48, 2], F32))
            prow = Pool_(ps, nc, 'prow', [2, 512], F32, 2, psum=True)
            P.dma('sp', cv[:], cvec_d.rearrange("(k p) m -> p k m", p=128), W=['cv'])
            P.dma('sp', mbcol[:], W['mod_b_col'], W=['mbcol'])
            for l in range(2):
                P.dma('sp', mbrow[:, l, :], W['mod_b'][l:l + 1, :].partition_broadcast(2) if False else W['mod_b'][l:l + 1, :].broadcast_to([2, 6144]), W=['mbrow'])
            P.dma('sp', ngcol[:], W['norm_g_col'], W=['ngcol'])
            P.act(scv[:], cv[:], AF.Silu, R=['cv'], W=['scv'])
            for l in range(2):
                for j in range(6):
                    mw, mwk = mwp.next()
                    P.dma('sp', mw[:], W['mod_w'][l, :, j * 1024:(j + 1) * 1024].rearrange("(k p) n -> p k n", p=128), W=[mwk])
                    for m in range(8):
                        for k in range(8):
                            P.mm(pcol[:, j * 8 + m, :], mw[:, k, m * 128:(m + 1) * 128], scv[:, k, :], k == 0, k == 7,
                                 R=[mwk, 'scv'], W=['pcol'])
                    for half in range(2):
                        pr, prk = prow.next()
                        for k in range(8):
                            P.mm(pr[:], scv[:, k, :], mw[:, k, half * 512:(half + 1) * 512], k == 0, k == 7, R=[mwk, 'scv'], W=[prk])
                        o = j * 1024 + half * 512
                        P.tt('dve', rowbuf[:, l, o:o + 512], pr[:], mbrow[:, l, o:o + 512], ALU.add, R=[prk, 'mbrow'], W=['rowbuf'])
                for m in range(2):
                    P.tt('dve', modcol[:, l, :, m], pcol[:, :, m], mbcol[:, l, :], ALU.add, R=['pcol', 'mbcol'], W=['modcol'])
                for i in range(2):
                    for m in range(2):
                        sc_ap = modcol[:, l, 8 + 24 * i:16 + 24 * i, m]
                        P.ts('dve', effs[:, l, i, m, :], sc_ap, 1.0, None, ALU.add, None, R=['modcol'], W=['effs'])
                        P.tt('dve', effs[:, l, i, m, :], effs[:, l, i, m, :], ngcol[:, l, i, :], ALU.mult, R=['effs', 'ngcol'], W=['effs'])
            P.dma('sp', MODROW.rearrange("l m n -> m l n"), rowbuf[:], R=['rowbuf'], W=['MODROW'])
            P.flush()

    def shift_col(l, i, m):
        return modcol[:, l, 24 * i:24 * i + 8, m]

    class FrontEnd:
        def __init__(self, ps, nblk, inplace=False, junk=None):
            self.nblk = nblk
            self.inplace = inplace
            self.junk_ap = junk
            if not inplace:
                self.xn = Pool_(ps, nc, 'fe_xn', [128, nblk, D], BF16, 1)
            if junk is None:
                self.junk = ps.enter_context(nc.sbuf_tensor('fe_junk', [128, D], BF16))
                self.junk_ap = self.junk[:]
            self.ss = Pool_(ps, nc, 'fe_ss', [128, nblk], F32, 2)
            self.pT = Pool_(ps, nc, 'fe_pT', [128, nblk * 128], BF16, 2, psum=True)

        def run(self, xt, xtk, hT, hTk, l, i, m):
            nblk = self.nblk
            ss, ssk = self.ss.next()
            if self.inplace:
                xn, xnk = xt, xtk
            else:
                xn, xnk = self.xn.next()
            for j in range(nblk):
                P.act(self.junk_ap, xt[:, j, :], AF.Square, R=[xtk], W=['fe_junk', ssk], accum_out=ss[:, j:j + 1])
            P.ts('dve', ss[:], ss[:], 1.0 / D, EPS, ALU.mult, ALU.add, R=[ssk], W=[ssk])
            P.act(ss[:], ss[:], AF.Sqrt, R=[ssk], W=[ssk])
            P.recip(ss[:], ss[:], R=[ssk], W=[ssk])
            for j in range(nblk):
                P.ts('pool', xn[:, j, :], xt[:, j, :], ss[:, j:j + 1], None, ALU.mult, None, R=[xtk, ssk], W=[xnk])
            for k in range(8):
                pT, pTk = self.pT.next()
                for j in range(nblk):
                    P.tr(pT[:, j * 128:(j + 1) * 128], xn[:, j, k * 128:(k + 1) * 128], identb[:], R=[xnk, 'identb'], W=[pTk])
                P.act(hT[:, k, :], pT[:], AF.Identity, R=[pTk, 'effs', 'modcol'], W=[hTk],
                      scale=effs[:, l, i, m, k:k + 1], bias=shift_col(l, i, m)[:, k:k + 1])

    def phase_h1(src, Tn, dst):
        nblk = 4 if Tn >= 512 else Tn // 128
        TT = nblk * 128
        with ExitStack() as ps:
            sb = lambda n, s, d: ps.enter_context(nc.sbuf_tensor(n, s, d))
            win = sb('win', [128, 8, 3 * D], BF16)
            bcol = sb('bcol', [128, 24], F32)
            fe = FrontEnd(ps, nblk)
            xtp = Pool_(ps, nc, 'xt', [128, nblk, D], F32, 2)
            hTp = Pool_(ps, nc, 'hT', [128, 8, TT], BF16, 2)
            pbp = Pool_(ps, nc, 'pb', [128, 6, TT], BF16, 2)
            pp = Pool_(ps, nc, 'pp', [128, TT], F32, 4, psum=True)
            for k in range(8):
                P.dma('pool', win[:, k, :], W['hy_w_in'][k * 128:(k + 1) * 128, :], W=['win'])
            P.dma('sp', bcol[:], W['hy_b_in_col'], W=['bcol'])
            m = 0 if Tn == T else 1

            def load(ti):
                xt, xtk = xtp.next()
                P.dma('sp', xt[:], src[ti * TT:(ti + 1) * TT, :].rearrange("(j p) d -> p j d", p=128), W=[xtk])
                return xt, xtk
            ntile = Tn // TT
            ld = load(0)
            for ti in range(ntile):
                t0 = ti * TT
                xt, xtk = ld
                if ti + 1 < ntile:
                    ld = load(ti + 1)
                hT, hTk = hTp.next()
                fe.run(xt, xtk, hT, hTk, 0, 0, m)
                for og in range(4):
                    pb, pbk = pbp.next()
                    for oi in range(6):
                        oc = og * 6 + oi
                        pt, ptk = pp.next()
                        for k in range(8):
                            P.mm(pt[:], win[:, k, oc * 128:(oc + 1) * 128], hT[:, k, :], k == 0, k == 7, R=['win', hTk], W=[ptk])
                        if oi % 2 == 0:
                            P.act(pb[:, oi, :], pt[:], AF.Identity, R=[ptk, 'bcol'], W=[pbk], bias=bcol[:, oc:oc + 1])
                        else:
                            P.ts('dve', pb[:, oi, :], pt[:], bcol[:, oc:oc + 1], None, ALU.add, None, R=[ptk, 'bcol'], W=[pbk])
                    P.dma('sp', dst[og * 768:(og + 1) * 768, t0:t0 + TT].rearrange("(c p) t -> p c t", p=128), pb[:], R=[pbk], W=[P.uq('P0')])
            P.flush()

    def phase_h2(src, Tn, zdst, x0dst):
        PW = 2048 if Tn >= 2048 else Tn
        npc = Tn // PW
        with ExitStack() as ps:
            sb = lambda n, s, d: ps.enter_context(nc.sbuf_tensor(n, s, d))
            scw = sb('scw', [128, 3, 24], F32)
            scb = sb('scb', [128, 24], F32)
            pinp = Pool_(ps, nc, 'pin', [128, 3, PW + 2], BF16, 2)
            accp = Pool_(ps, nc, 'acc', [128, 3, PW], F32, 2)
            zp = Pool_(ps, nc, 'zt', [128, PW], BF16, 2)
            x0p = Pool_(ps, nc, 'x0t', [128, PW], BF16, 2)
            P.dma('sp', scw[:], W['hy_sc_w_col'], W=['scw'])
            P.dma('sp', scb[:], W['hy_sc_b_col'], W=['scb'])
            srcv = src.rearrange("(j c p) t -> c p j t", j=3, c=8, p=128)
            def load(it):
                cc, pi = it // npc, it % npc
                t0 = pi * PW
                pin, pink = pinp.next()
                lo = 1 if pi == 0 else 0
                hi = PW + 1 if pi == npc - 1 else PW + 2
                if pi == 0:
                    P.memset('pool', pin[:, :, 0:1], 0.0, W=[pink])
                if pi == npc - 1:
                    P.memset('pool', pin[:, :, PW + 1:PW + 2], 0.0, W=[pink])
                P.dma('sp', pin[:, :, lo:hi], srcv[cc][:, :, t0 - 1 + lo:t0 - 1 + hi], W=[pink])
                return pin, pink
            ld = load(0)
            for cc in range(8):
                for pi in range(npc):
                    t0 = pi * PW
                    pin, pink = ld
                    if cc * npc + pi + 1 < 8 * npc:
                        ld = load(cc * npc + pi + 1)
                    acc, acck = accp.next()
                    zt, ztk = zp.next()
                    x0t, x0k = x0p.next()
                    for j in range(3):
                        oc = j * 8 + cc
                        P.act(acc[:, j, :], pin[:, j, 1:PW + 1], AF.Identity, R=[pink, 'scw', 'scb'], W=[(acck, j)],
                              scale=scw[:, 1, oc:oc + 1], bias=scb[:, oc:oc + 1])
                        P.stt(acc[:, j, :], pin[:, j, 0:PW], scw[:, 0, oc:oc + 1], acc[:, j, :], ALU.mult, ALU.add,
                              R=[pink, 'scw', (acck, j)], W=[(acck, j)])
                        if j == 0:
                            P.stt(x0t[:], pin[:, j, 2:PW + 2], scw[:, 2, oc:oc + 1], acc[:, j, :], ALU.mult, ALU.add,
                                  R=[pink, 'scw', (acck, j)], W=[x0k])
                        else:
                            P.stt(acc[:, j, :], pin[:, j, 2:PW + 2], scw[:, 2, oc:oc + 1], acc[:, j, :], ALU.mult, ALU.add,
                                  R=[pink, 'scw', (acck, j)], W=[(acck, j)])
                    P.tt('pool', zt[:], acc[:, 1, :], acc[:, 2, :], ALU.mult, R=[(acck, 1), (acck, 2)], W=[ztk])
                    P.dma('sp', zdst[cc * 128:(cc + 1) * 128, t0:t0 + PW], zt[:], R=[ztk], W=[P.uq('Zd')])
                    P.dma('sp', x0dst[cc * 128:(cc + 1) * 128, t0:t0 + PW], x0t[:], R=[x0k], W=[P.uq('X0d')])
            P.flush()

    def sin_layer(pre, prek, frc, frb, tmpa, tmpk, tmpb, tmpbk, out, outk):
        P.ts('dve', tmpa, pre, frc, frb, ALU.mult, ALU.add, R=[prek, 'mlpc'], W=[tmpk])
        P.ts('dve', tmpb, tmpa, 1.0 / TWO_PI, MAGIC, ALU.mult, ALU.add, R=[tmpk], W=[tmpbk])
        P.ts('dve', tmpb, tmpb, -MAGIC, -TWO_PI, ALU.add, ALU.mult, R=[tmpbk], W=[tmpbk])
        P.tt('dve', tmpa, tmpa, tmpb, ALU.add, R=[tmpk, tmpbk], W=[tmpk])
        P.act(out, tmpa, AF.Sin, R=[tmpk], W=[outk])

    class FilterMLP:
        def __init__(self, ps):
            sb = lambda n, s, d: ps.enter_context(nc.sbuf_tensor(n, s, d))
            self.w1 = sb('mlp_w1', [33, 128], F32)
            self.w2 = sb('mlp_w2', [128, 128], F32)
            self.w3 = sb('mlp_w3', [128, 128], F32)
            self.cols = sb('mlp_cols', [128, 8], F32)
            P.dma('sp', self.w1[:], W['hy_w1d'], W=['mlpw'])
            P.dma('sp', self.w2[:], W['hy_w2bd'], W=['mlpw'])
            P.dma('sp', self.w3[:], W['hy_w3bd'], W=['mlpw'])
            P.dma('sp', self.cols[:, 0:1], W['hy_freqd'], W=['mlpc'])
            P.dma('sp', self.cols[:, 1:2], W['hy_b1d'], W=['mlpc'])
            P.dma('sp', self.cols[:, 2:3], W['hy_b2d'], W=['mlpc'])
            P.dma('sp', self.cols[:, 3:4], W['hy_b3d'], W=['mlpc'])
            P.ts('dve', self.cols[:, 4:7], self.cols[:, 1:4], self.cols[:, 0:1], None, ALU.mult, None, R=['mlpc'], W=['mlpc'])
            self.posp = Pool_(ps, nc, 'mlp_pos', [33, 512], F32, 2)
            self.ta = Pool_(ps, nc, 'mlp_ta', [128, 512], F32, 2)
            self.tb = Pool_(ps, nc, 'mlp_tb', [128, 512], F32, 2)
            self.h = Pool_(ps, nc, 'mlp_h', [128, 512], F32, 2)
            self.pm = Pool_(ps, nc, 'mlp_pm', [128, 512], F32, 2, psum=True)

        def run(self, pos_d, c0, n, out, outk):
            pos, posk = self.posp.next()
            P.dma('sp', pos[:, 0:n], pos_d[:, c0:c0 + n], W=[posk])
            cur, curk, ws = pos[:, 0:n], posk, [self.w1, self.w2, self.w3]
            for li in range(3):
                pm, pmk = self.pm.next()
                P.mm(pm[:, 0:n], ws[li][:], cur, True, True, R=['mlpw', curk], W=[pmk])
                ta, tak = self.ta.next()
                tb, tbk = self.tb.next()
                if li < 2:
                    h, hk = self.h.next()
                    o, ok = h[:, 0:n], hk
                else:
                    o, ok = out, outk
                sin_layer(pm[:, 0:n], pmk, self.cols[:, 0:1], self.cols[:, 4 + li:5 + li], ta[:, 0:n], tak, tb[:, 0:n], tbk, o, ok)
                cur, curk = o, ok

    class FFTConsts:
        def __init__(self, ps, inverse):
            sb = lambda n, s, d: ps.enter_context(nc.sbuf_tensor(n, s, d))
            self.FA = sb('FA', [128, 128], BF16)
            self.TC = sb('TC', [128, 65, 2, 128], BF16)
            P.dma('pool', self.FA[:], K['FA'], W=['fftc'])
            P.dma('pool', self.TC[:], K['TC'], W=['fftc'])
            self.B = sb('Bbuf', [128, 3, 65, 64], BF16)
            P.memset('dve', self.B[:, 1, 0:1, :], 0.0, W=['Bbuf'])
            P.memset('dve', self.B[:, 1, 64:65, :], 0.0, W=['Bbuf'])
            self.pa = Pool_(ps, nc, 'pa', [128, 4, 128], F32, 2, psum=True)
            self.xp = Pool_(ps, nc, 'xp', [128, 2, 4, 64], F32, 2, psum=True)

        def stage_a(self, src, srck, Kp):
            B = self.B
            for c4 in range(16):
                pa, pak = self.pa.next()
                for i in range(4):
                    c = c4 * 4 + i
                    P.mm(pa[:, i, :], src[0:Kp, c, :], self.FA[0:Kp, :], True, True, R=[srck, 'fftc'], W=[pak])
                o0 = B[:, 0, :, c4 * 4:c4 * 4 + 4].rearrange("p k c -> p c k")
                o2 = B[:, 2, :, c4 * 4:c4 * 4 + 4].rearrange("p k c -> p c k")
                o1 = B[:, 1, 1:64, c4 * 4:c4 * 4 + 4].rearrange("p k c -> p c k")
                P.act(o0, pa[:, :, 0:65], AF.Identity, R=[pak], W=['Bbuf'])
                P.act(o2, pa[:, :, 0:65], AF.Identity, R=[pak], W=['Bbuf'], scale=-1.0)
                P.copy('dve', o1, pa[:, :, 65:128], R=[pak], W=['Bbuf'])

        def stage_c(self, consume):
            B, TC = self.B, self.TC
            for kq in range(17):
                k10 = kq * 4
                nk = min(4, 65 - k10)
                xp, xpk = self.xp.next()
                for i in range(nk):
                    k1 = k10 + i
                    P.mm(xp[:, 0, i, :], TC[:, k1, 0, :], B[:, 0, k1, :], True, False, R=['fftc', 'Bbuf'], W=[xpk])
                    P.mm(xp[:, 0, i, :], TC[:, k1, 1, :], B[:, 1, k1, :], False, True, R=['fftc', 'Bbuf'], W=[xpk])
                    P.mm(xp[:, 1, i, :], TC[:, k1, 0, :], B[:, 1, k1, :], True, False, R=['fftc', 'Bbuf'], W=[xpk])
                    P.mm(xp[:, 1, i, :], TC[:, k1, 1, :], B[:, 2, k1, :], False, True, R=['fftc', 'Bbuf'], W=[xpk])
                consume(xp, xpk, k10, nk)

    def phase_k():
        with ExitStack() as ps:
            sb = lambda n, s, d: ps.enter_context(nc.sbuf_tensor(n, s, d))
            h3 = sb('h3', [128, 16384], BF16)
            with ExitStack() as ps2:
                mlp = FilterMLP(ps2)
                for ct in range(32):
                    mlp.run(K['posR'], ct * 512, 512, h3[:, ct * 512:(ct + 1) * 512], 'h3')
                h3v = h3[:].rearrange("p (a b) -> p a b", b=128)
                P.memset('pool', h3v[0:64, :, 64:128], 0.0, W=['h3'])
                P.memset('pool', h3v[64:128, :, 0:64], 0.0, W=['h3'])
                P.memset('pool', h3v[64:128, 0:1, 64:65], 0.0, W=['h3'])
                P.flush()
            fc = FFTConsts(ps, False)
            wo = sb('wo', [128, D], BF16)
            decL = sb('decL', [3, 128], F32)
            ones = sb('onesf', [128, 1], F32)
            P.dma('pool', wo[:], W['hy_wo_st'], W=['wo'])
            P.dma('sp', decL[:], K['decL'], W=['decL'])
            P.memset('dve', ones[:], 1.0, W=['onesf'])
            KAp = Pool_(ps, nc, 'KA', [128, 64, 128], BF16, 2)
            KFp = Pool_(ps, nc, 'KFs', [128, 2, 65, 64], BF16, 2)
            decRp = Pool_(ps, nc, 'decR', [3, 8, 64], F32, 3)
            decp = Pool_(ps, nc, 'dec', [128, 8, 64], F32, 2)
            tmpp = Pool_(ps, nc, 'ktmp', [128, 8, 64], F32, 2)
            partp = Pool_(ps, nc, 'kpart', [128, 64], F32, 2)
            accp = Pool_(ps, nc, 'kacc', [128, 64], F32, 2)
            rnp = Pool_(ps, nc, 'rn', [64, 1], F32, 2)
            pkp = Pool_(ps, nc, 'pk', [128, 8, 64], F32, 2, psum=True)
            pep = Pool_(ps, nc, 'pe', [128, 8, 64], F32, 1, psum=True)
            pnp = Pool_(ps, nc, 'pn', [64, 1], F32, 1, psum=True)
            for g in range(16):
                c0 = g * 64
                KA, KAk = KAp.next()
                acc, acck = accp.next()
                for nb in range(16):
                    dr, drk = decRp.next()
                    P.dma('sp', dr[:], K['decR'][:, nb * 8:(nb + 1) * 8, c0:c0 + 64], W=[drk])
                    pk, pkk = pkp.next()
                    for i in range(8):
                        n2 = nb * 8 + i
                        P.mm(pk[:, i, :], h3[:, n2 * 128:(n2 + 1) * 128], wo[:, c0:c0 + 64], True, True, R=['h3', 'wo'], W=[pkk])
                    pe_, pek = pep.next()
                    P.mm(pe_[:].rearrange('p a b -> p (a b)'), decL[:], dr[:].rearrange('p a b -> p (a b)'), True, True, R=['decL', drk], W=[pek])
                    dec, deck = decp.next()
                    P.act(dec[:], pe_[:], AF.Exp, R=[pek], W=[deck])
                    tmp, tmpk = tmpp.next()
                    P.tt('dve', tmp[:], pk[:], dec[:], ALU.mult, R=[pkk, deck], W=[tmpk])
                    tv = tmp[:].rearrange("p n c -> p c n")
                    P.copy('pool', KA[:, :, nb * 8:(nb + 1) * 8], tv, R=[tmpk], W=[KAk])
                    if nb == 0:
                        P.op('dve', lambda e, o=acc[:], i_=tv: e.tensor_reduce(out=o, in_=i_, axis=AX.X, op=ALU.add, apply_absolute_value=True),
                             R=[tmpk], W=[acck])
                    else:
                        part, partk = partp.next()
                        P.op('dve', lambda e, o=part[:], i_=tv: e.tensor_reduce(out=o, in_=i_, axis=AX.X, op=ALU.add, apply_absolute_value=True),
                             R=[tmpk], W=[partk])
                        P.tt('pool', acc[:], acc[:], part[:], ALU.add, R=[acck, partk], W=[acck])
                pn, pnk = pnp.next()
                P.mm(pn[:], acc[:], ones[:], True, True, R=[acck, 'onesf'], W=[pnk])
                rn, rnk = rnp.next()
                P.recip(rn[:], pn[:], R=[pnk], W=[rnk])
                P.dma('sp', RNd[(g % 2) * 64:(g % 2) * 64 + 64, g // 2:g // 2 + 1], rn[:], R=[rnk], W=[P.uq('RNd')], allow_slow_non_contiguous=True)
                fc.stage_a(KA, KAk, 128)
                KFs, KFk = KFp.next()

                def consume(xp, xpk, k10, nk, KFs=KFs, KFk=KFk):
                    P.act(KFs[:, :, k10:k10 + nk, :], xp[:, :, 0:nk, :], AF.Identity, R=[xpk], W=[KFk])
                fc.stage_c(consume)
                P.dma('sp', KFd[g], KFs[:], R=[KFk], W=[P.uq('KFd')])
            P.flush()

    def phase_z():
        with ExitStack() as ps:
            sb = lambda n, s, d: ps.enter_context(nc.sbuf_tensor(n, s, d))
            fc = FFTConsts(ps, True)
            IR = sb('IR', [128, 2, 256], BF16)
            TWI = sb('TWI', [65, 2, 128], F32)
            G = sb('G', [65, 2, 64], BF16)
            P.dma('pool', IR[:], K['IR'], W=['IR'])
            P.dma('sp', TWI[:], K['TWI'], W=['TWI'])
            P.dma('pool', G[:], K['G'], W=['G'])
            ZAp = Pool_(ps, nc, 'ZA', [64, 64, 128], BF16, 2)
            KFp = Pool_(ps, nc, 'KF', [128, 2, 65, 64], BF16, 1)
            Yp = Pool_(ps, nc, 'Y', [128, 2, 64, 65], BF16, 1)
            t1p = Pool_(ps, nc, 'zt1', [128, 4, 64], F32, 2)
            t2p = Pool_(ps, nc, 'zt2', [128, 4, 64], F32, 2)
            crp = Pool_(ps, nc, 'craw', [65, 2, 2, 128], F32, 2)
            m1p = Pool_(ps, nc, 'm1', [65, 2, 2, 128], F32, 2)
            m2p = Pool_(ps, nc, 'm2', [65, 2, 2, 128], F32, 2)
            C2p = Pool_(ps, nc, 'C2', [65, 2, 16, 128], BF16, 2)
            ybp = Pool_(ps, nc, 'ybuf', [64, 16, 128], F32, 2)
            cpp = Pool_(ps, nc, 'cp', [65, 2, 2, 128], F32, 2, psum=True)
            ypp = Pool_(ps, nc, 'yp', [64, 4, 128], F32, 2, psum=True)
            kq_par = [0]
            for g in range(16):
                c0 = g * 64
                ZA, ZAk = ZAp.next()
                P.dma('sp', ZA[:], Zd[c0:c0 + 64, :].rearrange("c (a b) -> a c b", b=128), W=[ZAk])
                KF, KFk = KFp.next()
                P.dma('sp', KF[:], KFd[g], W=[KFk])
                fc.stage_a(ZA, ZAk, 64)
                Y, Yk = Yp.next()

                def consume(xp, xpk, k10, nk, KF=KF, KFk=KFk, Y=Y, Yk=Yk):
                    kq_par[0] += 1
                    t1, t1k = t1p.next()
                    t2, t2k = t2p.next()
                    yo = lambda ri: Y[:, ri, :, k10:k10 + nk].rearrange("p c k -> p k c")
                    P.tt('dve', t1[:, 0:nk, :], xp[:, 0, 0:nk, :], KF[:, 0, k10:k10 + nk, :], ALU.mult, R=[xpk, KFk], W=[t1k])
                    P.tt('dve', t2[:, 0:nk, :], xp[:, 1, 0:nk, :], KF[:, 1, k10:k10 + nk, :], ALU.mult, R=[xpk, KFk], W=[t2k])
                    P.tt('pool', yo(0), t1[:, 0:nk, :], t2[:, 0:nk, :], ALU.subtract, R=[t1k, t2k], W=[Yk])
                    t1, t1k = t1p.next()
                    t2, t2k = t2p.next()
                    P.tt('dve', t1[:, 0:nk, :], xp[:, 0, 0:nk, :], KF[:, 1, k10:k10 + nk, :], ALU.mult, R=[xpk, KFk], W=[t1k])
                    P.tt('dve', t2[:, 0:nk, :], xp[:, 1, 0:nk, :], KF[:, 0, k10:k10 + nk, :], ALU.mult, R=[xpk, KFk], W=[t2k])
                    P.tt('pool', yo(1), t1[:, 0:nk, :], t2[:, 0:nk, :], ALU.add, R=[t1k, t2k], W=[Yk])
                fc.stage_c(consume)
                for cs in range(4):
                    C2, C2k = C2p.next()
                    yb, ybk = ybp.next()
                    for cb in range(8):
                        cp, cpk = cpp.next()
                        for i in range(2):
                            c = cs * 16 + cb * 2 + i
                            o = cp[:, i, :, :].rearrange("p a b -> p (a b)")
                            P.mm(o, Y[:, 0, c, :], IR[:, 0, :], True, False, R=[Yk, 'IR'], W=[cpk])
                            P.mm(o, Y[:, 1, c, :], IR[:, 1, :], False, True, R=[Yk, 'IR'], W=[cpk])
                        cr, crk = crp.next()
                        P.copy('act', cr[:], cp[:], R=[cpk], W=[crk])
                        m1, m1k = m1p.next()
                        m2, m2k = m2p.next()
                        for i in range(2):
                            for r in range(2):
                                P.tt('dve' if r == 0 else 'pool', m1[:, i, r, :], cr[:, i, r, :], TWI[:, 0, :], ALU.mult, R=[crk, 'TWI'], W=[(m1k, i, r)])
                                P.tt('dve' if r == 1 else 'pool', m2[:, i, r, :], cr[:, i, r, :], TWI[:, 1, :], ALU.mult, R=[crk, 'TWI'], W=[(m2k, i, r)])
                        cc0 = cb * 2
                        P.tt('pool', C2[:, 0, cc0:cc0 + 2, :], m1[:, :, 0, :], m2[:, :, 1, :], ALU.subtract,
                             R=[(m1k, 0, 0), (m1k, 1, 0), (m2k, 0, 1), (m2k, 1, 1)], W=[(C2k, cb // 2)])
                        P.tt('dve', C2[:, 1, cc0:cc0 + 2, :], m2[:, :, 0, :], m1[:, :, 1, :], ALU.add,
                             R=[(m2k, 0, 0), (m2k, 1, 0), (m1k, 0, 1), (m1k, 1, 1)], W=[(C2k, cb // 2)])
                        if cb % 2 == 1:
                            q = cb // 2
                            yp, ypk = ypp.next()
                            o = yp[:].rearrange("p a b -> p (a b)")
                            P.mm(o, G[:, 0, :], C2[:, 0, q * 4:q * 4 + 4, :].rearrange("p a b -> p (a b)"), True, False, R=['G', (C2k, q)], W=[ypk])
                            P.mm(o, G[:, 1, :], C2[:, 1, q * 4:q * 4 + 4, :].rearrange("p a b -> p (a b)"), False, True, R=['G', (C2k, q)], W=[ypk])
                            P.copy('act', yb[:, q * 4:q * 4 + 4, :], yp[:], R=[ypk], W=[ybk])
                    cA = c0 + cs * 16
                    P.dma('sp', Ytd[cA:cA + 16, :].rearrange("c (a b) -> a c b", b=128), yb[:], R=[ybk], W=[P.uq('Ytd')])
            P.flush()

    def load_row(sb_tile, key, src1d, q='sp'):
        P.dma(q, sb_tile, src1d.partition_broadcast(128), W=[key])

    def phase_h5(ysrc, zsrc, x0src, xin, xout, Tn, m, rnsrc):
        TT = 512 if Tn >= 512 else Tn
        nb = TT // 128
        with ExitStack() as ps:
            sb = lambda n, s, d: ps.enter_context(nc.sbuf_tensor(n, s, d))
            wout = sb('wout', [128, 8, D], BF16)
            P.dma('pool', wout[:], W['hy_w_out'].rearrange("(k p) n -> p k n", p=128), W=['wout'])
            rn = sb('rncol', [128, 8], F32)
            bias = sb('hbias', [128, 8], F32)
            P.dma('sp', rn[:], rnsrc, W=['rncol'])
            P.dma('sp', bias[:], W['hy_bias_col'], W=['hbias'])
            brow = sb('brow', [128, D], F32)
            grow = sb('grow', [128, D], F32)
            load_row(brow[:], 'brow', W['hy_b_out'])
            load_row(grow[:], 'grow', MODROW[0, m, 2048:3072])
            ytp = Pool_(ps, nc, 'yt', [128, 8, TT], F32, 2)
            ztp = Pool_(ps, nc, 'zt5', [128, 8, TT], BF16, 2)
            x0p = Pool_(ps, nc, 'x05', [128, 8, TT], BF16, 2)
            a1p = Pool_(ps, nc, 'a1', [128, TT], F32, 2)
            gtp = Pool_(ps, nc, 'gt', [128, 8, TT], BF16, 2)
            xtp = Pool_(ps, nc, 'xt5', [128, nb, D], F32, 2)
            tmp = Pool_(ps, nc, 'tmp5', [128, 512], F32, 3)
            pop = Pool_(ps, nc, 'po', [128, 512], F32, 4, psum=True)
            def load(ti):
                t0 = ti * TT
                yt, ytk = ytp.next()
                zt, ztk = ztp.next()
                x0, x0k = x0p.next()
                xt, xtk = xtp.next()
                P.dma('sp', yt[:], ysrc[:, t0:t0 + TT].rearrange("(k p) t -> p k t", p=128), W=[ytk])
                P.dma('sp', zt[:], zsrc[:, t0:t0 + TT].rearrange("(k p) t -> p k t", p=128), W=[ztk])
                P.dma('sp', x0[:], x0src[:, t0:t0 + TT].rearrange("(k p) t -> p k t", p=128), W=[x0k])
                P.dma('sp', xt[:], xin[t0:t0 + TT, :].rearrange("(j p) d -> p j d", p=128), W=[xtk])
                return yt, ytk, zt, ztk, x0, x0k, xt, xtk
            ntile = Tn // TT
            ld = load(0)
            for ti in range(ntile):
                t0 = ti * TT
                yt, ytk, zt, ztk, x0, x0k, xt, xtk = ld
                if ti + 1 < ntile:
                    ld = load(ti + 1)
                gt, gtk = gtp.next()
                for k in range(8):
                    a1, a1k = a1p.next()
                    P.act(a1[:], zt[:, k, :], AF.Identity, R=[ztk, 'hbias'], W=[a1k], scale=bias[:, k:k + 1])
                    P.stt(a1[:], yt[:, k, :], rn[:, k:k + 1], a1[:], ALU.mult, ALU.add, R=[ytk, 'rncol', a1k], W=[a1k])
                    P.tt('dve' if k % 4 else 'pool', gt[:, k, :], a1[:], x0[:, k, :], ALU.mult, R=[a1k, x0k], W=[(gtk, k)])
                for tb in range(nb):
                    for dh in range(2):
                        po, pok = pop.next()
                        for k in range(8):
                            P.mm(po[:], gt[:, k, tb * 128:(tb + 1) * 128], wout[:, k, dh * 512:(dh + 1) * 512], k == 0, k == 7,
                                 R=[(gtk, k), 'wout'], W=[pok])
                        tm, tmk = tmp.next()
                        dsl = slice(dh * 512, (dh + 1) * 512)
                        P.tt('dve', tm[:], po[:], brow[:, dsl], ALU.add, R=[pok, 'brow'], W=[tmk])
                        P.tt('dve', tm[:], tm[:], grow[:, dsl], ALU.mult, R=[tmk, 'grow'], W=[tmk])
                        P.tt('pool' if dh == 0 else 'dve', xt[:, tb, dsl], tm[:], xt[:, tb, dsl], ALU.add, R=[tmk, xtk], W=[xtk])
                P.dma('sp', xout[t0:t0 + TT, :].rearrange("(j p) d -> p j d", p=128), xt[:], R=[xtk], W=[P.uq('xout')])
            P.flush()

    def phase_ffn(l, xin, xout, Tn, m, final=False):
        lat = Tn == T
        halo = 64 if lat else 0
        CEN = 256
        NTOK = CEN + 2 * halo
        nblk = NTOK // 128
        gc = 64 if lat else 256
        R_ = CEN // gc
        RH = NTOK // gc
        ntile = Tn // CEN
        with ExitStack() as ps:
            sb = lambda n, s, d: ps.enter_context(nc.sbuf_tensor(n, s, d))
            wup = sb('wup', [128, 8, 2 * FH], BF16)
            wdn = sb('wdn', [128, NFC, D], BF16)
            for k in range(8):
                P.dma('pool', wup[:, k, :], W['ffn_w_up'][l, k * 128:(k + 1) * 128, :], W=['wup'])
            for fcc in range(NFC):
                P.dma('pool', wdn[:, fcc, :], W['ffn_w_down'][l, fcc * 128:(fcc + 1) * 128, :], W=['wdn'])
            cw = sb('cw', [128, 2, 9, NFC], F32)
            cb = sb('cb', [128, 2, NFC], F32)
            P.dma('sp', cw[:], W['ffn_conv_w_col'], W=['cw'])
            P.dma('sp', cb[:], W['ffn_conv_b_col'], W=['cw'])
            grow = sb('grow2', [128, D], F32)
            load_row(grow[:], 'grow2', MODROW[l, m, 5120:6144])
            if final:
                fgrow = sb('fgrow', [128, D], F32)
                load_row(fgrow[:], 'fgrow', W['final_g'][0])
                fss = Pool_(ps, nc, 'fss', [128, 2], F32, 2)
            tmpt = sb('tmpf', [128, 512], F32)
            fe = FrontEnd(ps, nblk, inplace=True, junk=tmpt[:].bitcast(BF16))
            xhp = Pool_(ps, nc, 'xh', [128, nblk, D], BF16, 1)
            xcp = Pool_(ps, nc, 'xc', [128, 2, D], F32, 2)
            uTp = Pool_(ps, nc, 'uT', [128, 8, NTOK], BF16, 2)
            h2p = Pool_(ps, nc, 'h2', [128, NFC, CEN], BF16, 2)
            gsp = Pool_(ps, nc, 'gs', [128, NTOK], F32, 2)
            acp = Pool_(ps, nc, 'cacc', [128, CEN], F32, 3)
            pap = Pool_(ps, nc, 'pa_f', [128, CEN], F32, 3, psum=True)
            pgp = Pool_(ps, nc, 'pg_f', [128, NTOK], F32, 2, psum=True)
            pop = Pool_(ps, nc, 'po_f', [128, 512], F32, 1, psum=True)
            taps = [(kr, kc) for kr in range(3) for kc in range(3)] if lat else [(1, 0), (1, 1), (1, 2)]

            def load(ti):
                t0 = ti * CEN
                xh, xhk = xhp.next()
                xc, xck = xcp.next()
                ts_ = t0 - halo
                if lat and ti == 0:
                    P.memset('pool', xh[:, 0, :], 0.0, W=[xhk])
                    P.dma('pool', xh[64:128, 0, :], xin[0:64, :], W=[xhk])
                    P.dma('pool', xh[:, 1:3, :], xin[64:320, :].rearrange("(j p) d -> p j d", p=128), W=[xhk])
                elif lat and ti == ntile - 1:
                    P.memset('pool', xh[:, 2, :], 0.0, W=[xhk])
                    P.dma('pool', xh[0:64, 2, :], xin[Tn - 64:Tn, :], W=[xhk])
                    P.dma('pool', xh[:, 0:2, :], xin[ts_:ts_ + 256, :].rearrange("(j p) d -> p j d", p=128), W=[xhk])
                else:
                    P.dma('pool', xh[:], xin[ts_:ts_ + NTOK, :].rearrange("(j p) d -> p j d", p=128), W=[xhk])
                P.dma('sp', xc[:], xin[t0:t0 + CEN, :].rearrange("(j p) d -> p j d", p=128), W=[xck])
                return xh, xhk, xc, xck

            def front(ld):
                uT, uTk = uTp.next()
                fe.run(ld[0], ld[1], uT, uTk, l, 1, m)
                return uT, uTk

            def h2_op(pend):
                acc, acck, pa, pak, h2, h2k, fcc = pend
                P.act(acc[:], acc[:], AF.Silu, R=[acck], W=[acck])
                return pend

            def h2_fin(pend):
                acc, acck, pa, pak, h2, h2k, fcc = pend
                P.tt('dve', h2[:, fcc, :], acc[:], pa[:], ALU.mult, R=[acck, pak], W=[(h2k, fcc)])

            ld = load(0)
            fr = front(ld)
            for ti in range(ntile):
                t0 = ti * CEN
                xh, xhk, xc, xck = ld
                uT, uTk = fr
                if ti + 1 < ntile:
                    ld_n = load(ti + 1)
                h2, h2k = h2p.next()
                pend = None
                for fcc in range(NFC):
                    pa, pak = pap.next()
                    for k in range(8):
                        P.mm(pa[:], wup[:, k, fcc * 128:(fcc + 1) * 128], uT[:, k, halo:halo + CEN], k == 0, k == 7, R=['wup', uTk], W=[pak])
                    pg, pgk = pgp.next()
                    for k in range(8):
                        P.mm(pg[:], wup[:, k, FH + fcc * 128:FH + (fcc + 1) * 128], uT[:, k, :], k == 0, k == 7, R=['wup', uTk], W=[pgk])
                    gs, gsk = gsp.next()
                    P.copy('act', gs[:], pg[:], R=[pgk], W=[gsk])
                    if pend is not None:
                        h2_op(pend)
                    if lat and ti == 0:
                        P.memset('pool', gs[:, 0:64], 0.0, W=[gsk])
                    if lat and ti == ntile - 1:
                        P.memset('pool', gs[:, NTOK - 64:NTOK], 0.0, W=[gsk])
                    gv = gs[:].rearrange("p (r c) -> p r c", c=gc)
                    acc, acck = acp.next()
                    av = acc[:].rearrange("p (r c) -> p r c", c=gc)
                    r0 = 1 if lat else 0
                    P.ts('dve', av, gv[:, r0:r0 + R_, :], cw[:, l, 4, fcc:fcc + 1], cb[:, l, fcc:fcc + 1], ALU.mult, ALU.add,
                         R=[gsk, 'cw'], W=[acck])
                    for (kr, kc) in taps:
                        if (kr, kc) == (1, 1):
                            continue
                        rr = r0 + kr - 1
                        clo = 1 if kc == 0 else 0
                        chi = gc - 1 if kc == 2 else gc
                        P.stt(av[:, :, clo:chi], gv[:, rr:rr + R_, clo + kc - 1:chi + kc - 1], cw[:, l, kr * 3 + kc, fcc:fcc + 1],
                              av[:, :, clo:chi], ALU.mult, ALU.add, R=[gsk, 'cw', acck], W=[acck])
                    if pend is not None:
                        h2_fin(pend)
                    pend = (acc, acck, pa, pak, h2, h2k, fcc)
                    if fcc == 12 and ti + 1 < ntile:
                        fr_n = front(ld_n)
                h2_op(pend)
                h2_fin(pend)
                if final:
                    ss, ssk = fss.next()
                for tb in range(2):
                    for dh in range(2):
                        po, pok = pop.next()
                        for fcc in range(NFC):
                            P.mm(po[:], h2[:, fcc, tb * 128:(tb + 1) * 128], wdn[:, fcc, dh * 512:(dh + 1) * 512], fcc == 0, fcc == NFC - 1,
                                 R=[(h2k, fcc), 'wdn'], W=[pok])
                        tm, tmk = tmpt, 'fe_junk'
                        dsl = slice(dh * 512, (dh + 1) * 512)
                        P.tt('dve', tm[:], po[:], grow[:, dsl], ALU.mult, R=[pok, 'grow2'], W=[tmk])
                        P.tt('pool', xc[:, tb, dsl], tm[:], xc[:, tb, dsl], ALU.add, R=[tmk, xck], W=[xck])
                    if final:
                        P.act(fe.junk_ap, xc[:, tb, :], AF.Square, R=[xck], W=['fe_junk', ssk], accum_out=ss[:, tb:tb + 1])
                if final:
                    P.ts('dve', ss[:], ss[:], 1.0 / D, EPS, ALU.mult, ALU.add, R=[ssk], W=[ssk])
                    P.act(ss[:], ss[:], AF.Sqrt, R=[ssk], W=[ssk])
                    P.recip(ss[:], ss[:], R=[ssk], W=[ssk])
                    for tb in range(2):
                        P.stt(xc[:, tb, :], xc[:, tb, :], ss[:, tb:tb + 1], fgrow[:], ALU.mult, ALU.mult, R=[xck, ssk, 'fgrow'], W=[xck])
                P.dma('sp', xout[t0:t0 + CEN, :].rearrange("(j p) d -> p j d", p=128), xc[:], R=[xck], W=[P.uq('xout')])
                if ti + 1 < ntile:
                    ld, fr = ld_n, fr_n
            P.flush()

    def phase_kc(zsrc, ydst, rndst):
        with ExitStack() as ps:
            sb = lambda n, s, d: ps.enter_context(nc.sbuf_tensor(n, s, d))
            h3c = sb('h3c', [128, 512], F32)
            with ExitStack() as ps2:
                mlp = FilterMLP(ps2)
                mlp.run(K['posC'], 0, 512, h3c[:, 0:512], 'h3c')
                P.flush()
            wo = sb('wo_f', [128, 2, D], F32)
            P.memset('pool', wo[:], 0.0, W=['wo_f'])
            P.dma('sp', wo[0:64, 0, :], W['hy_wo_st'][0:64, :], W=['wo_f'])
            P.dma('sp', wo[64:128, 1, :], W['hy_wo_st'][64:128, :], W=['wo_f'])
            trow = sb('trow', [128, 511], F32)
            P.dma('sp', trow[:], K['tC'][0].partition_broadcast(128), W=['trow'])
            nd = sb('ndelta', [128, 8], F32)
            P.dma('sp', nd[:], K['ndelta'], W=['ndelta'])
            zcp = Pool_(ps, nc, 'zc', [128, TC], F32, 2)
            zbp = Pool_(ps, nc, 'zb', [128, TC], BF16, 2)
            decp = Pool_(ps, nc, 'decc', [128, 511], F32, 2)
            KLp = Pool_(ps, nc, 'KL', [128, 511], F32, 2)
            accp = Pool_(ps, nc, 'accc', [128, TC], F32, 2)
            nrm = sb('nrmc', [128, 8], F32)
            pkc = Pool_(ps, nc, 'pkc', [128, 512], F32, 2, psum=True)
            for k in range(8):
                pk, pkk = pkc.next()
                P.mm(pk[:, 0:256], wo[:, 1, k * 128:(k + 1) * 128], h3c[:, 0:256], True, True, R=['wo_f', 'h3c'], W=[pkk])
                P.mm(pk[:, 255:511], wo[:, 0, k * 128:(k + 1) * 128], h3c[:, 255:511], True, True, R=['wo_f', 'h3c'], W=[pkk])
                dec, deck = decp.next()
                P.act(dec[:], trow[:], AF.Exp, R=['trow', 'ndelta'], W=[deck], scale=nd[:, k:k + 1])
                KL, KLk = KLp.next()
                P.tt('dve', KL[:], pk[:, 0:511], dec[:], ALU.mult, R=[pkk, deck], W=[KLk])
                P.op('dve', lambda e, o=nrm[:, k:k + 1], i_=KL[:]: e.tensor_reduce(out=o, in_=i_, axis=AX.X, op=ALU.add, apply_absolute_value=True),
                     R=[KLk], W=[('nrmc', k)])
                zb, zbk = zbp.next()
                P.dma('sp', zb[:], zsrc[k * 128:(k + 1) * 128, :], W=[zbk])
                zc, zck = zcp.next()
                P.copy('pool', zc[:], zb[:], R=[zbk], W=[zck])
                acc, acck = accp.next()
                P.ts('dve', acc[:], KL[:, 255:511], zc[:, 0:1], None, ALU.mult, None, R=[KLk, zck], W=[acck])
                for s_ in range(1, TC):
                    P.stt(acc[:], KL[:, 255 - s_:511 - s_], zc[:, s_:s_ + 1], acc[:], ALU.mult, ALU.add, R=[KLk, zck, acck], W=[acck])
                P.dma('sp', ydst[k * 128:(k + 1) * 128, :], acc[:], R=[acck], W=[P.uq('ydst')])
            P.recip(nrm[:], nrm[:], R=[('nrmc', k) for k in range(8)], W=['nrmc'])
            P.dma('sp', rndst, nrm[:], R=['nrmc'], W=[P.uq('rndst')])
            P.flush()

    def phase_m1(src, Tn, xmdst, szdst):
        lat = Tn == T
        nblk = 4 if lat else Tn // 128
        TT = nblk * 128
        noc = 32 if lat else 16
        with ExitStack() as ps:
            sb = lambda n, s, d: ps.enter_context(nc.sbuf_tensor(n, s, d))
            win = sb('mwin', [128, 8, 2 * MI], BF16)
            fe = FrontEnd(ps, nblk)
            xtp = Pool_(ps, nc, 'mxt', [128, nblk, D], F32, 2)
            hTp = Pool_(ps, nc, 'mhT', [128, 8, TT], BF16, 2)
            pbp = Pool_(ps, nc, 'mpb', [128, 8, TT], BF16, 2)
            pp = Pool_(ps, nc, 'mpp', [128, TT], F32, 4, psum=True)
            for k in range(8):
                P.dma('pool', win[:, k, :], W['ml_w_in'][k * 128:(k + 1) * 128, :], W=['mwin'])
            m = 0 if lat else 1

            def load(ti):
                xt, xtk = xtp.next()
                P.dma('sp', xt[:], src[ti * TT:(ti + 1) * TT, :].rearrange("(j p) d -> p j d", p=128), W=[xtk])
                return xt, xtk
            ntile = Tn // TT
            ld = load(0)
            for ti in range(ntile):
                t0 = ti * TT
                xt, xtk = ld
                if ti + 1 < ntile:
                    ld = load(ti + 1)
                hT, hTk = hTp.next()
                fe.run(xt, xtk, hT, hTk, 1, 0, m)
                for og in range(noc // 8):
                    pb, pbk = pbp.next()
                    for oi in range(8):
                        oc = og * 8 + oi
                        pt, ptk = pp.next()
                        for k in range(8):
                            P.mm(pt[:], win[:, k, oc * 128:(oc + 1) * 128], hT[:, k, :], k == 0, k == 7, R=['mwin', hTk], W=[ptk])
                        if oc >= 16:
                            P.act(pb[:, oi, :], pt[:], AF.Silu, R=[ptk], W=[pbk])
                        elif oi % 2 == 0:
                            P.copy('act', pb[:, oi, :], pt[:], R=[ptk], W=[pbk])
                        else:
                            P.copy('dve', pb[:, oi, :], pt[:], R=[ptk], W=[pbk])
                    if og < 2:
                        P.dma('sp', xmdst[og * 1024:(og + 1) * 1024, t0:t0 + TT].rearrange("(c p) t -> p c t", p=128), pb[:], R=[pbk], W=[P.uq('xmdst')])
                    else:
                        o2 = og - 2
                        P.dma('sp', szdst[o2 * 1024:(o2 + 1) * 1024, t0:t0 + TT].rearrange("(c p) t -> p c t", p=128), pb[:], R=[pbk], W=[P.uq('szdst')])
            P.flush()

    def phase_m2(xmsrc, Tn, qdst, kdst, ktdst, vtdst, sxdst, gpdst):
        lat = Tn == T
        TT = 512 if lat else Tn
        nblk = TT // 128
        DHS = 512.0 ** -0.5
        with ExitStack() as ps:
            sb = lambda n, s, d: ps.enter_context(nc.sbuf_tensor(n, s, d))
            bd = sb('bd', [128, 3, 16, 128], BF16)
            for j, nm in enumerate(('ml_bdq', 'ml_bdk', 'ml_bdv')):
                P.dma('pool', bd[:, j, :, :], W[nm].rearrange("c p m -> p c m"), W=['bd'])
            wg = sb('wg', [128, 48, 16], BF16)
            P.dma('pool', wg[:], W['ml_w_gate'].rearrange("(c p) n -> p c n", p=128), W=['wg'])
            bgrow = sb('bgrow', [128, 16], F32)
            load_row(bgrow[:], 'bgrow', W['ml_b_gate'])
            cwc = sb('mcw', [128, 3, 16], F32)
            cbc = sb('mcb', [128, 16], F32)
            skc = sb('mskip', [128, 16], F32)
            P.dma('sp', cwc[:], W['ml_conv_w_col'], W=['mcw'])
            P.dma('sp', cbc[:], W['ml_conv_b_col'], W=['mcw'])
            P.dma('sp', skc[:], W['ml_skip_col'], W=['mcw'])
            Um = sb('Um', [128, 3, 128], F32)
            P.dma('sp', Um[:, 0, :], K['U'], W=['Um'])
            P.dma('sp', Um[:, 1, :], K['UT'], W=['Um'])
            P.dma('sp', Um[:, 2, :], K['ones'], W=['Um'])
            xmp = Pool_(ps, nc, 'xmh', [128, 16, TT + 2], BF16, 2)
            xcp = Pool_(ps, nc, 'xcm', [128, 16, TT], BF16, 1)
            sxp = Pool_(ps, nc, 'sxc', [128, 16, TT], BF16, 1)
            accp = Pool_(ps, nc, 'macc', [128, TT], F32, 2)
            qp = Pool_(ps, nc, 'qfm', [128, 16, TT], BF16, 1)
            kp = Pool_(ps, nc, 'kfm', [128, 16, TT], BF16, 1)
            vp = Pool_(ps, nc, 'vfm', [128, 16, TT], BF16, 1)
            ktp = Pool_(ps, nc, 'ktm', [128, nblk, MI], BF16, 1)
            vtp = Pool_(ps, nc, 'vtm', [128, nblk, MI], BF16, 1)
            gpp = Pool_(ps, nc, 'gp', [128, nblk, 4, 8], F32, 2)
            gtp = Pool_(ps, nc, 'gates', [128, 16], F32, 2)
            spp = Pool_(ps, nc, 'spl', [128, 2, 4], F32, 2)
            t8p = Pool_(ps, nc, 'tmp8', [128, 8], F32, 2)
            pfm = Pool_(ps, nc, 'pfm', [128, TT], F32, 2, psum=True)
            ptm = Pool_(ps, nc, 'ptm', [128, 512], F32, 2, psum=True)
            pgp = Pool_(ps, nc, 'pgate', [128, 16], F32, 2, psum=True)
            pbp = Pool_(ps, nc, 'pbcum', [128, 2, 8], F32, 2, psum=True)
            def load(ti):
                t0 = ti * TT
                xm, xmk = xmp.next()
                lo = 1 if ti == 0 else 0
                hi = TT + 1 if ti == Tn // TT - 1 else TT + 2
                if ti == 0:
                    P.memset('pool', xm[:, :, 0:1], 0.0, W=[xmk])
                if ti == Tn // TT - 1:
                    P.memset('pool', xm[:, :, TT + 1:TT + 2], 0.0, W=[xmk])
                P.dma('sp', xm[:, :, lo:hi], xmsrc[:, t0 - 1 + lo:t0 - 1 + hi].rearrange("(c p) t -> p c t", p=128), W=[xmk])
                return xm, xmk
            ld = load(0)
            for ti in range(Tn // TT):
                t0 = ti * TT
                xm, xmk = ld
                if ti + 1 < Tn // TT:
                    ld = load(ti + 1)
                xc, xck = xcp.next()
                sx, sxk = sxp.next()
                for cc in range(16):
                    acc, acck = accp.next()
                    P.act(acc[:], xm[:, cc, 1:TT + 1], AF.Identity, R=[xmk, 'mcw'], W=[acck], scale=cwc[:, 1, cc:cc + 1], bias=cbc[:, cc:cc + 1])
                    P.stt(acc[:], xm[:, cc, 0:TT], cwc[:, 0, cc:cc + 1], acc[:], ALU.mult, ALU.add, R=[xmk, 'mcw', acck], W=[acck])
                    P.stt(acc[:], xm[:, cc, 2:TT + 2], cwc[:, 2, cc:cc + 1], acc[:], ALU.mult, ALU.add, R=[xmk, 'mcw', acck], W=[acck])
                    P.act(xc[:, cc, :], acc[:], AF.Silu, R=[acck], W=[(xck, cc)])
                    if lat:
                        P.act(sx[:, cc, :], xc[:, cc, :], AF.Identity, R=[(xck, cc), 'mcw'], W=[sxk], scale=skc[:, cc:cc + 1])
                if lat:
                    P.dma('sp', sxdst[:, t0:t0 + TT].rearrange("(c p) t -> p c t", p=128), sx[:], R=[sxk], W=[P.uq('sxdst')])
                qf, qfk = qp.next()
                kf, kfk = kp.next()
                vf, vfk = vp.next()
                for cc in range(16):
                    for j, (dst_, dk, srct, srck) in enumerate(((qf, qfk, xc[:, cc, :], (xck, cc)), (kf, kfk, xc[:, cc, :], (xck, cc)),
                                                             (vf, vfk, xm[:, cc, 1:TT + 1], xmk))):
                        pf, pfk = pfm.next()
                        P.mm(pf[:], bd[:, j, cc, :], srct, True, True, R=['bd', srck], W=[pfk])
                        P.copy('act' if (cc + j) % 2 == 0 else 'dve', dst_[:, cc, :], pf[:], R=[pfk], W=[(dk, cc)])
                if lat:
                    P.dma('sp', qdst[:, t0:t0 + TT].rearrange("(c p) t -> p c t", p=128), qf[:], R=[(qfk, c_) for c_ in range(16)], W=[P.uq('qdst')])
                    P.dma('sp', kdst[:, t0:t0 + TT].rearrange("(c p) t -> p c t", p=128), kf[:], R=[(kfk, c_) for c_ in range(16)], W=[P.uq('kdst')])
                kt, ktk = ktp.next()
                vt, vtk = vtp.next()
                for blk in range(nblk):
                    bsl = slice(blk * 128, (blk + 1) * 128)
                    for j, (dst_, dk, which) in enumerate(((kt, ktk, 1), (vt, vtk, 2))):
                        for c4 in range(4):
                            pt, ptk = ptm.next()
                            for i in range(4):
                                cc = c4 * 4 + i
                                lhs = xc[:, cc, bsl] if which == 1 else xm[:, cc, 1 + blk * 128:1 + (blk + 1) * 128]
                                P.mm(pt[:, i * 128:(i + 1) * 128], lhs, bd[:, which, cc, :], True, True,
                                     R=['bd', (xck, cc) if which == 1 else xmk], W=[ptk])
                            P.copy('act' if (c4 + j) % 2 == 0 else 'dve', dst_[:, blk, c4 * 512:(c4 + 1) * 512], pt[:], R=[ptk], W=[dk])
                P.dma('sp', ktdst[t0:t0 + TT, :].rearrange("(j p) n -> p j n", p=128), kt[:], R=[ktk], W=[P.uq('ktdst')])
                P.dma('sp', vtdst[t0:t0 + TT, :].rearrange("(j p) n -> p j n", p=128), vt[:], R=[vtk], W=[P.uq('vtdst')])
                gp, gpk = gpp.next()
                for blk in range(nblk):
                    bsl = slice(blk * 128, (blk + 1) * 128)
                    pg, pgk = pgp.next()
                    n_ = 0
                    for j, (src_, sk) in enumerate(((qf, qfk), (kf, kfk), (vf, vfk))):
                        for cc in range(16):
                            P.mm(pg[:], src_[:, cc, bsl], wg[:, j * 16 + cc, :], n_ == 0, n_ == 47, R=[(sk, cc), 'wg'], W=[pgk])
                            n_ += 1
                    gt, gtk = gtp.next()
                    P.tt('dve', gt[:], pg[:], bgrow[:], ALU.add, R=[pgk, 'bgrow'], W=[gtk])
                    gv = gt[:].rearrange("p (d g h) -> p d g h", d=2, g=2)
                    sp_, spk = spp.next()
                    P.act(sp_[:], gv[:, :, 1, :], AF.Exp, R=[gtk], W=[spk], scale=-1.0)
                    P.act(sp_[:], sp_[:], AF.Ln, R=[spk], W=[spk], bias=1.0)
                    pb, pbk = pbp.next()
                    P.mm(pb[:, 0, 0:4], Um[:, 0, :], sp_[:, 0, :], True, True, R=['Um', spk], W=[pbk])
                    P.mm(pb[:, 0, 4:8], Um[:, 1, :], sp_[:, 1, :], True, True, R=['Um', spk], W=[pbk])
                    P.mm(pb[:, 1, :], Um[:, 2, :], sp_[:].rearrange("p d h -> p (d h)"), True, True, R=['Um', spk], W=[pbk])
                    P.act(gp[:, blk, 0:2, :], pb[:], AF.Exp, R=[pbk], W=[gpk], scale=-1.0)
                    t8, t8k = t8p.next()
                    P.tt('dve', t8[:].rearrange("p (d h) -> p d h", d=2), gv[:, :, 0, :], pb[:, 0, :].rearrange("p (d h) -> p d h", d=2), ALU.add,
                         R=[gtk, pbk], W=[t8k])
                    P.act(gp[:, blk, 2, :], t8[:], AF.Exp, R=[t8k], W=[gpk], bias=float(math.log(DHS)))
                    P.tt('dve', gp[:, blk, 3, :], gp[:, blk, 2, :], gp[:, blk, 1, :], ALU.mult, R=[gpk], W=[gpk])
                P.dma('sp', gpdst[t0:t0 + TT, :, :].rearrange("(j p) a b -> p j a b", p=128), gp[:], R=[gpk], W=[P.uq('gpdst')])
            P.flush()

    def phase_m3():
        with ExitStack() as ps:
            sb = lambda n, s, d: ps.enter_context(nc.sbuf_tensor(n, s, d))
            pdC = ps.enter_context(nc.psum_tensor('pdC', [128, 4, 512], F32))
            Ct = [sb(f'Ct{c}', [128, 4, 512], F32) for c in range(8)]
            Cb = [sb(f'Cb{c}', [128, 4, 512], BF16) for c in range(8)]
            nt = sb('nt', [128, 8, 4], F32)
            nb = sb('nb', [128, 8, 4, 2], BF16)
            maskf = sb('maskf', [128, 2, 128], F32)
            onesb = sb('onesb', [128, 2], BF16)
            P.dma('sp', maskf[:, 0, :], K['U'], W=['maskf'])
            P.dma('sp', maskf[:, 1, :], K['UT'], W=['maskf'])
            P.memset('dve', onesb[:], 1.0, W=['onesb'])
            for c in range(8):
                P.memset('pool', Ct[c][:], 0.0, W=[('Ct', c)])
                P.memset('dve', Cb[c][:], 0.0, W=[('Cb', c)])
            P.memset('dve', nt[:], 0.0, W=[('nt', c) for c in range(8)])
            P.memset('dve', nb[:], 0.0, W=[('nb', c) for c in range(8)])
            qTp = Pool_(ps, nc, 'qT', [128, 4, 128], BF16, 6)
            kTp = Pool_(ps, nc, 'kT', [128, 4, 128], BF16, 6)
            ktp = Pool_(ps, nc, 'ktm3', [128, 512], BF16, 6)
            vtp = Pool_(ps, nc, 'vtm3', [128, 512], BF16, 6)
            gpp = Pool_(ps, nc, 'gp3', [128, 4, 8], F32, 10)
            Stp = Pool_(ps, nc, 'St', [128, 128], BF16, 3)
            k2p = Pool_(ps, nc, 'k2', [128, 512], BF16, 3)
            hop = Pool_(ps, nc, 'hout', [128, 512], F32, 3)
            smp = Pool_(ps, nc, 'sm3', [128, 4], F32, 4)
            pmp = Pool_(ps, nc, 'pmisc', [128, 512], F32, 2, psum=True)
            pnp = Pool_(ps, nc, 'pnum', [128, 512], F32, 2, psum=True)

            gp_tiles = {}

            def get_gp(gpsrc, c):
                key = (id(gpsrc), c)
                if key not in gp_tiles:
                    if len(gp_tiles) >= 6:
                        gp_tiles.pop(next(iter(gp_tiles)))
                    gp, gpk = gpp.next()
                    P.dma('sp', gp[:], gpsrc[c * 128:(c + 1) * 128, :, :], W=[gpk])
                    gp_tiles[key] = (gp, gpk)
                return gp_tiles[key]

            def part_l(h, dr, c, srcs, full):
                ktsrc, vtsrc, gpsrc = srcs
                t0 = c * 128
                col = dr * 4 + h
                gp, gpk = get_gp(gpsrc, c)
                kt, ktk = ktp.next()
                vt, vtk = vtp.next()
                P.dma('sp', kt[:], ktsrc[t0:t0 + 128, h * 512:(h + 1) * 512], W=[ktk])
                P.dma('sp', vt[:], vtsrc[t0:t0 + 128, h * 512:(h + 1) * 512], W=[vtk])
                st = dict(h=h, dr=dr, c=c, gp=gp, gpk=gpk, kt=kt, ktk=ktk, vt=vt, vtk=vtk, col=col, full=full)
                if full:
                    qT, qTk = qTp.next()
                    kT, kTk = kTp.next()
                    P.dma('sp', qT[:], Qfm[h * 512:(h + 1) * 512, t0:t0 + 128].rearrange("(dc p) t -> p dc t", p=128), W=[qTk])
                    P.dma('sp', kT[:], Kfm[h * 512:(h + 1) * 512, t0:t0 + 128].rearrange("(dc p) t -> p dc t", p=128), W=[kTk])
                    st.update(qT=qT, qTk=qTk, kT=kT, kTk=kTk)
                return st

            def part_a(st):
                h, dr, c, col, full = st['h'], st['dr'], st['c'], st['col'], st['full']
                gp, gpk, kt, ktk = st['gp'], st['gpk'], st['kt'], st['ktk']
                pm, pmk = pmp.next()
                st.update(pm=pm, pmk=pmk)
                k2, k2k = k2p.next()
                P.act(k2[:], kt[:], AF.Identity, R=[ktk, gpk], W=[k2k], scale=gp[:, 3, col:col + 1])
                st.update(k2=k2, k2k=k2k)
                if full:
                    qT, qTk, kT, kTk = st['qT'], st['qTk'], st['kT'], st['kTk']
                    pS, pSk = pm[:, 0:128], (pmk, 'S')
                    for dc in range(4):
                        P.mm(pS, kT[:, dc, :], qT[:, dc, :], dc == 0, dc == 3, R=[kTk, qTk], W=[pSk])
                    St, Stk = Stp.next()
                    P.stt(St[:], pS, gp[:, 2, col:col + 1], maskf[:, dr, :], ALU.mult, ALU.mult, R=[pSk, gpk, 'maskf'], W=[Stk])
                    st.update(St=St, Stk=Stk)
                return st

            def part_b(st):
                h, dr, c, col = st['h'], st['dr'], st['c'], st['col']
                ch = h * 2 + dr
                gp, gpk = st['gp'], st['gpk']
                t0 = c * 128
                if st['full']:
                    qT, qTk, St, Stk, vt, vtk = st['qT'], st['qTk'], st['St'], st['Stk'], st['vt'], st['vtk']
                    pn, pnk = pnp.next()
                    P.mm(pn[:], St[:], vt[:], True, False, R=[Stk, vtk], W=[pnk])
                    for dc in range(4):
                        P.mm(pn[:], qT[:, dc, :], Cb[ch][:, dc, :], False, dc == 3, R=[qTk, ('Cb', ch)], W=[pnk])
                    pd, pdk = st['pm'][:, 128:130], (st['pmk'], 'den')
                    P.mm(pd, St[:], onesb[:], True, False, R=[Stk, 'onesb'], W=[pdk])
                    for dc in range(4):
                        P.mm(pd, qT[:, dc, :], nb[:, ch, dc, :], False, dc == 3, R=[qTk, ('nb', ch)], W=[pdk])
                    sm, smk = smp.next()
                    P.act(sm[:, 0:1], st['pm'][:, 128:129], AF.Abs, R=[pdk, gpk], W=[smk], scale=gp[:, 0, col:col + 1])
                    P.ts('dve', sm[:, 0:1], sm[:, 0:1], 1.0, None, ALU.max, None, R=[smk], W=[smk])
                    P.recip(sm[:, 1:2], sm[:, 0:1], R=[smk], W=[smk])
                    P.tt('dve', sm[:, 2:3], sm[:, 1:2], gp[:, 0, col:col + 1], ALU.mult, R=[smk, gpk], W=[smk])
                    ho, hok = hop.next()
                    P.act(ho[:], pn[:], AF.Identity, R=[pnk, smk], W=[hok], scale=sm[:, 2:3])
                    P.dma('act', HFB[dr, t0:t0 + 128, h * 512:(h + 1) * 512], ho[:], R=[hok], W=[P.uq('HFB')])
                k2, k2k, vt, vtk = st['k2'], st['k2k'], st['vt'], st['vtk']
                for dc in range(4):
                    P.mm(pdC[:, dc, :], k2[:, dc * 128:(dc + 1) * 128], vt[:], True, True, R=[k2k, vtk], W=[('pdC', dc)])
                pdn, pdnk = st['pm'][:, 256:264].rearrange("p (a b) -> p a b", b=2), (st['pmk'], 'dn')
                for dc in range(4):
                    P.mm(pdn[:, dc, :], k2[:, dc * 128:(dc + 1) * 128], onesb[:], True, True, R=[k2k, 'onesb'], W=[pdnk])
                glc = gp[:, 1, col:col + 1]
                for dc in range(4):
                    P.stt(Ct[ch][:, dc, :], Ct[ch][:, dc, :], glc, pdC[:, dc, :], ALU.mult, ALU.add, R=[('Ct', ch), gpk, ('pdC', dc)], W=[('Ct', ch)])
                P.copy('act', Cb[ch][:], Ct[ch][:], R=[('Ct', ch)], W=[('Cb', ch)])
                P.stt(nt[:, ch, :], nt[:, ch, :], glc, pdn[:, :, 0], ALU.mult, ALU.add, R=[('nt', ch), gpk, pdnk], W=[('nt', ch)])
                P.copy('pool', nb[:, ch, :, 0], nt[:, ch, :], R=[('nt', ch)], W=[('nb', ch)])
                P.copy('pool', nb[:, ch, :, 1], nt[:, ch, :], R=[('nt', ch)], W=[('nb', ch)])

            sched = []
            for s_ in range(2):
                for h in range(4):
                    for dr in range(2):
                        sched.append((h, dr, s_ if dr == 0 else 1 - s_, (KtmC, VtmC, GPdC), False))
            for s_ in range(64):
                for h in range(4):
                    for dr in range(2):
                        sched.append((h, dr, s_ if dr == 0 else 63 - s_, (Ktm, Vtm, GPd), True))
            LOOK = 3
            loaded = []
            nxt = 0
            prev = None
            for i in range(len(sched)):
                while nxt < len(sched) and nxt <= i + LOOK:
                    loaded.append(part_l(*sched[nxt]))
                    nxt += 1
                cur = part_a(loaded.pop(0))
                if prev is not None:
                    part_b(prev)
                prev = cur
            part_b(prev)
            P.flush()

    def phase_m4(xin, xout):
        with ExitStack() as ps:
            sb = lambda n, s, d: ps.enter_context(nc.sbuf_tensor(n, s, d))
            wdn = sb('mwdn', [128, 16, D], BF16)
            P.dma('pool', wdn[:], W['ml_w_down'].rearrange("(c p) n -> p c n", p=128), W=['mwdn'])
            nwc = sb('nwc', [128, 16], F32)
            P.dma('sp', nwc[:], W['ml_norm_w_col'], W=['nwc'])
            grow = sb('grow4', [128, D], F32)
            load_row(grow[:], 'grow4', MODROW[1, 0, 2048:3072])
            hfp = Pool_(ps, nc, 'hf', [128, MI], F32, 2)
            hbp = Pool_(ps, nc, 'hb', [128, MI], F32, 2)
            hnp = Pool_(ps, nc, 'hn', [128, MI], BF16, 2)
            stp = Pool_(ps, nc, 'bst', [128, 4, 6], F32, 2)
            mvp = Pool_(ps, nc, 'bmv', [128, 4, 2], F32, 2)
            sxp = Pool_(ps, nc, 'sx4', [128, 16, 128], BF16, 2)
            szp = Pool_(ps, nc, 'sz4', [128, 16, 128], BF16, 2)
            m1p = Pool_(ps, nc, 'm14', [128, 128], F32, 3)
            mfp = Pool_(ps, nc, 'mfm', [128, 16, 128], BF16, 2)
            xtp = Pool_(ps, nc, 'xt4', [128, D], F32, 2)
            tmp = Pool_(ps, nc, 'tmp4', [128, 512], F32, 2)
            pTp = Pool_(ps, nc, 'pT4', [128, 4, 128], BF16, 2, psum=True)
            pop = Pool_(ps, nc, 'po4', [128, 512], F32, 2, psum=True)
            def load(blk):
                t0 = blk * 128
                hf, hfk = hfp.next()
                hb, hbk = hbp.next()
                sx, sxk = sxp.next()
                sz, szk = szp.next()
                xt, xtk = xtp.next()
                P.dma('sp', hf[:], HFB[0, t0:t0 + 128, :], W=[hfk])
                P.dma('sp', hb[:], HFB[1, t0:t0 + 128, :], W=[hbk])
                P.dma('sp', sx[:], SXC[:, t0:t0 + 128].rearrange("(c p) t -> p c t", p=128), W=[sxk])
                P.dma('sp', sz[:], SZd[:, t0:t0 + 128].rearrange("(c p) t -> p c t", p=128), W=[szk])
                P.dma('sp', xt[:], xin[t0:t0 + 128, :], W=[xtk])
                return hf, hfk, hb, hbk, sx, sxk, sz, szk, xt, xtk
            ld = load(0)
            for blk in range(T // 128):
                t0 = blk * 128
                hf, hfk, hb, hbk, sx, sxk, sz, szk, xt, xtk = ld
                if blk + 1 < T // 128:
                    ld = load(blk + 1)
                P.tt('dve', hf[:], hf[:], hb[:], ALU.add, R=[hfk, hbk], W=[hfk])
                bst, bstk = stp.next()
                mv, mvk = mvp.next()
                for h in range(4):
                    P.op('dve', lambda e, o=bst[:, h, :], i_=hf[:, h * 512:(h + 1) * 512]: e.bn_stats(out=o, in_=i_), R=[hfk], W=[(bstk, h)])
                    P.op('dve', lambda e, o=mv[:, h, :], i_=bst[:, h, :]: e.bn_aggr(out=o, in_=i_), R=[(bstk, h)], W=[(mvk, h)])
                mvall = [(mvk, h) for h in range(4)]
                P.ts('dve', mv[:, :, 1], mv[:, :, 1], 1e-5, None, ALU.add, None, R=mvall, W=mvall)
                P.act(mv[:, :, 1], mv[:, :, 1], AF.Sqrt, R=mvall, W=mvall)
                P.recip(mv[:, :, 1], mv[:, :, 1], R=mvall, W=mvall)
                hn, hnk = hnp.next()
                for h in range(4):
                    P.ts('dve', hn[:, h * 512:(h + 1) * 512], hf[:, h * 512:(h + 1) * 512], mv[:, h, 0:1], mv[:, h, 1:2], ALU.subtract, ALU.mult,
                         R=[hfk] + mvall, W=[(hnk, h)])
                mf, mfk = mfp.next()
                for c4 in range(4):
                    pT, pTk = pTp.next()
                    for i in range(4):
                        cc = c4 * 4 + i
                        P.tr(pT[:, i, :], hn[:, cc * 128:(cc + 1) * 128], identb[:], R=[(hnk, cc // 4), 'identb'], W=[pTk])
                    for i in range(4):
                        cc = c4 * 4 + i
                        m1, m1k = m1p.next()
                        P.stt(m1[:], pT[:, i, :], nwc[:, cc:cc + 1], sx[:, cc, :], ALU.mult, ALU.add, R=[pTk, 'nwc', sxk], W=[m1k])
                        P.tt('pool', mf[:, cc, :], m1[:], sz[:, cc, :], ALU.mult, R=[m1k, szk], W=[(mfk, cc)])
                for dh in range(2):
                    po, pok = pop.next()
                    for cc in range(16):
                        P.mm(po[:], mf[:, cc, :], wdn[:, cc, dh * 512:(dh + 1) * 512], cc == 0, cc == 15, R=[(mfk, cc), 'mwdn'], W=[pok])
                    tm, tmk = tmp.next()
                    dsl = slice(dh * 512, (dh + 1) * 512)
                    P.tt('dve', tm[:], po[:], grow[:, dsl], ALU.mult, R=[pok, 'grow4'], W=[tmk])
                    P.tt('pool', xt[:, dsl], tm[:], xt[:, dsl], ALU.add, R=[tmk, xtk], W=[xtk])
                P.dma('sp', xout[t0:t0 + 128, :], xt[:], R=[xtk], W=[P.uq('xout')])
            P.flush()
    only = stop_after
    run = lambda name: (only is None) or (name in only)
    phase_adaln()
    if run('lat0'):
        phase_h1(x_d, T, P0)
        phase_h2(P0, T, Zd, X0d)
        phase_k()
        phase_z()
        phase_h5(Ytd, Zd, X0d, x_d, XA0, T, 0, RNd)
        phase_ffn(0, XA0, XB0, T, 0)
    if run('ctx0') or run('c1'):
        phase_h1(ctx_d, TC, P0C)
    if run('ctx0') or run('c2'):
        phase_h2(P0C, TC, ZdC, X0dC)
    if run('ctx0') or run('c3'):
        phase_kc(ZdC, YtC, RNdC)
    if run('ctx0') or run('c4'):
        phase_h5(YtC, ZdC, X0dC, ctx_d, CA0, TC, 1, RNdC)
    if run('ctx0') or run('c5'):
        phase_ffn(0, CA0, CB0, TC, 1)
    if run('m1'):
        phase_m1(XB0, T, XMd, SZd)
        phase_m1(CB0, TC, XMC, None)
    if run('m2'):
        phase_m2(XMd, T, Qfm, Kfm, Ktm, Vtm, SXC, GPd)
        phase_m2(XMC, TC, None, None, KtmC, VtmC, None, GPdC)
    if run('m3'):
        phase_m3()
    if run('m4'):
        phase_m4(XB0, XA1)
    if run('f1'):
        phase_ffn(1, XA1, out_d, T, 0, final=True)
    return nc_real, cx


def _blockdiag(w):
    out = np.zeros((16, 128, 128), dtype=np.float32)
    for ch in range(16):
        for n in range(32):
            out[ch, 4 * n:4 * n + 4, 4 * n:4 * n + 4] = w[32 * ch + n]
    return out


def prep_shared(inputs):
    f = lambda k: np.asarray(inputs[k], dtype=np.float32)
    S = {}
    for k in ('mod_w', 'mod_b', 'norm_g', 'ffn_w_up', 'ffn_conv_w', 'ffn_conv_b', 'ffn_w_down'):
        S[k] = f(k)
    S['final_g'] = f('final_g').reshape(1, D)
    for k in ('hy_w_in', 'hy_b_in', 'hy_sc_w', 'hy_sc_b', 'hy_bias', 'hy_w_out', 'hy_b_out', 'ml_w_in', 'ml_conv_w', 'ml_conv_b',
              'ml_w_gate', 'ml_b_gate', 'ml_norm_w', 'ml_skip', 'ml_w_down'):
        S[k] = f(k)[0]
    w1, w2, w3 = f('hy_f_w1')[0], f('hy_f_w2')[0], f('hy_f_w3')[0]
    S['hy_w1d'] = np.concatenate([w1, w1], axis=1)
    z = np.zeros((64, 64), np.float32)
    S['hy_w2bd'] = np.block([[w2, z], [z, w2]])
    S['hy_w3bd'] = np.block([[w3, z], [z, w3]])
    dup = lambda v: np.concatenate([v, v]).reshape(128, 1)
    S['hy_b1d'] = dup(f('hy_f_b1')[0])
    S['hy_b2d'] = dup(f('hy_f_b2')[0])
    S['hy_b3d'] = dup(f('hy_f_b3')[0])
    S['hy_freqd'] = dup(f('hy_freq')[0])
    wo = f('hy_f_wout')[0]
    S['hy_wo_st'] = np.concatenate([wo[:, :D], wo[:, D:]], axis=0)
    def col(v):
        v = np.asarray(v, dtype=np.float32)
        n = v.shape[-1] // 128
        v = v.reshape(v.shape[:-1] + (n, 128))
        return np.moveaxis(v, -1, 0)
    S['mod_b_col'] = col(S['mod_b'])
    S['norm_g_col'] = col(S['norm_g'])
    S['hy_b_in_col'] = col(S['hy_b_in'])
    S['hy_sc_w_col'] = col(S['hy_sc_w'])
    S['hy_sc_b_col'] = col(S['hy_sc_b'])
    S['hy_bias_col'] = col(S['hy_bias'])
    S['ml_conv_w_col'] = col(S['ml_conv_w'])
    S['ml_conv_b_col'] = col(S['ml_conv_b'])
    S['ml_norm_w_col'] = col(S['ml_norm_w'])
    S['ml_skip_col'] = col(S['ml_skip'])
    S['ffn_conv_w_col'] = col(S['ffn_conv_w'].reshape(2, 9, FH))
    S['ffn_conv_b_col'] = col(S['ffn_conv_b'])
    S['ml_bdq'] = _blockdiag(f('ml_wq')[0])
    S['ml_bdk'] = _blockdiag(f('ml_wk')[0])
    S['ml_bdv'] = _blockdiag(f('ml_wv')[0])
    return {k: np.ascontiguousarray(v, dtype=np.float32) for k, v in S.items()}


def prep_core(inputs, S, b):
    d = dict(S)
    d['x'] = np.ascontiguousarray(inputs['x'][b], dtype=np.float32)
    d['ctx'] = np.ascontiguousarray(inputs['ctx'][b], dtype=np.float32)
    d['cvec'] = np.ascontiguousarray(np.stack([inputs['c'][b], inputs['c_ctx']], axis=1), dtype=np.float32)
    return d


def kernel(**inputs):
    S = prep_shared(inputs)
    cores = [prep_core(inputs, S, b % 4) for b in range(8)]
    nc, cx = build(cores[0])
    HC = host_consts()
    in_maps = []
    for c in cores:
        m = dict(c)
        for k, v in HC.items():
            m['c_' + k] = v
        in_maps.append(m)
    res = run_bass_kernel_spmd(nc, in_maps, core_ids=list(range(8)))
    out = np.stack([np.asarray(res.results[b]['out'], dtype=np.float32) for b in range(4)], axis=0)
    return out
```
